# Optimizing a Trainium2 kernel written in Bass

```python
import math
import functools
import numpy as np
import jax
import jax.numpy as jnp
from jax import lax

D_MODEL = 2048
BATCH = 2
SEQ = 4096
DEPTH = 2

GRID_W = 64
CTX_LEN = 256
EPS = 1e-6

D_MIX = D_MODEL
N_MIXERS = 4
GROUP_W = D_MIX // N_MIXERS
HEAD_DIM = 128

ATT_HEADS = GROUP_W // HEAD_DIM
ATT_KV_HEADS = 2
ROPE_THETA = 10000.0
Q_BLOCK = 128

NA_HEADS = GROUP_W // HEAD_DIM
NA_ROWS = 8
NA_COLS = 16

SGU_CHUNK = 128
SGU_GROUPS = 4
SGU_GROUP_W = GROUP_W // SGU_GROUPS

SSM_HEAD_DIM = 64
SSM_HEADS = GROUP_W // SSM_HEAD_DIM
SSM_GROUPS = 2
SSM_HPG = SSM_HEADS // SSM_GROUPS
SSM_STATE = 128
SSM_CHUNK = 128
CONV_K = 5
CONV_CH = GROUP_W + 2 * SSM_GROUPS * SSM_STATE

D_FF = 5632
N_EXPERTS = 8
TOP_K = 2
D_FF_EXPERT = 7168
MOE_BLOCK = 128
N_DENSE = (DEPTH + 1) // 2
N_MOE = DEPTH // 2

Q_SIDE = (ATT_HEADS * HEAD_DIM, NA_HEADS * HEAD_DIM, GROUP_W, GROUP_W, GROUP_W, SSM_GROUPS * SSM_STATE)
K_SIDE = (ATT_KV_HEADS * HEAD_DIM, ATT_KV_HEADS * HEAD_DIM, NA_HEADS * HEAD_DIM, NA_HEADS * HEAD_DIM,
          GROUP_W, SSM_GROUPS * SSM_STATE, 2 * SSM_HEADS)
Q_COLS = sum(Q_SIDE)
IN_COLS = Q_COLS + sum(K_SIDE)

kernel_name = 'hymba_style_diffusion_hybrid_trunk'


def rms_norm(x, g):
    xf = x.astype(jnp.float32)
    y = xf * lax.rsqrt(jnp.mean(xf * xf, axis=-1, keepdims=True) + EPS)
    return (y * g.astype(jnp.float32)).astype(x.dtype)


def split_cols(t, widths):
    cuts = [int(v) for v in np.cumsum(widths)[:-1]]
    return jnp.split(t, cuts, axis=-1)


def heads(t, n):
    return t.reshape(t.shape[:-1] + (n, t.shape[-1] // n))


def modulation(cond, w_mod, b_mod, n):
    m = jax.nn.silu(cond) @ w_mod[:, : n * D_MODEL] + b_mod[: n * D_MODEL]
    return jnp.split(m[..., None, :], n, axis=-1)


def axial_rope(n_tokens):
    t = jnp.arange(n_tokens)
    row = (t // GRID_W).astype(jnp.float32)
    col = (t % GRID_W).astype(jnp.float32)
    n_freq = HEAD_DIM // 4
    inv = jnp.power(ROPE_THETA, -jnp.arange(n_freq, dtype=jnp.float32) / n_freq)
    ang = jnp.concatenate([row[:, None] * inv, col[:, None] * inv], axis=-1)
    return jnp.cos(ang), jnp.sin(ang)


def apply_rope(x, cos, sin):
    xf = x.astype(jnp.float32).reshape(x.shape[:-1] + (HEAD_DIM // 2, 2))
    x0, x1 = xf[..., 0], xf[..., 1]
    cs, sn = cos[None, :, None, :], sin[None, :, None, :]
    out = jnp.stack([x0 * cs - x1 * sn, x0 * sn + x1 * cs], axis=-1)
    return out.reshape(x.shape).astype(x.dtype)


def gqa_block_attention(q, k, v):
    bsz, lq, g, r, dh = q.shape
    nb = lq // Q_BLOCK
    qb = jnp.moveaxis(q.reshape(bsz, nb, Q_BLOCK, g, r, dh), 1, 0)
    scale = dh ** -0.5

    def one_block(qi):
        s = jnp.einsum('bqgrd,bkgd->bgrqk', qi, k).astype(jnp.float32) * scale
        p = jax.nn.softmax(s, axis=-1).astype(v.dtype)
        return jnp.einsum('bgrqk,bkgd->bqgrd', p, v)

    o = lax.map(one_block, qb)
    return jnp.moveaxis(o, 0, 1).reshape(bsz, lq, g * r * dh)


def neighbourhood_attention(q, k, v, k_ctx, v_ctx, rpb):
    bsz, s_len, n_h, dh = q.shape
    rows = s_len // GRID_W
    kh = min(NA_ROWS, rows)
    n_cb = GRID_W // NA_COLS
    band = 2 * NA_COLS
    n_loc = kh * band
    scale = dh ** -0.5
    qcol = jnp.arange(GRID_W).reshape(n_cb, NA_COLS)
    win_c0 = jnp.clip(qcol - NA_COLS // 2, 0, GRID_W - NA_COLS)
    band_c0 = jnp.clip(jnp.arange(n_cb) * NA_COLS - NA_COLS // 2, 0, GRID_W - band)
    kcol = band_c0[:, None] + jnp.arange(band)
    col_ok = (kcol[:, None, :] >= win_c0[:, :, None]) & (kcol[:, None, :] < win_c0[:, :, None] + NA_COLS)
    mask = jnp.broadcast_to(col_ok[:, :, None, :], (n_cb, NA_COLS, kh, band)).reshape(n_cb, NA_COLS, n_loc)
    dcol = jnp.clip(kcol[:, None, :] - qcol[:, :, None], 1 - NA_COLS, NA_COLS - 1) + NA_COLS - 1
    rpb32 = rpb.astype(jnp.float32)
    q_rows = jnp.moveaxis(q.reshape(bsz, rows, GRID_W, n_h, dh), 1, 0)

    def one_row(args):
        r, q_r = args
        krow = jnp.clip(r - kh // 2, 0, rows - kh) + jnp.arange(kh)
        idx = (krow[None, :, None] * GRID_W + kcol[:, None, :]).reshape(n_cb, n_loc)
        kb = jnp.take(k, idx, axis=1)
        vb = jnp.take(v, idx, axis=1)
        drow = krow - r + NA_ROWS - 1
        bias = rpb32[:, drow[None, None, :, None], dcol[:, :, None, :]].reshape(n_h, n_cb, NA_COLS, n_loc)
        qb = q_r.reshape(bsz, n_cb, NA_COLS, n_h, dh)
        s_loc = jnp.einsum('bnqhd,bnkhd->bhnqk', qb, kb).astype(jnp.float32) * scale + bias
        s_loc = jnp.where(mask, s_loc, -1e30)
        s_ctx = jnp.einsum('bnqhd,bchd->bhnqc', qb, k_ctx).astype(jnp.float32) * scale
        p = jax.nn.softmax(jnp.concatenate([s_loc, s_ctx], axis=-1), axis=-1).astype(v.dtype)
        o = (jnp.einsum('bhnqk,bnkhd->bnqhd', p[..., :n_loc], vb)
             + jnp.einsum('bhnqc,bchd->bnqhd', p[..., n_loc:], v_ctx))
        return o.reshape(bsz, GRID_W, n_h, dh)

    o = lax.map(one_row, (jnp.arange(rows), q_rows))
    return jnp.moveaxis(o, 0, 1).reshape(bsz, s_len, n_h * dh)


def spatial_gating_unit(u, v, norm_g, w_s, b_s):
    bsz, length, _ = v.shape
    n = length // SGU_CHUNK
    vn = rms_norm(v, norm_g).reshape(bsz, n, SGU_CHUNK, SGU_GROUPS, SGU_GROUP_W)
    mixed = jnp.einsum('gij,bnjgc->bnigc', w_s, vn) + b_s.T[:, :, None]
    return u * mixed.reshape(bsz, length, GROUP_W)


def depthwise_conv(t, w, b):
    y = lax.conv_general_dilated(t, w[:, None, :].astype(t.dtype), window_strides=(1,),
                                 padding=[(CONV_K // 2, CONV_K // 2)],
                                 dimension_numbers=('NWC', 'WIO', 'NWC'),
                                 feature_group_count=t.shape[-1])
    return y + b


def ssm_inputs(dx, db, dc, ddt, conv_w, conv_b, dt_bias):
    parts = [dx, db] if dc is None else [dx, db, dc]
    xbc = jnp.concatenate(parts, axis=-1)
    n_ch = xbc.shape[-1]
    xbc = jax.nn.silu(depthwise_conv(xbc, conv_w[:, :n_ch], conv_b[:n_ch]))
    bsz, length = dx.shape[:2]
    bc_w = SSM_GROUPS * SSM_STATE
    xs = xbc[..., :GROUP_W].reshape(bsz, length, SSM_GROUPS, SSM_HPG, SSM_HEAD_DIM)
    bm = xbc[..., GROUP_W:GROUP_W + bc_w].reshape(bsz, length, SSM_GROUPS, SSM_STATE)
    cm = None if dc is None else xbc[..., GROUP_W + bc_w:].reshape(bsz, length, SSM_GROUPS, SSM_STATE)
    dt = jax.nn.softplus(ddt.astype(jnp.float32).reshape(bsz, length, 2, SSM_GROUPS, SSM_HPG)
                         + dt_bias.astype(jnp.float32).reshape(2, SSM_GROUPS, SSM_HPG))
    return xs, bm, cm, dt


def ssd_chunked(x, dt, a, bm, cm, h0):
    bsz, length, g, r, p = x.shape
    nc = length // SSM_CHUNK

    def chunks(t):
        return t.astype(jnp.float32).reshape((bsz, nc, SSM_CHUNK) + t.shape[2:])

    xc, dtc, bc, cc = chunks(x), chunks(dt), chunks(bm), chunks(cm)
    cum = jnp.cumsum(dtc * a, axis=2)
    xdt = xc * dtc[..., None]
    tri = jnp.tril(jnp.ones((SSM_CHUNK, SSM_CHUNK), dtype=bool))
    seg = jnp.where(tri[None, None, :, :, None, None], cum[:, :, :, None] - cum[:, :, None, :], -jnp.inf)
    scores = jnp.einsum('bcign,bcjgn->bcijg', cc, bc)
    y_diag = jnp.einsum('bcijg,bcijgr,bcjgrp->bcigrp', scores, jnp.exp(seg), xdt)
    decay_end = jnp.exp(cum[:, :, -1:] - cum)
    states = jnp.einsum('bcjgn,bcjgr,bcjgrp->bcgrpn', bc, decay_end, xdt)
    chunk_decay = jnp.exp(cum[:, :, -1])

    def carry_step(h, inp):
        s, dcy = inp
        return h * dcy[..., None, None] + s, h

    h_last, h_in = lax.scan(carry_step, h0.astype(jnp.float32),
                            (jnp.moveaxis(states, 1, 0), jnp.moveaxis(chunk_decay, 1, 0)))
    h_in = jnp.moveaxis(h_in, 0, 1)
    y_off = jnp.einsum('bcign,bcgrpn,bcigr->bcigrp', cc, h_in, jnp.exp(cum))
    return (y_diag + y_off).reshape(bsz, length, g, r, p), h_last


def ssd_final_state(x, dt, a, bm):
    dt = dt.astype(jnp.float32)
    cum = jnp.cumsum(dt * a, axis=1)
    w = jnp.exp(cum[:, -1:] - cum) * dt
    return jnp.einsum('blgn,blgr,blgrp->bgrpn', bm.astype(jnp.float32), w, x.astype(jnp.float32))


def orient(t, d):
    return jnp.flip(t, axis=1) if d == 1 else t


def bidirectional_ssd(lat, ctx, a_log, d_skip, ctx_out):
    x_l, b_l, c_l, dt_l = lat
    x_c, b_c, c_c, dt_c = ctx
    h0 = jnp.zeros((x_l.shape[0], SSM_GROUPS, SSM_HPG, SSM_HEAD_DIM, SSM_STATE), jnp.float32)
    y_l, y_c = 0.0, 0.0
    for d in range(2):
        a = -jnp.exp(a_log[d].astype(jnp.float32)).reshape(SSM_GROUPS, SSM_HPG)
        skip = d_skip[d].astype(jnp.float32).reshape(SSM_GROUPS, SSM_HPG, 1)
        if ctx_out:
            yc, h_c = ssd_chunked(orient(x_c, d), orient(dt_c[:, :, d], d), a, orient(b_c, d), orient(c_c, d), h0)
            y_c = y_c + orient(yc, d) + skip * x_c.astype(jnp.float32)
        else:
            h_c = ssd_final_state(orient(x_c, d), orient(dt_c[:, :, d], d), a, orient(b_c, d))
        yl, _ = ssd_chunked(orient(x_l, d), orient(dt_l[:, :, d], d), a, orient(b_l, d), orient(c_l, d), h_c)
        y_l = y_l + orient(yl, d) + skip * x_l.astype(jnp.float32)
    return y_l, (y_c if ctx_out else None)


def merge_groups(outs, out_norm, w_out):
    normed = [rms_norm(o, out_norm[g * GROUP_W:(g + 1) * GROUP_W]) for g, o in enumerate(outs)]
    return jnp.concatenate(normed, axis=-1) @ w_out


def swiglu(h, w13, w2):
    gate, up = jnp.split(h @ w13, 2, axis=-1)
    return (jax.nn.silu(gate) * up) @ w2


def moe_swiglu(h, router, w13, w2):
    lead = h.shape[:-1]
    t = h.reshape(-1, h.shape[-1])
    n_tok = t.shape[0]
    n_assign = n_tok * TOP_K
    n_blocks = -(-n_assign // MOE_BLOCK) + N_EXPERTS
    n_slots = n_blocks * MOE_BLOCK
    logits = (t @ router).astype(jnp.float32)
    top_v, top_e = lax.top_k(logits, TOP_K)
    top_w = jax.nn.softmax(top_v, axis=-1).reshape(-1)
    flat_e = top_e.reshape(-1)
    order = jnp.argsort(flat_e * n_assign + jnp.arange(n_assign))
    sorted_e = flat_e[order]
    counts = jnp.zeros((N_EXPERTS,), jnp.int32).at[flat_e].add(1)
    padded = (counts + MOE_BLOCK - 1) // MOE_BLOCK * MOE_BLOCK
    seg_start = jnp.cumsum(counts) - counts
    pad_end = jnp.cumsum(padded)
    dest = pad_end[sorted_e] - padded[sorted_e] + jnp.arange(n_assign) - seg_start[sorted_e]
    slot_tok = jnp.zeros((n_slots,), jnp.int32).at[dest].set(order // TOP_K)
    slot_w = jnp.zeros((n_slots,), jnp.float32).at[dest].set(top_w[order])
    block_e = jnp.minimum(jnp.searchsorted(pad_end, jnp.arange(n_blocks) * MOE_BLOCK, side='right'), N_EXPERTS - 1)
    xs = t[slot_tok].reshape(n_blocks, MOE_BLOCK, t.shape[-1])

    def expert_block(args):
        e, xb = args
        return swiglu(xb, w13[e], w2[e])

    ys = lax.map(expert_block, (block_e, xs)).reshape(n_slots, -1)
    out = jnp.zeros_like(t).at[slot_tok].add(ys * slot_w[:, None].astype(ys.dtype))
    return out.reshape(lead + (t.shape[-1],))


def hybrid_layer(x, xc, c, c_ctx, rope, ffn, ctx_out, w_mod, b_mod, norm_mix, norm_ffn, w_in, q_norm, k_norm,
                 rpb, sgu_norm, sgu_w, sgu_b, conv_w, conv_b, a_log, dt_bias, d_skip, out_norm, w_out):
    cos, sin = rope
    bsz, s_len, _ = x.shape
    rep = ATT_HEADS // ATT_KV_HEADS
    sh_l, sc_l, g_l, sh2_l, sc2_l, g2_l = modulation(c, w_mod, b_mod, 6)
    mod_c = modulation(c_ctx, w_mod, b_mod, 6 if ctx_out else 2)
    h_l = rms_norm(x, norm_mix) * (1 + sc_l) + sh_l
    h_c = rms_norm(xc, norm_mix) * (1 + mod_c[1]) + mod_c[0]

    (aq_l, bq_l, cu_l, cv_l, dz_l, dc_l, ak_l, av_l, bk_l, bv_l, dx_l, db_l, ddt_l) = split_cols(h_l @ w_in, Q_SIDE + K_SIDE)
    if ctx_out:
        (aq_c, bq_c, cu_c, cv_c, dz_c, dc_c, ak_c, av_c, bk_c, bv_c, dx_c, db_c, ddt_c) = split_cols(h_c @ w_in, Q_SIDE + K_SIDE)
    else:
        (ak_c, av_c, bk_c, bv_c, dx_c, db_c, ddt_c) = split_cols(h_c @ w_in[:, Q_COLS:], K_SIDE)
        dc_c = None

    qa_l = apply_rope(rms_norm(heads(aq_l, ATT_HEADS), q_norm), cos, sin)
    ka_l = apply_rope(rms_norm(heads(ak_l, ATT_KV_HEADS), k_norm), cos, sin)
    ka_c = rms_norm(heads(ak_c, ATT_KV_HEADS), k_norm)
    va_c = heads(av_c, ATT_KV_HEADS)
    o_a_l = gqa_block_attention(qa_l.reshape(bsz, s_len, ATT_KV_HEADS, rep, HEAD_DIM),
                                jnp.concatenate([ka_c, ka_l], axis=1),
                                jnp.concatenate([va_c, heads(av_l, ATT_KV_HEADS)], axis=1))

    kb_c, vb_c = heads(bk_c, NA_HEADS), heads(bv_c, NA_HEADS)
    o_b_l = neighbourhood_attention(heads(bq_l, NA_HEADS), heads(bk_l, NA_HEADS), heads(bv_l, NA_HEADS),
                                    kb_c, vb_c, rpb)

    o_c_l = spatial_gating_unit(jax.nn.gelu(cu_l), jax.nn.gelu(cv_l), sgu_norm, sgu_w, sgu_b)

    y_l, y_c = bidirectional_ssd(ssm_inputs(dx_l, db_l, dc_l, ddt_l, conv_w, conv_b, dt_bias),
                                 ssm_inputs(dx_c, db_c, dc_c, ddt_c, conv_w, conv_b, dt_bias),
                                 a_log, d_skip, ctx_out)
    o_d_l = y_l.reshape(bsz, s_len, GROUP_W).astype(x.dtype) * jax.nn.silu(dz_l)

    x = x + g_l * merge_groups((o_a_l, o_b_l, o_c_l, o_d_l), out_norm, w_out)
    x = x + g2_l * ffn(rms_norm(x, norm_ffn) * (1 + sc2_l) + sh2_l)
    if not ctx_out:
        return x, None

    n_ctx = xc.shape[1]
    o_a_c = gqa_block_attention(rms_norm(heads(aq_c, ATT_HEADS), q_norm).reshape(bsz, n_ctx, ATT_KV_HEADS, rep, HEAD_DIM),
                                ka_c, va_c)
    o_b_c = gqa_block_attention(heads(bq_c, NA_HEADS)[:, :, :, None, :], kb_c, vb_c)
    o_c_c = spatial_gating_unit(jax.nn.gelu(cu_c), jax.nn.gelu(cv_c), sgu_norm, sgu_w, sgu_b)
    o_d_c = y_c.reshape(bsz, n_ctx, GROUP_W).astype(xc.dtype) * jax.nn.silu(dz_c)
    xc = xc + mod_c[2] * merge_groups((o_a_c, o_b_c, o_c_c, o_d_c), out_norm, w_out)
    xc = xc + mod_c[5] * ffn(rms_norm(xc, norm_ffn) * (1 + mod_c[4]) + mod_c[3])
    return x, xc


def setup_inputs(seed: int = 0) -> dict:
    key = jax.random.key(seed)
    ks = jax.random.split(key, 28)
    d = D_MODEL
    f32 = jnp.float32

    def normal(k, shape, scale):
        return scale * jax.random.normal(k, shape, f32)

    def gain(k, shape):
        return 1.0 + 0.05 * jax.random.normal(k, shape, f32)

    dt0 = jnp.exp(jax.random.uniform(ks[17], (DEPTH, 2, SSM_HEADS), f32, math.log(1e-3), math.log(1e-1)))
    return {
        'x': normal(ks[0], (BATCH, SEQ, d), 1.0),
        'c': normal(ks[1], (BATCH, d), 1.0),
        'ctx': normal(ks[2], (BATCH, CTX_LEN, d), 1.0),
        'c_ctx': normal(ks[3], (d,), 1.0),
        'w_mod': normal(ks[4], (DEPTH, d, 6 * d), 0.5 * d ** -0.5),
        'b_mod': normal(ks[5], (DEPTH, 6 * d), 0.02),
        'norm_mix': gain(ks[6], (DEPTH, d)),
        'norm_ffn': gain(ks[7], (DEPTH, d)),
        'w_in': normal(ks[8], (DEPTH, d, IN_COLS), d ** -0.5),
        'q_norm': gain(ks[9], (DEPTH, HEAD_DIM)),
        'k_norm': gain(ks[10], (DEPTH, HEAD_DIM)),
        'rpb': normal(ks[11], (DEPTH, NA_HEADS, 2 * NA_ROWS - 1, 2 * NA_COLS - 1), 0.1),
        'sgu_norm': gain(ks[12], (DEPTH, GROUP_W)),
        'sgu_w': normal(ks[13], (DEPTH, SGU_GROUPS, SGU_CHUNK, SGU_CHUNK), SGU_CHUNK ** -0.5),
        'sgu_b': 1.0 + normal(ks[14], (DEPTH, SGU_GROUPS, SGU_CHUNK), 0.1),
        'conv_w': normal(ks[15], (DEPTH, CONV_K, CONV_CH), CONV_K ** -0.5),
        'conv_b': normal(ks[16], (DEPTH, CONV_CH), 0.02),
        'a_log': jnp.log(jax.random.uniform(ks[18], (DEPTH, 2, SSM_HEADS), f32, 1.0, 16.0)),
        'dt_bias': dt0 + jnp.log(-jnp.expm1(-dt0)),
        'd_skip': gain(ks[19], (DEPTH, 2, SSM_HEADS)),
        'out_norm': gain(ks[20], (DEPTH, D_MIX)),
        'w_out': normal(ks[21], (DEPTH, D_MIX, d), D_MIX ** -0.5),
        'ffn_w13': normal(ks[22], (N_DENSE, d, 2 * D_FF), d ** -0.5),
        'ffn_w2': normal(ks[23], (N_DENSE, D_FF, d), D_FF ** -0.5),
        'router': normal(ks[24], (N_MOE, d, N_EXPERTS), d ** -0.5),
        'moe_w13': normal(ks[25], (N_MOE, N_EXPERTS, d, 2 * D_FF_EXPERT), d ** -0.5),
        'moe_w2': normal(ks[26], (N_MOE, N_EXPERTS, D_FF_EXPERT, d), D_FF_EXPERT ** -0.5),
        'final_norm': gain(ks[27], (d,)),
    }


def reference(x, c, ctx, c_ctx, w_mod, b_mod, norm_mix, norm_ffn, w_in, q_norm, k_norm, rpb, sgu_norm, sgu_w,
              sgu_b, conv_w, conv_b, a_log, dt_bias, d_skip, out_norm, w_out, ffn_w13, ffn_w2, router, moe_w13,
              moe_w2, final_norm):
    rope = axial_rope(x.shape[1])
    xc = ctx
    for i in range(DEPTH):
        if i % 2 == 0:
            ffn = functools.partial(swiglu, w13=ffn_w13[i // 2], w2=ffn_w2[i // 2])
        else:
            ffn = functools.partial(moe_swiglu, router=router[i // 2], w13=moe_w13[i // 2], w2=moe_w2[i // 2])
        x, xc = hybrid_layer(x, xc, c, c_ctx, rope, ffn, i < DEPTH - 1, w_mod[i], b_mod[i], norm_mix[i],
                             norm_ffn[i], w_in[i], q_norm[i], k_norm[i], rpb[i], sgu_norm[i], sgu_w[i], sgu_b[i],
                             conv_w[i], conv_b[i], a_log[i], dt_bias[i], d_skip[i], out_norm[i], w_out[i])
    return rms_norm(x, final_norm)
```

```python
import numpy as np
from contextlib import ExitStack
import concourse.bass as bass
import concourse.mybir as mybir
from concourse.bass_utils import run_bass_kernel_spmd

F32 = mybir.dt.float32
BF16 = mybir.dt.bfloat16
AF = mybir.ActivationFunctionType
ALU = mybir.AluOpType
AX = mybir.AxisListType
NCORES = 8


class Buf:
    __slots__ = ("name", "w", "r")

    def __init__(self, name=""):
        self.name = name
        self.w = None
        self.r = {}


class _Op:
    __slots__ = ("eng", "fn", "deps", "sig", "val", "dma", "key")


class Prog:
    ENGS = ("pe", "act", "dve", "pool", "sp")

    def __init__(self, nc):
        self.nc = nc
        self.ops = []
        self.last = {}
        self.dmas = []
        self.bar = {}

    def barrier(self):
        deps = list(self.last.values()) + list(self.dmas)
        for d in deps:
            d.sig = True
        self.dmas = []
        for e in self.ENGS:
            self.bar[e] = list(self.bar.get(e, [])) + deps

    def add(self, eng, fn, reads=(), writes=(), dma=False):
        op = _Op()
        op.eng, op.fn, op.dma, op.sig, op.val = eng, fn, dma, dma, 0
        deps = set()
        for b in reads:
            if b.w is not None:
                deps.add(b.w)
        for b in writes:
            if b.w is not None:
                deps.add(b.w)
            for r in b.r.values():
                deps.add(r)
        key = (eng, dma)
        for b in reads:
            b.r[key] = op
        for b in writes:
            b.w = op
            b.r = {}
        deps.discard(op)
        if eng == "pe" and not dma:
            deps = {d for d in deps if not (d.eng == "pe" and not d.dma)}
        if self.bar.get(eng):
            deps.update(d for d in self.bar[eng] if not (d.eng == eng and not d.dma and eng == "pe"))
            self.bar[eng] = []
        if dma:
            self.dmas.append(op)
        else:
            self.last[eng] = op
        op.deps = deps
        for d in deps:
            d.sig = True
        self.ops.append(op)
        return op

    def dma(self, out, in_, reads=(), writes=(), q="sp"):
        return self.add(q, lambda e: e.dma_start(out=out, in_=in_), reads, writes, dma=True)

    def mm(self, out, lhsT, rhs, start, stop, reads=(), writes=()):
        return self.add("pe", lambda e: e.matmul(out, lhsT, rhs, start=start, stop=stop), reads, writes)

    NDS = 24

    def finish(self):
        nc = self.nc
        cnt = {}
        dma_hist = {}
        for op in self.ops:
            if op.dma:
                hist = dma_hist.setdefault(op.eng, [])
                j = len(hist)
                if j >= self.NDS:
                    op.deps.add(hist[j - self.NDS])
                hist.append(op)
                op.key = (op.eng, True, j % self.NDS)
                op.val = 16 * (j // self.NDS + 1)
                cnt[op.key] = op.val
            elif op.sig:
                op.key = (op.eng, False, 0)
                cnt[op.key] = cnt.get(op.key, 0) + 1
                op.val = cnt[op.key]
        with ExitStack() as es:
            sems = {}
            for k in cnt:
                sems[k] = es.enter_context(nc.semaphore("s_%s_%d_%d" % (k[0], int(k[1]), k[2])))
            block = es.enter_context(nc.Block())
            reg = {"pe": block.tensor, "act": block.scalar, "dve": block.vector,
                   "pool": block.gpsimd, "sp": block.sync}
            for eng in self.ENGS:
                ops_e = [op for op in self.ops if op.eng == eng]
                if not ops_e:
                    continue

                def body(e, ops_e=ops_e, eng=eng):
                    waited = {}
                    for op in ops_e:
                        need = {}
                        for d in op.deps:
                            if d.val > need.get(d.key, 0):
                                need[d.key] = d.val
                        for k, v in need.items():
                            if waited.get(k, 0) < v:
                                e.wait_ge(sems[k], v)
                                waited[k] = v
                        ins = op.fn(e)
                        if op.sig:
                            ins.then_inc(sems[op.key], 16 if op.dma else 1)
                    for k, v in cnt.items():
                        if k[0] == eng and k[1] and waited.get(k, 0) < v:
                            e.wait_ge(sems[k], v)

                reg[eng](body)


def _run(nc, in_maps):
    res = run_bass_kernel_spmd(nc, in_maps, core_ids=list(range(NCORES)))
    return res.results


MOD_COLS = 12288 // NCORES


def build_mod():
    nc = bass.Bass("TRN2", target_bir_lowering=False)
    cT = nc.dram_tensor("cT", [128, 16, 3], F32, kind="ExternalInput").ap()
    w = nc.dram_tensor("w", [2, 2048, MOD_COLS], F32, kind="ExternalInput").ap()
    b = nc.dram_tensor("b", [2, 3, MOD_COLS], F32, kind="ExternalInput").ap()
    m = nc.dram_tensor("m", [2, 3, MOD_COLS], F32, kind="ExternalOutput").ap()
    P = Prog(nc)
    with ExitStack() as es:
        sb = lambda name, shape, dt=F32: es.enter_context(nc.sbuf_tensor(name, shape, dt))
        c_sb = sb("c_sb", [128, 16, 3])
        s_sb = sb("s_sb", [128, 16, 3])
        b_sb = sb("b_sb", [3, 2, MOD_COLS])
        o_sb = sb("o_sb", [3, 2, MOD_COLS])
        wt = [sb("wt%d" % i, [128, 16, 512]) for i in range(2)]
        ps = [es.enter_context(nc.psum_tensor("ps%d" % i, [128, 512], F32)) for i in range(2)]
        Bc, Bs, Bb, Bo = Buf("c"), Buf("s"), Buf("b"), Buf("o")
        Bw = [Buf("w0"), Buf("w1")]
        Bp = [Buf("p0"), Buf("p1")]
        P.dma(c_sb[:], cT, writes=[Bc])
        P.dma(b_sb[:], b.rearrange("l r c -> r l c"), writes=[Bb])
        P.add("act", lambda e: e.activation(out=s_sb[:], in_=c_sb[:], func=AF.Silu), [Bc], [Bs])
        it = 0
        for l in range(2):
            for n in range(MOD_COLS // 512):
                j = it % 2
                P.dma(wt[j][:], w[l, :, n * 512:(n + 1) * 512].rearrange("(k p) c -> p k c", p=128),
                      writes=[Bw[j]])
                for k in range(16):
                    P.mm(ps[j][0:3, :], s_sb[:, k, :], wt[j][:, k, :], k == 0, k == 15,
                         reads=[Bs, Bw[j]], writes=[Bp[j]])
                P.add("dve", lambda e, j=j, l=l, n=n: e.tensor_tensor(
                    out=o_sb[:, l, n * 512:(n + 1) * 512], in0=ps[j][0:3, :],
                    in1=b_sb[:, l, n * 512:(n + 1) * 512], op=ALU.add), [Bp[j], Bb], [Bo])
                it += 1
        P.dma(m.rearrange("l r c -> r l c"), o_sb[:], reads=[Bo])
        P.finish()
    return nc


def run_mod(c, c_ctx, w_mod, b_mod):
    call = np.concatenate([c, c_ctx[None]], 0).astype(np.float32)
    cT = np.ascontiguousarray(call.T.reshape(16, 128, 3).transpose(1, 0, 2))
    nc = build_mod()
    in_maps = []
    for i in range(NCORES):
        sl = slice(i * MOD_COLS, (i + 1) * MOD_COLS)
        in_maps.append({"cT": cT, "w": np.ascontiguousarray(w_mod[:, :, sl]),
                        "b": np.ascontiguousarray(np.broadcast_to(b_mod[:, None, sl], (2, 3, MOD_COLS)))})
    res = _run(nc, in_maps)
    return np.concatenate([r["m"] for r in res], axis=2)


class Ring:
    def __init__(self, items):
        self.items = items
        self.i = 0

    def next(self):
        it = self.items[self.i % len(self.items)]
        self.i += 1
        return it


class Ctx:
    def __init__(self):
        self.nc = bass.Bass("TRN2", target_bir_lowering=False)
        self.P = Prog(self.nc)
        self.es = ExitStack()
        self.stacks = [self.es]
        self.n = 0

    def push(self):
        self.stacks.append(ExitStack())

    def pop(self):
        self.P.barrier()
        self.stacks.pop().close()

    def din(self, name, shape, dt=F32):
        return self.nc.dram_tensor(name, list(shape), dt, kind="ExternalInput").ap()

    def dout(self, name, shape, dt=F32):
        return self.nc.dram_tensor(name, list(shape), dt, kind="ExternalOutput").ap()

    def sb(self, shape, dt=F32, name=None):
        self.n += 1
        return self.stacks[-1].enter_context(self.nc.sbuf_tensor(name or "t%d" % self.n, list(shape), dt))

    def ring(self, n, shape, dt=F32):
        return Ring([(self.sb(shape, dt), Buf()) for _ in range(n)])

    def psum_ring(self, n=8):
        items = []
        for i in range(n):
            t = self.es.enter_context(self.nc.psum_tensor("ps%d" % i, [128, 512], F32))
            items.append((t, Buf("ps%d" % i)))
        return Ring(items)

    def done(self):
        self.P.finish()
        self.es.close()
        return self.nc


EPS = 1e-6
C_AQ, C_BQ, C_CU, C_CV, C_DZ, C_DC = 0, 512, 1024, 1536, 2048, 2560
C_AK, C_AV, C_BK, C_BV, C_DX, C_DB, C_DT = 2816, 3072, 3328, 3840, 4352, 4864, 5120
IN_COLS = 5136
T1 = 1088
TBLK = ((0, 512), (512, 512), (1024, 64))


def build_k1():
    C = Ctx()
    nc, P = C.nc, C.P
    T = T1
    xT = C.din("xT", [128, 16, T])
    modv = C.din("modv", [128, 16, 5])
    w_in = C.din("w_in", [2048, IN_COLS])
    qkg = C.din("qkg", [128, 2])
    ropeC = C.din("ropeC", [128, 1024])
    ropeS = C.din("ropeS", [128, 1024])
    sgug = C.din("sgug", [128, 512])
    rmat = C.din("rmat", [128, 128])
    o_qkA = C.dout("qkA", [6, 128, T], BF16)
    o_bqk = C.dout("bqk", [8, 128, T], BF16)
    o_cu = C.dout("cu", [4, 128, T])
    o_dz = C.dout("dz", [4, 128, T])
    o_dcxb = C.dout("dcxb", [8, 128, T])
    o_av = C.dout("av", [T, 256], BF16)
    o_bv = C.dout("bv", [T, 512], BF16)
    o_vn = C.dout("vn", [T, 512])
    o_ddt = C.dout("ddt", [T, 16])

    x_sb = C.sb([128, 16, T]); Bx = Buf("x")
    h_sb = C.sb([128, 16, T], BF16); Bh = Buf("h")
    mod_sb = C.sb([128, 16, 5]); Bmod = Buf()
    gs_sb = C.sb([128, 16, 2]); Bgs = Buf()
    qkg_sb = C.sb([128, 2]); Bqkg = Buf()
    rc_sb = C.sb([128, 1024]); rs_sb = C.sb([128, 1024]); Brope = Buf()
    sg_sb = C.sb([128, 512]); Bsg = Buf()
    rm_sb = C.sb([128, 128]); Brm = Buf()
    ones_d = C.sb([128, 128]); ones_h = C.sb([128, 128]); Bones = Buf()
    ps = C.psum_ring(8)
    sq_ring = C.ring(2, [128, 4, 512])
    tmp = C.ring(6, [128, 512])
    small = C.ring(4, [128, 2])
    w_ring = C.ring(2, [128, 16, 512], BF16)
    of32 = C.ring(2, [128, T])
    obf = C.ring(2, [128, T], BF16)
    otm = C.ring(2, [128, 512])
    otb = C.ring(2, [128, 512], BF16)
    rs_ring = C.ring(1, [128, 512])

    for q4 in range(4):
        P.dma(x_sb[:, 4 * q4:4 * q4 + 4, :], xT[:, 4 * q4:4 * q4 + 4, :], writes=[Bx])
    P.dma(mod_sb[:], modv, writes=[Bmod])
    P.dma(qkg_sb[:], qkg, writes=[Bqkg])
    P.dma(rc_sb[:], ropeC, writes=[Brope])
    P.dma(rs_sb[:], ropeS, writes=[Brope])
    P.dma(sg_sb[:], sgug, writes=[Bsg])
    P.dma(rm_sb[:], rmat, writes=[Brm])
    P.add("pool", lambda e: e.memset(ones_d[:], 1.0 / 2048.0), [], [Bones])
    P.add("pool", lambda e: e.memset(ones_h[:], 1.0 / 128.0), [], [Bones])
    for j, col in enumerate((1, 3)):
        P.add("dve", lambda e, j=j, col=col: e.scalar_tensor_tensor(
            out=gs_sb[:, :, j], in0=mod_sb[:, :, col], scalar=1.0, in1=mod_sb[:, :, 0],
            op0=ALU.add, op1=ALU.mult), [Bmod], [Bgs])

    for bi, (n0, N) in enumerate(TBLK):
        seg = 0 if bi < 2 else 1
        pt, Bp = ps.next()
        for k4 in range(4):
            sq, Bsq = sq_ring.next()
            P.add("act", lambda e, sq=sq, n0=n0, N=N, k4=k4: e.activation(
                out=sq[:, :, :N], in_=x_sb[:, 4 * k4:4 * k4 + 4, n0:n0 + N], func=AF.Square), [Bx], [Bsq])
            for kk in range(4):
                k = 4 * k4 + kk
                P.mm(pt[:, :N], ones_d[:], sq[:, kk, :N], k == 0, k == 15, reads=[Bones, Bsq], writes=[Bp])
        rstd, Br = rs_ring.next()
        P.add("act", lambda e, rstd=rstd, pt=pt, N=N: e.activation(
            out=rstd[:, :N], in_=pt[:, :N], func=AF.Ln, bias=EPS), [Bp], [Br])
        P.add("act", lambda e, rstd=rstd, N=N: e.activation(
            out=rstd[:, :N], in_=rstd[:, :N], func=AF.Exp, scale=-0.5), [Br], [Br])
        for k in range(16):
            t, Bt = tmp.next()
            P.add("dve", lambda e, t=t, k=k, n0=n0, N=N, seg=seg, rstd=rstd: e.scalar_tensor_tensor(
                out=t[:, :N], in0=x_sb[:, k, n0:n0 + N], scalar=gs_sb[:, k, seg:seg + 1], in1=rstd[:, :N],
                op0=ALU.mult, op1=ALU.mult), [Bx, Bgs, Br], [Bt])
            P.add("act", lambda e, t=t, k=k, n0=n0, N=N, seg=seg: e.activation(
                out=h_sb[:, k, n0:n0 + N], in_=t[:, :N], func=AF.Identity,
                bias=mod_sb[:, k, 2 + 2 * seg:3 + 2 * seg], scale=1.0), [Bt, Bmod], [Bh])

    groups = [
        (C_AQ, 512, "fm", "qn", o_qkA, 0), (C_AK, 256, "fm", "kn", o_qkA, 4),
        (C_BQ, 512, "fm", "cbf", o_bqk, 0), (C_BK, 512, "fm", "cbf", o_bqk, 4),
        (C_CU, 512, "fm", "gelu", o_cu, 0), (C_DZ, 512, "fm", "silu", o_dz, 0),
        (C_DC, 256, "fm", "copy", o_dcxb, 0), (C_DX, 512, "fm", "copy", o_dcxb, 2),
        (C_DB, 256, "fm", "copy", o_dcxb, 6),
        (C_CV, 512, "tm", "vn", o_vn, 0), (C_AV, 256, "tm", "cbf", o_av, 0),
        (C_BV, 512, "tm", "cbf", o_bv, 0), (C_DT, 16, "tm", "copy", o_ddt, 0),
    ]
    wtiles = {}

    def load_w(gi):
        if gi >= len(groups):
            return
        c0, cw = groups[gi][0], groups[gi][1]
        wt, Bw = w_ring.next()
        P.dma(wt[:, :, :cw], w_in[:, c0:c0 + cw].rearrange("(k p) c -> p k c", p=128), writes=[Bw], q="pool")
        wtiles[gi] = (wt, Bw)

    load_w(0)
    for gi, (c0, cw, mode, kind, outT, oi0) in enumerate(groups):
        load_w(gi + 1)
        wt, Bw = wtiles[gi]
        if mode == "fm":
            for j in range(cw // 128):
                isbf = kind in ("qn", "kn", "cbf")
                o, Bo = (obf if isbf else of32).next()
                for bi, (n0, N) in enumerate(TBLK):
                    pt, Bp = ps.next()
                    for k in range(16):
                        P.mm(pt[:, :N], wt[:, k, j * 128:(j + 1) * 128], h_sb[:, k, n0:n0 + N], k == 0, k == 15,
                             reads=[Bw, Bh], writes=[Bp])
                    dst = o[:, n0:n0 + N]
                    if kind in ("cbf", "copy"):
                        P.add("dve", lambda e, dst=dst, pt=pt, N=N: e.tensor_copy(out=dst, in_=pt[:, :N]), [Bp], [Bo])
                    elif kind in ("gelu", "silu"):
                        f = AF.Gelu_apprx_tanh if kind == "gelu" else AF.Silu
                        P.add("act", lambda e, dst=dst, pt=pt, N=N, f=f: e.activation(out=dst, in_=pt[:, :N], func=f),
                              [Bp], [Bo])
                    else:
                        gcol = 0 if kind == "qn" else 1
                        qs, Bqs = tmp.next()
                        sq, Bsq = tmp.next()
                        P.add("act", lambda e, qs=qs, pt=pt, N=N: e.activation(out=qs[:, :N], in_=pt[:, :N], func=AF.Identity),
                              [Bp], [Bqs])
                        P.add("act", lambda e, sq=sq, pt=pt, N=N: e.activation(out=sq[:, :N], in_=pt[:, :N], func=AF.Square),
                              [Bp], [Bsq])
                        p2, Bp2 = ps.next()
                        P.mm(p2[:, :N], ones_h[:], sq[:, :N], True, True, reads=[Bones, Bsq], writes=[Bp2])
                        rstd, Br = tmp.next()
                        P.add("act", lambda e, rstd=rstd, p2=p2, N=N: e.activation(
                            out=rstd[:, :N], in_=p2[:, :N], func=AF.Ln, bias=EPS), [Bp2], [Br])
                        P.add("act", lambda e, rstd=rstd, N=N: e.activation(
                            out=rstd[:, :N], in_=rstd[:, :N], func=AF.Exp, scale=-0.5), [Br], [Br])
                        qn, Bqn = tmp.next()
                        P.add("dve", lambda e, qn=qn, qs=qs, rstd=rstd, N=N, gcol=gcol: e.scalar_tensor_tensor(
                            out=qn[:, :N], in0=qs[:, :N], scalar=qkg_sb[:, gcol:gcol + 1], in1=rstd[:, :N],
                            op0=ALU.mult, op1=ALU.mult), [Bqs, Br, Bqkg], [Bqn])
                        if bi < 2:
                            p3, Bp3 = ps.next()
                            P.mm(p3[:, :N], rm_sb[:], qn[:, :N], True, True, reads=[Brm, Bqn], writes=[Bp3])
                            t1, Bt1 = tmp.next()
                            t2, Bt2 = tmp.next()
                            P.add("dve", lambda e, t1=t1, qn=qn, n0=n0, N=N: e.tensor_tensor(
                                out=t1[:, :N], in0=qn[:, :N], in1=rc_sb[:, n0:n0 + N], op=ALU.mult), [Bqn, Brope], [Bt1])
                            P.add("dve", lambda e, t2=t2, p3=p3, n0=n0, N=N: e.tensor_tensor(
                                out=t2[:, :N], in0=p3[:, :N], in1=rs_sb[:, n0:n0 + N], op=ALU.mult), [Bp3, Brope], [Bt2])
                            P.add("pool", lambda e, dst=dst, t1=t1, t2=t2, N=N: e.tensor_tensor(
                                out=dst, in0=t1[:, :N], in1=t2[:, :N], op=ALU.add), [Bt1, Bt2], [Bo])
                        else:
                            P.add("dve", lambda e, dst=dst, qn=qn, N=N: e.tensor_copy(out=dst, in_=qn[:, :N]), [Bqn], [Bo])
                P.dma(outT[oi0 + j], o[:], reads=[Bo])
        else:
            for ti in range(9):
                t0 = ti * 128
                M = 128 if ti < 8 else 64
                pt, Bp = ps.next()
                for k in range(16):
                    P.mm(pt[:M, :cw], h_sb[:, k, t0:t0 + M], wt[:, k, :cw], k == 0, k == 15,
                         reads=[Bw, Bh], writes=[Bp])
                if kind == "vn":
                    g, Bg = tmp.next()
                    sq, Bsq = tmp.next()
                    ss, Bss = small.next()
                    o, Bo = otm.next()
                    P.add("act", lambda e, g=g, pt=pt, M=M: e.activation(out=g[:M, :], in_=pt[:M, :], func=AF.Gelu_apprx_tanh),
                          [Bp], [Bg])
                    P.add("act", lambda e, g=g, sq=sq, M=M: e.activation(out=sq[:M, :], in_=g[:M, :], func=AF.Square),
                          [Bg], [Bsq])
                    P.add("dve", lambda e, ss=ss, sq=sq, M=M: e.tensor_reduce(out=ss[:M, 0:1], in_=sq[:M, :], axis=AX.X, op=ALU.add),
                          [Bsq], [Bss])
                    P.add("act", lambda e, ss=ss, M=M: e.activation(out=ss[:M, 1:2], in_=ss[:M, 0:1], func=AF.Ln,
                                                                     bias=EPS, scale=1.0 / 512.0), [Bss], [Bss])
                    P.add("act", lambda e, ss=ss, M=M: e.activation(out=ss[:M, 0:1], in_=ss[:M, 1:2], func=AF.Exp, scale=-0.5),
                          [Bss], [Bss])
                    P.add("dve", lambda e, o=o, g=g, ss=ss, M=M: e.scalar_tensor_tensor(
                        out=o[:M, :], in0=g[:M, :], scalar=ss[:M, 0:1], in1=sg_sb[:M, :], op0=ALU.mult, op1=ALU.mult),
                          [Bg, Bss, Bsg], [Bo])
                else:
                    o, Bo = (otb if kind == "cbf" else otm).next()
                    P.add("dve", lambda e, o=o, pt=pt, M=M, cw=cw: e.tensor_copy(out=o[:M, :cw], in_=pt[:M, :cw]), [Bp], [Bo])
                P.dma(outT[t0:t0 + M, :], o[:M, :cw], reads=[Bo])
    return C.done()


def _fm(v):
    return np.ascontiguousarray(v.reshape(16, 128).T)


def _tok_to_fm(t):
    T = t.shape[0]
    return np.ascontiguousarray(t.T.reshape(16, 128, T).transpose(1, 0, 2))


def _fm_to_tok(f):
    T = f.shape[2]
    return np.ascontiguousarray(f.transpose(1, 0, 2).reshape(2048, T).T)


def _rope_tables():
    t = np.arange(4096)
    row = (t // 64).astype(np.float32)
    col = (t % 64).astype(np.float32)
    inv = np.power(np.float32(10000.0), -np.arange(32, dtype=np.float32) / np.float32(32)).astype(np.float32)
    ang = np.concatenate([row[:, None] * inv, col[:, None] * inv], axis=-1).astype(np.float32)
    cs = np.repeat(np.cos(ang), 2, axis=1).T.astype(np.float32)
    sn = np.repeat(np.sin(ang), 2, axis=1).T.astype(np.float32)
    return np.ascontiguousarray(cs), np.ascontiguousarray(sn)


def _rmat():
    r = np.zeros((128, 128), np.float32)
    for i in range(64):
        r[2 * i + 1, 2 * i] = -1.0
        r[2 * i, 2 * i + 1] = 1.0
    return r


_NC_CACHE = {}


def _get_nc(name, builder):
    if name not in _NC_CACHE:
        _NC_CACHE[name] = builder()
    return _NC_CACHE[name]


def run_k1(l, xfm, m, p):
    cs, sn = _rope_tables()
    rm = _rmat()
    in_maps = []
    w_in = np.ascontiguousarray(p["w_in"][l])
    qkg = np.ascontiguousarray(np.stack([p["q_norm"][l], p["k_norm"][l]], axis=1))
    sgug = np.ascontiguousarray(np.broadcast_to(p["sgu_norm"][l][None, :], (128, 512)))
    for i in range(NCORES):
        b, q = i // 4, i % 4
        modv = np.stack([_fm(p["norm_mix"][l]), _fm(m[l, b, 2048:4096]), _fm(m[l, b, 0:2048]),
                         _fm(m[l, 2, 2048:4096]), _fm(m[l, 2, 0:2048])], axis=2)
        in_maps.append({"xT": xfm[i], "modv": np.ascontiguousarray(modv), "w_in": w_in, "qkg": qkg,
                        "ropeC": np.ascontiguousarray(cs[:, q * 1024:(q + 1) * 1024]),
                        "ropeS": np.ascontiguousarray(sn[:, q * 1024:(q + 1) * 1024]),
                        "sgug": sgug, "rmat": rm})
    return _run(build_k1(), in_maps)


TK = 4352
NT = 34
SCALE = 128 ** -0.5


def build_k2():
    C = Ctx()
    nc, P = C.nc, C.P
    d_qa = C.din("qa", [128, TK], BF16); d_ka = C.din("ka", [128, TK], BF16); d_va = C.din("va", [TK, 128], BF16)
    d_qb = C.din("qb", [128, TK], BF16); d_kb = C.din("kb", [128, TK], BF16); d_vb = C.din("vb", [TK, 128], BF16)
    d_nab = C.din("nab", [5, 128, 7, 128])
    d_cu = C.din("cu", [128, TK]); d_vn = C.din("vn", [TK, 128])
    d_wsT = C.din("wsT", [128, 128]); d_bsb = C.din("bsb", [128, 512])
    d_dx = C.din("dx", [128, TK]); d_db = C.din("db", [128, TK]); d_dc = C.din("dc", [128, TK]); d_dz = C.din("dz", [128, TK])
    d_ddt = C.din("ddt", [TK, 4])
    d_cw = C.din("cw", [128, 3, 5]); d_cb = C.din("cb", [128, 3])
    d_dtb = C.din("dtb", [128, NT * 4]); d_alog = C.din("alog", [128, NT * 4]); d_dsk = C.din("dsk", [128, 2])
    d_tri = C.din("tri", [2, 128, 128]); d_ident = C.din("ident", [128, 128])
    o_a = C.dout("oa", [128, TK]); o_b = C.dout("ob", [128, TK]); o_c = C.dout("oc", [128, TK]); o_d = C.dout("od", [128, TK])

    banks = C.psum_ring(8).items
    C.push()
    psS = Ring(banks[0:3]); psO = Ring(banks[3:5]); psD = Ring(banks[5:7]); psX = Ring(banks[7:8])
    ones_bf = C.sb([128, 128], BF16); Bones = Buf()
    P.add("pool", lambda e: e.memset(ones_bf[:], 1.0), [], [Bones])
    e_ring = C.ring(3, [128, 896], BF16)
    tmp = C.ring(4, [128, 896])
    stage = C.ring(3, [128, 512])

    def load_qkv(dq, dk, dv):
        q = C.sb([128, TK], BF16); k = C.sb([128, TK], BF16); v = C.sb([128, NT, 128], BF16)
        Bq, Bk, Bv = Buf(), Buf(), Buf()
        P.dma(q[:], dq, writes=[Bq]); P.dma(k[:], dk, writes=[Bk])
        P.dma(v[:], dv.rearrange("(t p) c -> p t c", p=128), writes=[Bv])
        return (q, Bq), (k, Bk), (v, Bv)

    def finish_block(pO, BpO, pD, BpD, nq, dst):
        rd, Brd = tmp.next()
        P.add("dve", lambda e: e.reciprocal(out=rd[:, :nq], in_=pD[:, :nq]), [BpD], [Brd])
        st, Bst = stage.next()
        P.add("dve", lambda e: e.tensor_tensor(out=st[:, :nq], in0=pO[:, :nq], in1=rd[:, :nq], op=ALU.mult),
              [BpO, Brd], [Bst])
        P.dma(dst, st[:, :nq], reads=[Bst])

    def attn_dense(Q, K, V, q0, nq, ktiles, dst):
        (q, Bq), (k, Bk), (v, Bv) = Q, K, V
        pO, BpO = psO.next(); pD, BpD = psD.next()
        n = len(ktiles)

        def S(kt):
            pS, BpS = psS.next()
            P.mm(pS[:, :nq], k[:, kt * 128:(kt + 1) * 128], q[:, q0:q0 + nq], True, True, reads=[Bk, Bq], writes=[BpS])
            return pS, BpS
        cur = S(ktiles[0])
        for ii, kt in enumerate(ktiles):
            pS, BpS = cur
            if ii + 1 < n:
                cur = S(ktiles[ii + 1])
            E, BE = e_ring.next()
            P.add("act", lambda e, E=E, pS=pS: e.activation(out=E[:, :nq], in_=pS[:, :nq], func=AF.Exp, scale=SCALE),
                  [BpS], [BE])
            P.mm(pO[:, :nq], v[:, kt, :], E[:, :nq], ii == 0, ii == n - 1, reads=[Bv, BE], writes=[BpO])
            P.mm(pD[:, :nq], ones_bf[:], E[:, :nq], ii == 0, ii == n - 1, reads=[Bones, BE], writes=[BpD])
        finish_block(pO, BpO, pD, BpD, nq, dst)

    QA, KA, VA = load_qkv(d_qa, d_ka, d_va)
    attn_dense(QA, KA, VA, 0, 256, [0, 1], o_a[:, 0:256])
    for qb_ in range(8):
        q0 = 256 + qb_ * 512
        attn_dense(QA, KA, VA, q0, 512, list(range(NT)), o_a[:, q0:q0 + 512])

    QB, KB, VB = load_qkv(d_qb, d_kb, d_vb)
    nab = C.sb([128, 5, 7 * 128]); Bnab = Buf()
    for t5 in range(5):
        P.dma(nab[:, t5, :], d_nab[t5].rearrange("p t q -> p (t q)"), writes=[Bnab])
    attn_dense(QB, KB, VB, 0, 256, [0, 1], o_b[:, 0:256])
    (qb, Bqb), (kb, Bkb), (vb, Bvb) = QB, KB, VB
    for m in range(32):
        rs = min(max(2 * m - 4, 0), 54)
        tab = {0: 0, 1: 1, 30: 3, 31: 4}.get(m, 2)
        q0 = 256 + m * 128
        ktl = [0, 1] + [2 + rs // 2 + t for t in range(5)]
        pA, BpA = psS.next(); pB, BpB = psS.next()
        for ii, kt in enumerate(ktl):
            pt, Bpt, off = (pA, BpA, ii * 128) if ii < 4 else (pB, BpB, (ii - 4) * 128)
            P.mm(pt[:, off:off + 128], kb[:, kt * 128:(kt + 1) * 128], qb[:, q0:q0 + 128], True, True,
                 reads=[Bkb, Bqb], writes=[Bpt])
        sc, Bsc = tmp.next()
        P.add("dve", lambda e, sc=sc, pA=pA, tab=tab: e.scalar_tensor_tensor(
            out=sc[:, 0:512], in0=pA[:, 0:512], scalar=SCALE, in1=nab[:, tab, 0:512], op0=ALU.mult, op1=ALU.add),
              [BpA, Bnab], [Bsc])
        P.add("dve", lambda e, sc=sc, pB=pB, tab=tab: e.scalar_tensor_tensor(
            out=sc[:, 512:896], in0=pB[:, 0:384], scalar=SCALE, in1=nab[:, tab, 512:896], op0=ALU.mult, op1=ALU.add),
              [BpB, Bnab], [Bsc])
        E, BE = e_ring.next()
        P.add("act", lambda e, E=E, sc=sc: e.activation(out=E[:, :], in_=sc[:, :], func=AF.Exp), [Bsc], [BE])
        pO, BpO = psO.next(); pD, BpD = psD.next()
        for ii, kt in enumerate(ktl):
            P.mm(pO[:, :128], vb[:, kt, :], E[:, ii * 128:(ii + 1) * 128], ii == 0, ii == 6, reads=[Bvb, BE], writes=[BpO])
            P.mm(pD[:, :128], ones_bf[:], E[:, ii * 128:(ii + 1) * 128], ii == 0, ii == 6, reads=[Bones, BE], writes=[BpD])
        finish_block(pO, BpO, pD, BpD, 128, o_b[:, q0:q0 + 128])

    cu = C.sb([128, TK]); Bcu = Buf()
    vn = C.sb([128, NT, 128]); Bvn = Buf()
    wsT = C.sb([128, 128]); bsb = C.sb([128, 512]); Bws = Buf()
    P.dma(cu[:], d_cu, writes=[Bcu])
    P.dma(vn[:], d_vn.rearrange("(t p) c -> p t c", p=128), writes=[Bvn])
    P.dma(wsT[:], d_wsT, writes=[Bws]); P.dma(bsb[:], d_bsb, writes=[Bws])
    for g0 in range(0, NT, 4):
        ng = min(4, NT - g0)
        pt, Bpt = psX.next()
        for t in range(ng):
            P.mm(pt[:, t * 128:(t + 1) * 128], vn[:, g0 + t, :], wsT[:], True, True, reads=[Bvn, Bws], writes=[Bpt])
        w = ng * 128
        t1, Bt1 = tmp.next()
        P.add("dve", lambda e, t1=t1, pt=pt, w=w: e.tensor_tensor(out=t1[:, :w], in0=pt[:, :w], in1=bsb[:, :w], op=ALU.add),
              [Bpt, Bws], [Bt1])
        st, Bst = stage.next()
        P.add("pool", lambda e, st=st, t1=t1, w=w, g0=g0: e.tensor_tensor(
            out=st[:, :w], in0=t1[:, :w], in1=cu[:, g0 * 128:g0 * 128 + w], op=ALU.mult), [Bt1, Bcu], [Bst])
        P.dma(o_c[:, g0 * 128:g0 * 128 + w], st[:, :w], reads=[Bst])
    C.pop()

    C.push()
    psR = Ring(banks[0:8])
    raw = [C.sb([128, TK]) for _ in range(3)]; Braw = [Buf() for _ in range(3)]
    cv = [C.sb([128, TK]) for _ in range(3)]; Bcv = [Buf() for _ in range(3)]
    for i3, dd in enumerate((d_dx, d_db, d_dc)):
        P.dma(raw[i3][:], dd, writes=[Braw[i3]])
    dz = C.sb([128, TK]); Bdz = Buf()
    P.dma(dz[:], d_dz, writes=[Bdz])
    cw = C.sb([128, 3, 5]); cb = C.sb([128, 3]); Bcw = Buf()
    P.dma(cw[:], d_cw, writes=[Bcw]); P.dma(cb[:], d_cb, writes=[Bcw])
    ddt = C.sb([128, NT, 4]); Bddt = Buf()
    P.dma(ddt[:], d_ddt.rearrange("(t p) c -> p t c", p=128), writes=[Bddt])
    dtb = C.sb([128, NT * 4]); alog = C.sb([128, NT * 4]); dsk = C.sb([128, 2]); Bsm = Buf()
    P.dma(dtb[:], d_dtb, writes=[Bsm]); P.dma(alog[:], d_alog, writes=[Bsm]); P.dma(dsk[:], d_dsk, writes=[Bsm])
    tri = C.sb([128, 2, 128]); ident = C.sb([128, 128]); Btri = Buf()
    P.dma(tri[:], d_tri.rearrange("d p i -> p d i"), writes=[Btri]); P.dma(ident[:], d_ident, writes=[Btri])
    ones_f = C.sb([128, 128]); Bof = Buf()
    P.add("pool", lambda e: e.memset(ones_f[:], 1.0), [], [Bof])

    for i3 in range(3):
        for (s0, L) in ((0, 256), (256, 4096)):
            P.add("dve", lambda e, i3=i3, s0=s0, L=L: e.tensor_scalar(
                out=cv[i3][:, s0:s0 + L], in0=raw[i3][:, s0:s0 + L], scalar1=cw[:, i3, 2:3], scalar2=None, op0=ALU.mult),
                  [Braw[i3], Bcw], [Bcv[i3]])
            for kk in (0, 1, 3, 4):
                d = kk - 2
                lo, hi = max(0, -d), min(L, L - d)
                P.add("dve", lambda e, i3=i3, s0=s0, lo=lo, hi=hi, d=d, kk=kk: e.scalar_tensor_tensor(
                    out=cv[i3][:, s0 + lo:s0 + hi], in0=raw[i3][:, s0 + lo + d:s0 + hi + d], scalar=cw[:, i3, kk:kk + 1],
                    in1=cv[i3][:, s0 + lo:s0 + hi], op0=ALU.mult, op1=ALU.add), [Braw[i3], Bcw, Bcv[i3]], [Bcv[i3]])
        P.add("act", lambda e, i3=i3: e.activation(out=cv[i3][:, :], in_=cv[i3][:, :], func=AF.Silu, bias=cb[:, i3:i3 + 1]),
              [Bcv[i3], Bcw], [Bcv[i3]])
    xc, Bc_, Cc = cv
    Bxc, BBc, BCc = Bcv
    xtm = raw[0][:].rearrange("p (t c) -> p t c", c=128)
    btm = raw[1][:].rearrange("p (t c) -> p t c", c=128)
    Bxtm, Bbtm = Braw[0], Braw[1]
    for (src, Bsrc, dst, Bdst) in ((xc, Bxc, xtm, Bxtm), (Bc_, BBc, btm, Bbtm)):
        for g0 in range(0, NT, 4):
            ng = min(4, NT - g0)
            pt, Bpt = psR.next()
            for t in range(ng):
                P.add("pe", lambda e, pt=pt, t=t, src=src, g0=g0: e.transpose(
                    pt[:, t * 128:(t + 1) * 128], src[:, (g0 + t) * 128:(g0 + t + 1) * 128], ident[:]),
                      [Bsrc, Btri], [Bpt])
            P.add("dve", lambda e, pt=pt, dst=dst, g0=g0, ng=ng: e.tensor_copy(
                out=dst[:, g0:g0 + ng, :], in_=pt[:, :ng * 128].rearrange("p (t c) -> p t c", c=128)), [Bpt], [Bdst])
    sm = [C.sb([128, NT * 4]) for _ in range(5)]; Bs_ = [Buf() for _ in range(5)]
    ddf = ddt[:].rearrange("p t c -> p (t c)")
    P.add("dve", lambda e: e.tensor_tensor(out=sm[0][:], in0=ddf, in1=dtb[:], op=ALU.add), [Bddt, Bsm], [Bs_[0]])
    P.add("act", lambda e: e.activation(out=sm[1][:], in_=sm[0][:], func=AF.Abs), [Bs_[0]], [Bs_[1]])
    P.add("act", lambda e: e.activation(out=sm[1][:], in_=sm[1][:], func=AF.Exp, scale=-1.0), [Bs_[1]], [Bs_[1]])
    P.add("act", lambda e: e.activation(out=sm[1][:], in_=sm[1][:], func=AF.Ln, bias=1.0), [Bs_[1]], [Bs_[1]])
    P.add("dve", lambda e: e.tensor_scalar(out=sm[2][:], in0=sm[0][:], scalar1=0.0, scalar2=None, op0=ALU.max), [Bs_[0]], [Bs_[2]])
    dt_t, Bdt = sm[3], Bs_[3]
    P.add("dve", lambda e: e.tensor_tensor(out=dt_t[:], in0=sm[2][:], in1=sm[1][:], op=ALU.add), [Bs_[1], Bs_[2]], [Bdt])
    P.add("act", lambda e: e.activation(out=sm[0][:], in_=alog[:], func=AF.Exp), [Bsm, Bs_[0]], [Bs_[0]])
    a_t, Ba = sm[4], Bs_[4]
    P.add("dve", lambda e: e.scalar_tensor_tensor(out=a_t[:], in0=sm[0][:], scalar=-1.0, in1=dt_t[:], op0=ALU.mult, op1=ALU.mult),
          [Bs_[0], Bdt], [Ba])
    dt3 = dt_t[:].rearrange("p (t c) -> p t c", c=4)
    a3 = a_t[:].rearrange("p (t c) -> p t c", c=4)

    Y = C.sb([128, TK]); BY = Buf()
    hp = [C.sb([128, 128]) for _ in range(2)]; Bhp = [Buf(), Buf()]
    t128 = C.ring(12, [128, 128])
    t64 = C.ring(6, [128, 128])
    tsm = C.ring(8, [128, 4])
    for d in range(2):
        order = [0, 1] + list(range(2, NT)) if d == 0 else [1, 0] + list(range(NT - 1, 1, -1))
        for r in range(2):
            P.add("pool", lambda e, r=r: e.memset(hp[r][:], 0.0), [], [Bhp[r]])
        last = 127 if d == 0 else 0
        for c in order:
            cs = slice(c * 128, (c + 1) * 128)
            acol = a3[:, c, 2 * d:2 * d + 2]
            pc, Bpc = psR.next()
            P.mm(pc[:, 0:2], tri[:, d, :], acol, True, True, reads=[Btri, Ba], writes=[Bpc])
            cc, Bcc = tsm.next()
            P.add("dve", lambda e, cc=cc, pc=pc: e.tensor_copy(out=cc[:, 0:2], in_=pc[:, 0:2]), [Bpc], [Bcc])
            pr, Bpr = psR.next()
            for r in range(2):
                abc, Babc = t128.next()
                P.add("dve", lambda e, abc=abc, c=c, r=r, d=d: e.tensor_scalar(
                    out=abc[:], in0=ones_f[:], scalar1=a3[:, c, 2 * d + r:2 * d + r + 1], scalar2=None, op0=ALU.mult),
                      [Bof, Ba], [Babc])
                P.mm(pr[:, r * 128:(r + 1) * 128], abc[:], tri[:, d, :], True, True, reads=[Babc, Btri], writes=[Bpr])
            pss, Bpss = psR.next()
            P.mm(pss[:, 0:128], Bc_[:, cs], Cc[:, cs], True, True, reads=[BBc, BCc], writes=[Bpss])
            xw, Bxw = t64.next()
            py, Bpy = psR.next()
            tot, Btot = tsm.next()
            P.add("dve", lambda e, tot=tot, pr=pr, last=last: e.tensor_copy(
                out=tot[:, 0:2], in_=pr[:, 0:256].rearrange("p (r i) -> p r i", i=128)[:, :, last]), [Bpr], [Btot])
            dec, Bdec = tsm.next()
            for r in range(2):
                P.add("act", lambda e, dec=dec, cc=cc, tot=tot, r=r: e.activation(
                    out=dec[:, r:r + 1], in_=cc[:, r:r + 1], func=AF.Exp, scale=-1.0, bias=tot[:, r:r + 1]),
                      [Bcc, Btot], [Bdec])
            P.add("act", lambda e, dec=dec, tot=tot: e.activation(out=dec[:, 2:4], in_=tot[:, 0:2], func=AF.Exp),
                  [Btot, Bdec], [Bdec])
            first = True
            for r in range(2):
                hs = slice(r * 64, (r + 1) * 64)
                arg, Barg = t128.next()
                P.add("dve", lambda e, arg=arg, pr=pr, cc=cc, r=r: e.tensor_scalar(
                    out=arg[:], in0=pr[:, r * 128:(r + 1) * 128], scalar1=cc[:, r:r + 1], scalar2=0.0,
                    op0=ALU.subtract, op1=ALU.min), [Bpr, Bcc], [Barg])
                P.add("act", lambda e, arg=arg: e.activation(out=arg[:], in_=arg[:], func=AF.Exp), [Barg], [Barg])
                P.add("pool", lambda e, arg=arg, d=d: e.tensor_tensor(out=arg[:], in0=arg[:], in1=tri[:, d, :], op=ALU.mult),
                      [Barg, Btri], [Barg])
                mt, Bmt = t128.next()
                P.add("dve", lambda e, mt=mt, pss=pss, arg=arg: e.tensor_tensor(
                    out=mt[:], in0=pss[:, 0:128], in1=arg[:], op=ALU.mult), [Bpss, Barg], [Bmt])
                xd, Bxd = t64.next()
                P.add("pool", lambda e, xd=xd: e.memset(xd[:], 0.0), [], [Bxd])
                P.add("dve", lambda e, xd=xd, c=c, r=r, hs=hs, d=d: e.tensor_scalar(
                    out=xd[:, hs], in0=xtm[:, c, hs], scalar1=dt3[:, c, 2 * d + r:2 * d + r + 1], scalar2=None, op0=ALU.mult),
                      [Bxtm, Bdt, Bxd], [Bxd])
                P.add("dve", lambda e, xw=xw, xd=xd, dec=dec, r=r, hs=hs: e.tensor_scalar(
                    out=xw[:, hs], in0=xd[:, hs], scalar1=dec[:, r:r + 1], scalar2=None, op0=ALU.mult), [Bxd, Bdec], [Bxw])
                ecr, Becr = t128.next()
                P.add("act", lambda e, ecr=ecr, pr=pr, r=r: e.activation(
                    out=ecr[:], in_=pr[:, r * 128:(r + 1) * 128], func=AF.Exp), [Bpr], [Becr])
                P.add("pool", lambda e, ecr=ecr, cs=cs: e.tensor_tensor(out=ecr[:], in0=ecr[:], in1=Cc[:, cs], op=ALU.mult),
                      [Becr, BCc], [Becr])
                P.mm(py[:, 0:128], xd[:], mt[:], first, False, reads=[Bxd, Bmt], writes=[Bpy])
                first = False
                P.mm(py[:, 0:128], hp[r][:], ecr[:], False, r == 1, reads=[Bhp[r], Becr], writes=[Bpy])
            if d == 0:
                P.add("dve", lambda e, py=py, cs=cs: e.tensor_copy(out=Y[:, cs], in_=py[:, 0:128]), [Bpy], [BY])
            else:
                P.add("dve", lambda e, py=py, cs=cs: e.tensor_tensor(out=Y[:, cs], in0=py[:, 0:128], in1=Y[:, cs], op=ALU.add),
                      [Bpy, BY], [BY])
            pst, Bpst = psR.next()
            P.mm(pst[:, 0:128], btm[:, c, :], xw[:], True, True, reads=[Bbtm, Bxw], writes=[Bpst])
            for r in range(2):
                hs = slice(r * 64, (r + 1) * 64)
                P.add("dve", lambda e, r=r, hs=hs, dec=dec, pst=pst: e.scalar_tensor_tensor(
                    out=hp[r][:, hs], in0=hp[r][:, hs], scalar=dec[:, 2 + r:3 + r], in1=pst[:, hs], op0=ALU.mult, op1=ALU.add),
                      [Bhp[r], Bdec, Bpst], [Bhp[r]])
    dsum = C.sb([128, 1]); Bds = Buf()
    P.add("dve", lambda e: e.tensor_tensor(out=dsum[:], in0=dsk[:, 0:1], in1=dsk[:, 1:2], op=ALU.add), [Bsm], [Bds])
    for c0 in range(0, TK, 1088):
        sl = slice(c0, c0 + 1088)
        P.add("dve", lambda e, sl=sl: e.scalar_tensor_tensor(out=Y[:, sl], in0=xc[:, sl], scalar=dsum[:, 0:1], in1=Y[:, sl],
                                                              op0=ALU.mult, op1=ALU.add), [Bxc, Bds, BY], [BY])
        P.add("pool", lambda e, sl=sl: e.tensor_tensor(out=Y[:, sl], in0=Y[:, sl], in1=dz[:, sl], op=ALU.mult), [BY, Bdz], [BY])
        P.dma(o_d[:, sl], Y[:, sl], reads=[BY])
    C.pop()
    return C.done()


def _na_tables(rpb_l):
    out = np.zeros((4, 5, 7, 128, 128), np.float32)
    specs = [(0, 0), (2, 0), (8, 4), (60, 54), (62, 54)]
    kk = np.arange(640)
    qq = np.arange(128)
    for ti, (r0, rs) in enumerate(specs):
        krow = rs + kk // 64
        kcol = kk % 64
        qrow = r0 + qq // 64
        qcol = qq % 64
        kr0 = np.clip(qrow - 4, 0, 56)
        wc0 = np.clip(qcol - 8, 0, 48)
        ok = ((krow[:, None] >= kr0[None, :]) & (krow[:, None] < kr0[None, :] + 8) &
              (kcol[:, None] >= wc0[None, :]) & (kcol[:, None] < wc0[None, :] + 16))
        drow = np.clip(krow[:, None] - qrow[None, :] + 7, 0, 14)
        dcol = np.clip(kcol[:, None] - qcol[None, :], -15, 15) + 15
        for h in range(4):
            g = rpb_l[h][drow, dcol]
            g = np.where(ok, g, np.float32(-30000.0))
            out[h, ti, 2:7] = g.reshape(5, 128, 128)
    return np.ascontiguousarray(out.transpose(0, 1, 3, 2, 4))


def _tri_consts():
    j = np.arange(128)[:, None]
    i = np.arange(128)[None, :]
    return np.ascontiguousarray(np.stack([(j <= i), (j >= i)]).astype(np.float32))


def _gather_tok(res, key, b, fm):
    parts = [np.asarray(res[4 * b + q][key]) for q in range(4)]
    if fm:
        return np.concatenate([p_[:, :, 1024:] for p_ in parts] + [p_[:, :, :1024] for p_ in parts], axis=2)
    return np.concatenate([p_[1024:] for p_ in parts] + [p_[:1024] for p_ in parts], axis=0)


def run_k2(l, r1, p):
    nab = _na_tables(p["rpb"][l])
    tri = _tri_consts()
    ident = np.eye(128, dtype=np.float32)
    in_maps = []
    g_ = {}
    for b in range(2):
        for key, fm in (("qkA", 1), ("bqk", 1), ("cu", 1), ("dz", 1), ("dcxb", 1), ("av", 0), ("bv", 0), ("vn", 0), ("ddt", 0)):
            g_[(b, key)] = _gather_tok(r1, key, b, fm)
    for i in range(NCORES):
        b, hh = i // 4, i % 4
        g = hh // 2
        ch = slice(hh * 128, (hh + 1) * 128)
        dcxb = g_[(b, "dcxb")]
        cwf = p["conv_w"][l]
        cbf = p["conv_b"][l]
        xs = slice(hh * 128, (hh + 1) * 128)
        bs = slice(512 + g * 128, 512 + (g + 1) * 128)
        cs_ = slice(768 + g * 128, 768 + (g + 1) * 128)
        cw = np.stack([cwf[:, xs].T, cwf[:, bs].T, cwf[:, cs_].T], axis=1)
        cb = np.stack([cbf[xs], cbf[bs], cbf[cs_]], axis=1)
        hsel = [2 * hh, 2 * hh + 1, 8 + 2 * hh, 8 + 2 * hh + 1]
        dtb4 = p["dt_bias"][l].reshape(16)[hsel]
        al4 = p["a_log"][l].reshape(16)[hsel]
        heads_p = np.repeat([2 * hh, 2 * hh + 1], 64)
        dsk = np.stack([p["d_skip"][l][0][heads_p], p["d_skip"][l][1][heads_p]], axis=1)
        in_maps.append({
            "qa": np.ascontiguousarray(g_[(b, "qkA")][hh]), "ka": np.ascontiguousarray(g_[(b, "qkA")][4 + hh // 2]),
            "va": np.ascontiguousarray(g_[(b, "av")][:, (hh // 2) * 128:(hh // 2 + 1) * 128]),
            "qb": np.ascontiguousarray(g_[(b, "bqk")][hh]), "kb": np.ascontiguousarray(g_[(b, "bqk")][4 + hh]),
            "vb": np.ascontiguousarray(g_[(b, "bv")][:, ch]),
            "nab": nab[hh],
            "cu": np.ascontiguousarray(g_[(b, "cu")][hh]), "vn": np.ascontiguousarray(g_[(b, "vn")][:, ch]),
            "wsT": np.ascontiguousarray(p["sgu_w"][l][hh].T),
            "bsb": np.ascontiguousarray(np.broadcast_to(np.tile(p["sgu_b"][l][hh], 4)[None, :], (128, 512))),
            "dx": np.ascontiguousarray(dcxb[2 + hh]), "db": np.ascontiguousarray(dcxb[6 + g]),
            "dc": np.ascontiguousarray(dcxb[g]), "dz": np.ascontiguousarray(g_[(b, "dz")][hh]),
            "ddt": np.ascontiguousarray(g_[(b, "ddt")][:, hsel]),
            "cw": np.ascontiguousarray(cw.astype(np.float32)), "cb": np.ascontiguousarray(cb.astype(np.float32)),
            "dtb": np.ascontiguousarray(np.broadcast_to(np.tile(dtb4, NT)[None, :], (128, NT * 4))),
            "alog": np.ascontiguousarray(np.broadcast_to(np.tile(al4, NT)[None, :], (128, NT * 4))),
            "dsk": np.ascontiguousarray(dsk.astype(np.float32)),
            "tri": tri, "ident": ident,
        })
    return _run(build_k2(), in_maps)


D_FF = 5632
D_FFE = 7168


def build_k3(moe):
    C = Ctx()
    nc, P = C.nc, C.P
    T = 1024 if moe else T1
    blks = TBLK[:2] if moe else TBLK
    nsp = 4 if moe else 2
    xT = C.din("xT", [128, 16, T])
    mixT = C.din("mixT", [128, 16, T])
    modv = C.din("modv", [128, 16, 11])
    w_out = C.din("w_out", [2048, 2048])
    if moe:
        router = C.din("router", [128, 16, 8]); d_ident = C.din("ident", [128, 128])
        d_sel = C.din("sel", [8, 8, 128])
        nff = D_FFE // 128
        xo = C.dout("xo", [128, 16, 1024])
        go = C.dout("gates", [1024, 8])
        h2o = C.dout("h2o", [128, 16, 1024], BF16)
    else:
        w13 = C.din("w13", [2048, 2 * D_FF]); w2 = C.din("w2", [D_FF, 2048])
        nff = D_FF // 128
        xo = C.dout("xo", [128, 16, T])
    nh = nff // nsp

    banks = C.psum_ring(8).items
    ps = Ring(banks)
    x_sb = C.sb([128, 16, T]); Bx = Buf("x")
    h_sb = C.sb([128, 16, T], BF16); Bh = Buf("h")
    mod_sb = C.sb([128, 16, 11]); Bmod = Buf()
    gs_sb = C.sb([128, 16, 2]); Bgs = Buf()
    ones_d = C.sb([128, 128]); ones_g = C.sb([128, 128]); Bones = Buf()
    tmp = C.ring(6, [128, 512])
    rs_ring = C.ring(1, [128, 512])
    sq_ring = C.ring(1, [128, 4, 512])
    for q4 in range(4):
        P.dma(x_sb[:, 4 * q4:4 * q4 + 4, :], xT[:, 4 * q4:4 * q4 + 4, :], writes=[Bx])
    P.dma(mod_sb[:], modv, writes=[Bmod])
    P.add("pool", lambda e: e.memset(ones_d[:], 1.0 / 2048.0), [], [Bones])
    P.add("pool", lambda e: e.memset(ones_g[:], 1.0 / 512.0), [], [Bones])
    for j, col in enumerate((4, 6)):
        P.add("dve", lambda e, j=j, col=col: e.scalar_tensor_tensor(
            out=gs_sb[:, :, j], in0=mod_sb[:, :, col], scalar=1.0, in1=mod_sb[:, :, 3],
            op0=ALU.add, op1=ALU.mult), [Bmod], [Bgs])

    def rstd_from(pt, Bp, N):
        rstd, Br = rs_ring.next()
        P.add("act", lambda e: e.activation(out=rstd[:, :N], in_=pt[:, :N], func=AF.Ln, bias=EPS), [Bp], [Br])
        P.add("act", lambda e: e.activation(out=rstd[:, :N], in_=rstd[:, :N], func=AF.Exp, scale=-0.5), [Br], [Br])
        return rstd, Br

    C.push()
    mix_ring = C.ring(2, [128, 4, T])
    w_ring = C.ring(2, [128, 16, 256], BF16)
    for mg in range(4):
        mix, Bmix = mix_ring.next()
        P.dma(mix[:], mixT[:, 4 * mg:4 * mg + 4, :], writes=[Bmix])
        for bi, (n0, N) in enumerate(blks):
            sq, Bsq = sq_ring.next()
            P.add("act", lambda e, sq=sq, n0=n0, N=N, mix=mix: e.activation(
                out=sq[:, :, :N], in_=mix[:, :, n0:n0 + N], func=AF.Square), [Bmix], [Bsq])
            pt, Bp = ps.next()
            for kk in range(4):
                P.mm(pt[:, :N], ones_g[:], sq[:, kk, :N], kk == 0, kk == 3, reads=[Bones, Bsq], writes=[Bp])
            rstd, Br = rstd_from(pt, Bp, N)
            for kk in range(4):
                k = 4 * mg + kk
                P.add("dve", lambda e, k=k, kk=kk, n0=n0, N=N, rstd=rstd, mix=mix: e.scalar_tensor_tensor(
                    out=h_sb[:, k, n0:n0 + N], in0=mix[:, kk, n0:n0 + N], scalar=mod_sb[:, k, 0:1], in1=rstd[:, :N],
                    op0=ALU.mult, op1=ALU.mult), [Bmix, Bmod, Br], [Bh])
    wt_next = None

    def load_wo(jj):
        wt, Bw = w_ring.next()
        P.dma(wt[:], w_out[:, jj * 256:(jj + 1) * 256].rearrange("(k p) c -> p k c", p=128), writes=[Bw], q="pool")
        return wt, Bw
    wt_next = load_wo(0)
    for jj in range(8):
        wt, Bw = wt_next
        if jj + 1 < 8:
            wt_next = load_wo(jj + 1)
        for j2 in range(2):
            j = 2 * jj + j2
            for bi, (n0, N) in enumerate(blks):
                seg = 0 if bi < 2 else 1
                pt, Bp = ps.next()
                for k in range(16):
                    P.mm(pt[:, :N], wt[:, k, j2 * 128:(j2 + 1) * 128], h_sb[:, k, n0:n0 + N], k == 0, k == 15,
                         reads=[Bw, Bh], writes=[Bp])
                P.add("dve", lambda e, pt=pt, j=j, n0=n0, N=N, seg=seg: e.scalar_tensor_tensor(
                    out=x_sb[:, j, n0:n0 + N], in0=pt[:, :N], scalar=mod_sb[:, j, 1 + seg:2 + seg], in1=x_sb[:, j, n0:n0 + N],
                    op0=ALU.mult, op1=ALU.add), [Bp, Bmod, Bx], [Bx])
    C.pop()

    C.push()
    if moe:
        rt_sb = C.sb([128, 16, 8]); ident = C.sb([128, 128]); sel = C.sb([8, 8, 128]); Brt = Buf()
        P.dma(rt_sb[:], router, writes=[Brt]); P.dma(ident[:], d_ident, writes=[Brt]); P.dma(sel[:], d_sel, writes=[Brt])
        lg = C.sb([8, 1024]); Blg = Buf()
        gate_tm = C.sb([128, 8, 8]); Bgtm = Buf()
        hf_ring = C.ring(2, [128, 512])
        sm8 = C.ring(8, [128, 8])
        sm1 = C.ring(8, [128, 1])
    for bi, (n0, N) in enumerate(blks):
        seg = 0 if bi < 2 else 1
        pt, Bp = ps.next()
        for k4 in range(4):
            sq, Bsq = sq_ring.next()
            P.add("act", lambda e, sq=sq, n0=n0, N=N, k4=k4: e.activation(
                out=sq[:, :, :N], in_=x_sb[:, 4 * k4:4 * k4 + 4, n0:n0 + N], func=AF.Square), [Bx], [Bsq])
            for kk in range(4):
                k = 4 * k4 + kk
                P.mm(pt[:, :N], ones_d[:], sq[:, kk, :N], k == 0, k == 15, reads=[Bones, Bsq], writes=[Bp])
        rstd, Br = rstd_from(pt, Bp, N)
        if moe:
            pl, Bpl = ps.next()
        for k in range(16):
            t, Bt = tmp.next()
            P.add("dve", lambda e, t=t, k=k, n0=n0, N=N, seg=seg, rstd=rstd: e.scalar_tensor_tensor(
                out=t[:, :N], in0=x_sb[:, k, n0:n0 + N], scalar=gs_sb[:, k, seg:seg + 1], in1=rstd[:, :N],
                op0=ALU.mult, op1=ALU.mult), [Bx, Bgs, Br], [Bt])
            P.add("act", lambda e, t=t, k=k, n0=n0, N=N, seg=seg: e.activation(
                out=h_sb[:, k, n0:n0 + N], in_=t[:, :N], func=AF.Identity,
                bias=mod_sb[:, k, 5 + 2 * seg:6 + 2 * seg], scale=1.0), [Bt, Bmod], [Bh])
            if moe:
                hf, Bhf = hf_ring.next()
                P.add("pool", lambda e, hf=hf, t=t, k=k, N=N: e.tensor_scalar(
                    out=hf[:, :N], in0=t[:, :N], scalar1=mod_sb[:, k, 5:6], scalar2=None, op0=ALU.add), [Bt, Bmod], [Bhf])
                P.mm(pl[0:8, :N], rt_sb[:, k, :], hf[:, :N], k == 0, k == 15, reads=[Brt, Bhf], writes=[Bpl])
        if moe:
            P.add("dve", lambda e, pl=pl, n0=n0, N=N: e.tensor_copy(out=lg[:, n0:n0 + N], in_=pl[0:8, :N]), [Bpl], [Blg])
    if moe:
        for ti in range(8):
            pt, Bp = ps.next()
            P.add("pe", lambda e, pt=pt, ti=ti: e.transpose(pt[:, 0:8], lg[0:8, ti * 128:(ti + 1) * 128], ident[0:8, 0:8]),
                  [Blg, Brt], [Bp])
            l_, Bl = sm8.next(); m1, Bm1 = sm1.next(); e1, Be1 = sm8.next(); l2, Bl2 = sm8.next(); m2, Bm2 = sm1.next()
            P.add("dve", lambda e, l_=l_, pt=pt: e.tensor_copy(out=l_[:], in_=pt[:, 0:8]), [Bp], [Bl])
            P.add("dve", lambda e, l_=l_, m1=m1: e.tensor_reduce(out=m1[:], in_=l_[:], axis=AX.X, op=ALU.max), [Bl], [Bm1])
            P.add("dve", lambda e, l_=l_, m1=m1, e1=e1: e.tensor_scalar(
                out=e1[:], in0=l_[:], scalar1=m1[:, 0:1], scalar2=None, op0=ALU.is_equal), [Bl, Bm1], [Be1])
            P.add("dve", lambda e, l_=l_, e1=e1, l2=l2: e.scalar_tensor_tensor(
                out=l2[:], in0=e1[:], scalar=-1e30, in1=l_[:], op0=ALU.mult, op1=ALU.add), [Bl, Be1], [Bl2])
            P.add("dve", lambda e, l2=l2, m2=m2: e.tensor_reduce(out=m2[:], in_=l2[:], axis=AX.X, op=ALU.max), [Bl2], [Bm2])
            s2, Bs2 = sm8.next(); ex, Bex = sm8.next(); nm1, Bnm1 = sm1.next(); ssum, Bss = sm1.next()
            P.add("dve", lambda e, l_=l_, m2=m2, s2=s2: e.tensor_scalar(
                out=s2[:], in0=l_[:], scalar1=m2[:, 0:1], scalar2=None, op0=ALU.is_ge), [Bl, Bm2], [Bs2])
            P.add("dve", lambda e, m1=m1, nm1=nm1: e.tensor_scalar(
                out=nm1[:], in0=m1[:], scalar1=-1.0, scalar2=None, op0=ALU.mult), [Bm1], [Bnm1])
            P.add("act", lambda e, l_=l_, ex=ex, nm1=nm1: e.activation(out=ex[:], in_=l_[:], func=AF.Exp, bias=nm1[:, 0:1]),
                  [Bl, Bnm1], [Bex])
            P.add("dve", lambda e, ex=ex, s2=s2: e.tensor_tensor(out=ex[:], in0=ex[:], in1=s2[:], op=ALU.mult), [Bex, Bs2], [Bex])
            P.add("dve", lambda e, ex=ex, ssum=ssum: e.tensor_reduce(out=ssum[:], in_=ex[:], axis=AX.X, op=ALU.add), [Bex], [Bss])
            P.add("dve", lambda e, ssum=ssum: e.reciprocal(out=ssum[:], in_=ssum[:]), [Bss], [Bss])
            P.add("dve", lambda e, ex=ex, ssum=ssum, ti=ti: e.tensor_scalar(
                out=gate_tm[:, ti, :], in0=ex[:], scalar1=ssum[:, 0:1], scalar2=None, op0=ALU.mult), [Bex, Bss], [Bgtm])
            p2, Bp2 = ps.next()
            P.add("pe", lambda e, p2=p2, ti=ti: e.transpose(p2[0:8, 0:128], gate_tm[:, ti, :], ident[:]), [Bgtm, Brt], [Bp2])
            P.add("dve", lambda e, p2=p2, ti=ti: e.tensor_copy(out=lg[:, ti * 128:(ti + 1) * 128], in_=p2[0:8, 0:128]),
                  [Bp2, Blg], [Blg])
        P.dma(go.rearrange("(t p) e -> p t e", p=128), gate_tm[:], reads=[Bgtm])
        for q4 in range(4):
            P.dma(xo[:, 4 * q4:4 * q4 + 4, :], x_sb[:, 4 * q4:4 * q4 + 4, :], reads=[Bx])
            P.dma(h2o[:, 4 * q4:4 * q4 + 4, :], h_sb[:, 4 * q4:4 * q4 + 4, :], reads=[Bh])
        C.pop()
        return C.done()

    act = C.sb([128, nh, T], BF16); Bact = Buf()
    w13_ring = C.ring(2, [128, 16, 256], BF16)
    w2_ring = C.ring(2, [128, nh, 128], BF16)
    gbc_ring = C.ring(1, [128, 1024]) if moe else None
    ffw = D_FFE if moe else D_FF
    for ex_i in range(8 if moe else 1):
        w13e = w13[ex_i] if moe else w13
        w2e = w2[ex_i] if moe else w2
        if moe:
            gbc, Bgbc = gbc_ring.next()
            for bi, (n0, N) in enumerate(blks):
                pt, Bp = ps.next()
                P.mm(pt[:, :N], sel[:, ex_i, :], lg[:, n0:n0 + N], True, True, reads=[Brt, Blg], writes=[Bp])
                P.add("dve", lambda e, gbc=gbc, pt=pt, n0=n0, N=N: e.tensor_copy(out=gbc[:, n0:n0 + N], in_=pt[:, :N]),
                      [Bp], [Bgbc])
        for half in range(nsp):
            def load13(f):
                wt, Bw = w13_ring.next()
                c0 = (half * nh + f) * 128
                P.dma(wt[:, :, 0:128], w13e[:, c0:c0 + 128].rearrange("(k p) c -> p k c", p=128), writes=[Bw], q="pool")
                P.dma(wt[:, :, 128:256], w13e[:, ffw + c0:ffw + c0 + 128].rearrange("(k p) c -> p k c", p=128),
                      writes=[Bw], q="pool")
                return wt, Bw
            nxt = load13(0)
            for f in range(nh):
                wt, Bw = nxt
                if f + 1 < nh:
                    nxt = load13(f + 1)
                for bi, (n0, N) in enumerate(blks):
                    pg, Bpg = ps.next(); pu, Bpu = ps.next()
                    for k in range(16):
                        P.mm(pg[:, :N], wt[:, k, 0:128], h_sb[:, k, n0:n0 + N], k == 0, k == 15, reads=[Bw, Bh], writes=[Bpg])
                    for k in range(16):
                        P.mm(pu[:, :N], wt[:, k, 128:256], h_sb[:, k, n0:n0 + N], k == 0, k == 15, reads=[Bw, Bh], writes=[Bpu])
                    t, Bt = tmp.next()
                    P.add("act", lambda e, t=t, pg=pg, N=N: e.activation(out=t[:, :N], in_=pg[:, :N], func=AF.Silu), [Bpg], [Bt])
                    P.add("dve", lambda e, t=t, pu=pu, f=f, n0=n0, N=N: e.tensor_tensor(
                        out=act[:, f, n0:n0 + N], in0=pu[:, :N], in1=t[:, :N], op=ALU.mult), [Bpu, Bt], [Bact])

            def load2(j):
                wt, Bw = w2_ring.next()
                r0 = half * nh * 128
                P.dma(wt[:], w2e[r0:r0 + nh * 128, j * 128:(j + 1) * 128].rearrange("(f p) c -> p f c", p=128),
                      writes=[Bw], q="pool")
                return wt, Bw
            nxt = load2(0)
            for j in range(16):
                wt, Bw = nxt
                if j + 1 < 16:
                    nxt = load2(j + 1)
                for bi, (n0, N) in enumerate(blks):
                    seg = 0 if bi < 2 else 1
                    pt, Bp = ps.next()
                    for f in range(nh):
                        P.mm(pt[:, :N], wt[:, f, :], act[:, f, n0:n0 + N], f == 0, f == nh - 1, reads=[Bw, Bact], writes=[Bp])
                    if moe:
                        t, Bt = tmp.next()
                        P.add("dve", lambda e, t=t, pt=pt, j=j, n0=n0, N=N, gbc=gbc: e.scalar_tensor_tensor(
                            out=t[:, :N], in0=pt[:, :N], scalar=mod_sb[:, j, 8:9], in1=gbc[:, n0:n0 + N],
                            op0=ALU.mult, op1=ALU.mult), [Bp, Bmod, Bgbc], [Bt])
                        P.add("pool", lambda e, t=t, j=j, n0=n0, N=N: e.tensor_tensor(
                            out=x_sb[:, j, n0:n0 + N], in0=x_sb[:, j, n0:n0 + N], in1=t[:, :N], op=ALU.add), [Bt, Bx], [Bx])
                    else:
                        P.add("dve", lambda e, pt=pt, j=j, n0=n0, N=N, seg=seg: e.scalar_tensor_tensor(
                            out=x_sb[:, j, n0:n0 + N], in0=pt[:, :N], scalar=mod_sb[:, j, 8 + seg:9 + seg],
                            in1=x_sb[:, j, n0:n0 + N], op0=ALU.mult, op1=ALU.add), [Bp, Bmod, Bx], [Bx])

    if moe:
        for bi, (n0, N) in enumerate(blks):
            pt, Bp = ps.next()
            for k4 in range(4):
                sq, Bsq = sq_ring.next()
                P.add("act", lambda e, sq=sq, n0=n0, N=N, k4=k4: e.activation(
                    out=sq[:, :, :N], in_=x_sb[:, 4 * k4:4 * k4 + 4, n0:n0 + N], func=AF.Square), [Bx], [Bsq])
                for kk in range(4):
                    k = 4 * k4 + kk
                    P.mm(pt[:, :N], ones_d[:], sq[:, kk, :N], k == 0, k == 15, reads=[Bones, Bsq], writes=[Bp])
            rstd, Br = rstd_from(pt, Bp, N)
            for k in range(16):
                P.add("dve", lambda e, k=k, n0=n0, N=N, rstd=rstd: e.scalar_tensor_tensor(
                    out=x_sb[:, k, n0:n0 + N], in0=x_sb[:, k, n0:n0 + N], scalar=mod_sb[:, k, 10:11], in1=rstd[:, :N],
                    op0=ALU.mult, op1=ALU.mult), [Bx, Bmod, Br], [Bx])
        for q4 in range(4):
            P.dma(xo[:, 4 * q4:4 * q4 + 4, :], x_sb[:, 4 * q4:4 * q4 + 4, :], reads=[Bx])
    else:
        for q4 in range(4):
            P.dma(xo[:, 4 * q4:4 * q4 + 4, :], x_sb[:, 4 * q4:4 * q4 + 4, :], reads=[Bx])
    C.pop()
    return C.done()


def build_k4():
    C = Ctx()
    nc, P = C.nc, C.P
    NTT = 8
    h2 = C.din("h2", [NTT, 128, 16, 1024], BF16)
    gate = C.din("gate", [128, NTT * 1024])
    w13 = C.din("w13", [2048, 2 * D_FFE]); w2 = C.din("w2", [D_FFE, 2048])
    yo = C.dout("y", [NTT, 128, 16, 1024])
    nsp = 4
    nh = (D_FFE // 128) // nsp
    ps = Ring(C.psum_ring(8).items)
    h_ring = C.ring(2, [128, 16, 1024], BF16)
    acc_ring = C.ring(1, [128, 16, 1024])
    g_ring = C.ring(2, [128, 1024])
    act = C.sb([128, nh, 1024], BF16); Bact = Buf()
    w13_ring = C.ring(3, [128, 16, 256], BF16)
    w2_ring = C.ring(3, [128, nh, 128], BF16)
    tmp = C.ring(4, [128, 512])
    blks = TBLK[:2]
    nxt_h = None

    def load_h(tt):
        h_sb, Bh = h_ring.next(); g_sb, Bg = g_ring.next()
        for q4 in range(4):
            P.dma(h_sb[:, 4 * q4:4 * q4 + 4, :], h2[tt, :, 4 * q4:4 * q4 + 4, :], writes=[Bh])
        P.dma(g_sb[:], gate[:, tt * 1024:(tt + 1) * 1024], writes=[Bg])
        return h_sb, Bh, g_sb, Bg
    nxt_h = load_h(0)
    for tt in range(NTT):
        h_sb, Bh, g_sb, Bg = nxt_h
        if tt + 1 < NTT:
            nxt_h = load_h(tt + 1)
        acc, Bacc = acc_ring.next()
        for sp in range(nsp):
            def load13(f):
                wt, Bw = w13_ring.next()
                c0 = (sp * nh + f) * 128
                P.dma(wt[:, :, 0:128], w13[:, c0:c0 + 128].rearrange("(k p) c -> p k c", p=128), writes=[Bw], q="pool")
                P.dma(wt[:, :, 128:256], w13[:, D_FFE + c0:D_FFE + c0 + 128].rearrange("(k p) c -> p k c", p=128),
                      writes=[Bw], q="pool")
                return wt, Bw
            q13 = [load13(0), load13(1)]
            for f in range(nh):
                wt, Bw = q13.pop(0)
                if f + 2 < nh:
                    q13.append(load13(f + 2))
                for bi, (n0, N) in enumerate(blks):
                    pg, Bpg = ps.next(); pu, Bpu = ps.next()
                    for k in range(16):
                        P.mm(pg[:, :N], wt[:, k, 0:128], h_sb[:, k, n0:n0 + N], k == 0, k == 15, reads=[Bw, Bh], writes=[Bpg])
                    for k in range(16):
                        P.mm(pu[:, :N], wt[:, k, 128:256], h_sb[:, k, n0:n0 + N], k == 0, k == 15, reads=[Bw, Bh], writes=[Bpu])
                    t, Bt = tmp.next()
                    P.add("act", lambda e, t=t, pg=pg, N=N: e.activation(out=t[:, :N], in_=pg[:, :N], func=AF.Silu), [Bpg], [Bt])
                    P.add("dve", lambda e, t=t, pu=pu, f=f, n0=n0, N=N: e.tensor_tensor(
                        out=act[:, f, n0:n0 + N], in0=pu[:, :N], in1=t[:, :N], op=ALU.mult), [Bpu, Bt], [Bact])

            def load2(j):
                wt, Bw = w2_ring.next()
                r0 = sp * nh * 128
                P.dma(wt[:], w2[r0:r0 + nh * 128, j * 128:(j + 1) * 128].rearrange("(f p) c -> p f c", p=128),
                      writes=[Bw], q="pool")
                return wt, Bw
            q2 = [load2(0), load2(1)]
            for j in range(16):
                wt, Bw = q2.pop(0)
                if j + 2 < 16:
                    q2.append(load2(j + 2))
                for bi, (n0, N) in enumerate(blks):
                    pt, Bp = ps.next()
                    for f in range(nh):
                        P.mm(pt[:, :N], wt[:, f, :], act[:, f, n0:n0 + N], f == 0, f == nh - 1, reads=[Bw, Bact], writes=[Bp])
                    if sp == 0:
                        P.add("dve", lambda e, pt=pt, j=j, n0=n0, N=N, acc=acc, g_sb=g_sb: e.tensor_tensor(
                            out=acc[:, j, n0:n0 + N], in0=pt[:, :N], in1=g_sb[:, n0:n0 + N], op=ALU.mult), [Bp, Bg], [Bacc])
                    else:
                        t, Bt = tmp.next()
                        P.add("dve", lambda e, t=t, pt=pt, n0=n0, N=N, g_sb=g_sb: e.tensor_tensor(
                            out=t[:, :N], in0=pt[:, :N], in1=g_sb[:, n0:n0 + N], op=ALU.mult), [Bp, Bg], [Bt])
                        P.add("pool", lambda e, t=t, j=j, n0=n0, N=N, acc=acc: e.tensor_tensor(
                            out=acc[:, j, n0:n0 + N], in0=acc[:, j, n0:n0 + N], in1=t[:, :N], op=ALU.add), [Bt, Bacc], [Bacc])
        for q4 in range(4):
            P.dma(yo[tt, :, 4 * q4:4 * q4 + 4, :], acc[:, 4 * q4:4 * q4 + 4, :], reads=[Bacc])
    return C.done()


def build_k5():
    C = Ctx()
    nc, P = C.nc, C.P
    xm = C.din("xm", [128, 16, 1024])
    ys = C.din("ys", [8, 128, 16, 1024])
    gv = C.din("gv", [128, 16, 2])
    out = C.dout("out", [128, 16, 1024])
    ps = Ring(C.psum_ring(8).items)
    x_sb = C.sb([128, 16, 1024]); Bx = Buf()
    g_sb = C.sb([128, 16, 2]); Bg = Buf()
    ones_d = C.sb([128, 128]); Bones = Buf()
    y_ring = C.ring(3, [128, 4, 1024])
    acc_ring = C.ring(2, [128, 4, 1024])
    sq_ring = C.ring(1, [128, 4, 512])
    rs_ring = C.ring(1, [128, 512])
    P.dma(g_sb[:], gv, writes=[Bg])
    P.add("pool", lambda e: e.memset(ones_d[:], 1.0 / 2048.0), [], [Bones])
    for k4 in range(4):
        ks = slice(4 * k4, 4 * k4 + 4)
        P.dma(x_sb[:, ks, :], xm[:, ks, :], writes=[Bx])
        acc, Bacc = acc_ring.next()
        for e_ in range(8):
            y, By = y_ring.next()
            P.dma(y[:], ys[e_, :, ks, :], writes=[By])
            if e_ == 0:
                continue_first = (y, By)
                continue
            eng = "dve" if e_ % 2 else "pool"
            if e_ == 1:
                y0, By0 = continue_first
                P.add(eng, lambda e, acc=acc, y=y, y0=y0: e.tensor_tensor(out=acc[:], in0=y0[:], in1=y[:], op=ALU.add),
                      [By, By0], [Bacc])
            else:
                P.add(eng, lambda e, acc=acc, y=y: e.tensor_tensor(out=acc[:], in0=acc[:], in1=y[:], op=ALU.add),
                      [By, Bacc], [Bacc])
        for kk in range(4):
            k = 4 * k4 + kk
            P.add("dve", lambda e, acc=acc, kk=kk, k=k: e.scalar_tensor_tensor(
                out=x_sb[:, k, :], in0=acc[:, kk, :], scalar=g_sb[:, k, 0:1], in1=x_sb[:, k, :], op0=ALU.mult, op1=ALU.add),
                  [Bacc, Bg, Bx], [Bx])
    for bi, (n0, N) in enumerate(TBLK[:2]):
        pt, Bp = ps.next()
        for k4 in range(4):
            sq, Bsq = sq_ring.next()
            P.add("act", lambda e, sq=sq, n0=n0, N=N, k4=k4: e.activation(
                out=sq[:, :, :N], in_=x_sb[:, 4 * k4:4 * k4 + 4, n0:n0 + N], func=AF.Square), [Bx], [Bsq])
            for kk in range(4):
                k = 4 * k4 + kk
                P.mm(pt[:, :N], ones_d[:], sq[:, kk, :N], k == 0, k == 15, reads=[Bones, Bsq], writes=[Bp])
        rstd, Br = rs_ring.next()
        P.add("act", lambda e, rstd=rstd, pt=pt, N=N: e.activation(out=rstd[:, :N], in_=pt[:, :N], func=AF.Ln, bias=EPS), [Bp], [Br])
        P.add("act", lambda e, rstd=rstd, N=N: e.activation(out=rstd[:, :N], in_=rstd[:, :N], func=AF.Exp, scale=-0.5), [Br], [Br])
        for k in range(16):
            P.add("dve", lambda e, k=k, n0=n0, N=N, rstd=rstd: e.scalar_tensor_tensor(
                out=x_sb[:, k, n0:n0 + N], in0=x_sb[:, k, n0:n0 + N], scalar=g_sb[:, k, 1:2], in1=rstd[:, :N],
                op0=ALU.mult, op1=ALU.mult), [Bx, Bg, Br], [Bx])
    for q4 in range(4):
        P.dma(out[:, 4 * q4:4 * q4 + 4, :], x_sb[:, 4 * q4:4 * q4 + 4, :], reads=[Bx])
    return C.done()


def _mix_for_core(r2, b, q, T):
    mix = np.empty((128, 16, T), np.float32)
    for mi, key in enumerate(("oa", "ob", "oc", "od")):
        for hh in range(4):
            o = np.asarray(r2[4 * b + hh][key])
            mix[:, mi * 4 + hh, :1024] = o[:, 256 + q * 1024:256 + (q + 1) * 1024]
            if T > 1024:
                mix[:, mi * 4 + hh, 1024:] = o[:, q * 64:(q + 1) * 64]
    return mix


def _modv3(l, b, m, p):
    ch = lambda r, c: _fm(m[l, r, c * 2048:(c + 1) * 2048])
    cols = [_fm(p["out_norm"][l]), ch(b, 2), ch(2, 2), _fm(p["norm_ffn"][l]), ch(b, 4), ch(b, 3), ch(2, 4), ch(2, 3),
            ch(b, 5), ch(2, 5), _fm(p["final_norm"])]
    return np.ascontiguousarray(np.stack(cols, axis=2).astype(np.float32))


def run_k3_dense(l, xfm, r2, m, p):
    in_maps = []
    w_out = np.ascontiguousarray(p["w_out"][l]); w13 = np.ascontiguousarray(p["ffn_w13"][l // 2])
    w2 = np.ascontiguousarray(p["ffn_w2"][l // 2])
    for i in range(NCORES):
        b, q = i // 4, i % 4
        in_maps.append({"xT": xfm[i], "mixT": _mix_for_core(r2, b, q, T1), "modv": _modv3(l, b, m, p),
                        "w_out": w_out, "w13": w13, "w2": w2})
    res = _run(build_k3(False), in_maps)
    return [np.asarray(r["xo"]) for r in res]


def run_moe_layer(l, xfm, r2, m, p):
    w_out = np.ascontiguousarray(p["w_out"][l])
    router = np.ascontiguousarray(p["router"][l // 2].reshape(16, 128, 8).transpose(1, 0, 2))
    ident = np.eye(128, dtype=np.float32)
    sel = np.zeros((8, 8, 128), np.float32)
    for e in range(8):
        sel[e, e, :] = 1.0
    in_maps = []
    for i in range(NCORES):
        b, q = i // 4, i % 4
        in_maps.append({"xT": np.ascontiguousarray(xfm[i][:, :, :1024]), "mixT": _mix_for_core(r2, b, q, 1024),
                        "modv": _modv3(l, b, m, p), "w_out": w_out, "router": router, "ident": ident, "sel": sel})
    r3 = _run(build_k3(True), in_maps)
    h2_all = np.ascontiguousarray(np.stack([np.asarray(r["h2o"]) for r in r3], axis=0))
    gates = np.concatenate([np.asarray(r["gates"]) for r in r3], axis=0)
    in_maps = []
    for e in range(NCORES):
        in_maps.append({"h2": h2_all, "gate": np.ascontiguousarray(np.broadcast_to(gates[None, :, e], (128, 8192))),
                        "w13": np.ascontiguousarray(p["moe_w13"][l // 2][e]), "w2": np.ascontiguousarray(p["moe_w2"][l // 2][e])})
    r4 = _run(build_k4(), in_maps)
    in_maps = []
    for i in range(NCORES):
        b = i // 4
        ys = np.ascontiguousarray(np.stack([np.asarray(r4[e]["y"][i]) for e in range(8)], axis=0))
        gv = np.ascontiguousarray(np.stack([_fm(m[l, b, 5 * 2048:6 * 2048]), _fm(p["final_norm"])], axis=2).astype(np.float32))
        in_maps.append({"xm": np.asarray(r3[i]["xo"]), "ys": ys, "gv": gv})
    r5 = _run(build_k5(), in_maps)
    return [np.asarray(r["out"]) for r in r5]


def kernel(**inputs):
    p = {k: np.asarray(v) for k, v in inputs.items()}
    x, ctx = p["x"], p["ctx"]
    m = run_mod(p["c"], p["c_ctx"], p["w_mod"], p["b_mod"])
    xfm = []
    for i in range(NCORES):
        b, q = i // 4, i % 4
        tok = np.concatenate([x[b, q * 1024:(q + 1) * 1024], ctx[b, q * 64:(q + 1) * 64]], axis=0)
        xfm.append(_tok_to_fm(tok))
    r1 = run_k1(0, xfm, m, p)
    r2 = run_k2(0, r1, p)
    xfm = run_k3_dense(0, xfm, r2, m, p)
    r1 = run_k1(1, xfm, m, p)
    r2 = run_k2(1, r1, p)
    outs = run_moe_layer(1, xfm, r2, m, p)
    out = np.empty((2, 4096, 2048), np.float32)
    for i in range(NCORES):
        b, q = i // 4, i % 4
        out[b, q * 1024:(q + 1) * 1024] = _fm_to_tok(outs[i])
    return out
```

```python
import numpy as np
from contextlib import ExitStack
import concourse.bass as bass
import concourse.mybir as mybir
from concourse.bass_utils import run_bass_kernel_spmd

F32 = mybir.dt.float32
BF16 = mybir.dt.bfloat16
AF = mybir.ActivationFunctionType
ALU = mybir.AluOpType
AX = mybir.AxisListType
NCORES = 8


class Buf:
    __slots__ = ("name", "w", "r")

    def __init__(self, name=""):
        self.name = name
        self.w = None
        self.r = {}


class _Op:
    __slots__ = ("eng", "fn", "deps", "sig", "val", "dma", "key", "cc")


class Prog:
    ENGS = ("pe", "act", "dve", "pool", "sp")

    def __init__(self, nc):
        self.nc = nc
        self.ops = []
        self.last = {}
        self.dmas = []
        self.bar = {}

    def barrier(self):
        deps = list(self.last.values()) + list(self.dmas)
        for d in deps:
            d.sig = True
        self.dmas = []
        for e in self.ENGS:
            self.bar[e] = list(self.bar.get(e, [])) + deps

    def add(self, eng, fn, reads=(), writes=(), dma=False, cc=False):
        op = _Op()
        op.eng, op.fn, op.dma, op.sig, op.val = eng, fn, dma, dma, 0
        op.cc = cc
        deps = set()
        for b in reads:
            if b.w is not None:
                deps.add(b.w)
        for b in writes:
            if b.w is not None:
                deps.add(b.w)
            for r in b.r.values():
                deps.add(r)
        key = (eng, dma)
        for b in reads:
            b.r[key] = op
        for b in writes:
            b.w = op
            b.r = {}
        deps.discard(op)
        if eng == "pe" and not dma:
            deps = {d for d in deps if not (d.eng == "pe" and not d.dma)}
        if self.bar.get(eng):
            deps.update(d for d in self.bar[eng] if not (d.eng == eng and not d.dma and eng == "pe"))
            self.bar[eng] = []
        if dma:
            self.dmas.append(op)
        else:
            self.last[eng] = op
        op.deps = deps
        for d in deps:
            d.sig = True
        self.ops.append(op)
        return op

    def dma(self, out, in_, reads=(), writes=(), q="sp"):
        return self.add(q, lambda e: e.dma_start(out=out, in_=in_), reads, writes, dma=True)

    def mm(self, out, lhsT, rhs, start, stop, reads=(), writes=()):
        return self.add("pe", lambda e: e.matmul(out, lhsT, rhs, start=start, stop=stop), reads, writes)

    NDS = 24

    def finish(self):
        nc = self.nc
        cnt = {}
        dma_hist = {}
        ncc = 0
        for op in self.ops:
            if op.cc:
                op.key = ("cc", True, ncc)
                ncc += 1
                op.val = 1
                cnt[op.key] = 1
            elif op.dma:
                hist = dma_hist.setdefault(op.eng, [])
                j = len(hist)
                if j >= self.NDS:
                    op.deps.add(hist[j - self.NDS])
                hist.append(op)
                op.key = (op.eng, True, j % self.NDS)
                op.val = 16 * (j // self.NDS + 1)
                cnt[op.key] = op.val
            elif op.sig:
                op.key = (op.eng, False, 0)
                cnt[op.key] = cnt.get(op.key, 0) + 1
                op.val = cnt[op.key]
        with ExitStack() as es:
            sems = {}
            for k in cnt:
                sems[k] = es.enter_context(nc.semaphore("s_%s_%d_%d" % (k[0], int(k[1]), k[2])))
            block = es.enter_context(nc.Block())
            reg = {"pe": block.tensor, "act": block.scalar, "dve": block.vector,
                   "pool": block.gpsimd, "sp": block.sync}
            for eng in self.ENGS:
                ops_e = [op for op in self.ops if op.eng == eng]
                if not ops_e:
                    continue

                def body(e, ops_e=ops_e, eng=eng):
                    waited = {}
                    for op in ops_e:
                        need = {}
                        for d in op.deps:
                            if d.val > need.get(d.key, 0):
                                need[d.key] = d.val
                        for k, v in need.items():
                            if waited.get(k, 0) < v:
                                e.wait_ge(sems[k], v)
                                waited[k] = v
                        ins = op.fn(e)
                        if op.cc:
                            ins.then_inc(sems[op.key])
                        elif op.sig:
                            ins.then_inc(sems[op.key], 16 if op.dma else 1)
                    for k, v in cnt.items():
                        if (k[0] == eng or (k[0] == "cc" and eng == "pool")) and k[1] and waited.get(k, 0) < v:
                            e.wait_ge(sems[k], v)

                reg[eng](body)


def _run(nc, in_maps):
    res = run_bass_kernel_spmd(nc, in_maps, core_ids=list(range(NCORES)))
    return res.results


MOD_COLS = 12288 // NCORES


def build_mod():
    nc = bass.Bass("TRN2", target_bir_lowering=False)
    cT = nc.dram_tensor("cT", [128, 16, 3], F32, kind="ExternalInput").ap()
    w = nc.dram_tensor("w", [2, 2048, MOD_COLS], F32, kind="ExternalInput").ap()
    b = nc.dram_tensor("b", [2, 3, MOD_COLS], F32, kind="ExternalInput").ap()
    m = nc.dram_tensor("m", [2, 3, MOD_COLS], F32, kind="ExternalOutput").ap()
    P = Prog(nc)
    with ExitStack() as es:
        sb = lambda name, shape, dt=F32: es.enter_context(nc.sbuf_tensor(name, shape, dt))
        c_sb = sb("c_sb", [128, 16, 3])
        s_sb = sb("s_sb", [128, 16, 3])
        b_sb = sb("b_sb", [3, 2, MOD_COLS])
        o_sb = sb("o_sb", [3, 2, MOD_COLS])
        wt = [sb("wt%d" % i, [128, 16, 512]) for i in range(2)]
        ps = [es.enter_context(nc.psum_tensor("ps%d" % i, [128, 512], F32)) for i in range(2)]
        Bc, Bs, Bb, Bo = Buf("c"), Buf("s"), Buf("b"), Buf("o")
        Bw = [Buf("w0"), Buf("w1")]
        Bp = [Buf("p0"), Buf("p1")]
        P.dma(c_sb[:], cT, writes=[Bc])
        P.dma(b_sb[:], b.rearrange("l r c -> r l c"), writes=[Bb])
        P.add("act", lambda e: e.activation(out=s_sb[:], in_=c_sb[:], func=AF.Silu), [Bc], [Bs])
        it = 0
        for l in range(2):
            for n in range(MOD_COLS // 512):
                j = it % 2
                P.dma(wt[j][:], w[l, :, n * 512:(n + 1) * 512].rearrange("(k p) c -> p k c", p=128),
                      writes=[Bw[j]])
                for k in range(16):
                    P.mm(ps[j][0:3, :], s_sb[:, k, :], wt[j][:, k, :], k == 0, k == 15,
                         reads=[Bs, Bw[j]], writes=[Bp[j]])
                P.add("dve", lambda e, j=j, l=l, n=n: e.tensor_tensor(
                    out=o_sb[:, l, n * 512:(n + 1) * 512], in0=ps[j][0:3, :],
                    in1=b_sb[:, l, n * 512:(n + 1) * 512], op=ALU.add), [Bp[j], Bb], [Bo])
                it += 1
        P.dma(m.rearrange("l r c -> r l c"), o_sb[:], reads=[Bo])
        P.finish()
    return nc


def run_mod(c, c_ctx, w_mod, b_mod):
    call = np.concatenate([c, c_ctx[None]], 0).astype(np.float32)
    cT = np.ascontiguousarray(call.T.reshape(16, 128, 3).transpose(1, 0, 2))
    nc = build_mod()
    in_maps = []
    for i in range(NCORES):
        sl = slice(i * MOD_COLS, (i + 1) * MOD_COLS)
        in_maps.append({"cT": cT, "w": np.ascontiguousarray(w_mod[:, :, sl]),
                        "b": np.ascontiguousarray(np.broadcast_to(b_mod[:, None, sl], (2, 3, MOD_COLS)))})
    res = _run(nc, in_maps)
    return np.concatenate([r["m"] for r in res], axis=2)


class Ring:
    def __init__(self, items):
        self.items = items
        self.i = 0

    def next(self):
        it = self.items[self.i % len(self.items)]
        self.i += 1
        return it


class Ctx:
    def __init__(self):
        self.nc = bass.Bass("TRN2", target_bir_lowering=False)
        self.P = Prog(self.nc)
        self.es = ExitStack()
        self.stacks = [self.es]
        self.n = 0

    def push(self):
        self.stacks.append(ExitStack())

    def pop(self):
        self.P.barrier()
        self.stacks.pop().close()

    def din(self, name, shape, dt=F32):
        return self.nc.dram_tensor(name, list(shape), dt, kind="ExternalInput").ap()

    def dout(self, name, shape, dt=F32):
        return self.nc.dram_tensor(name, list(shape), dt, kind="ExternalOutput").ap()

    def sb(self, shape, dt=F32, name=None):
        self.n += 1
        return self.stacks[-1].enter_context(self.nc.sbuf_tensor(name or "t%d" % self.n, list(shape), dt))

    def ring(self, n, shape, dt=F32):
        return Ring([(self.sb(shape, dt), Buf()) for _ in range(n)])

    def psum_ring(self, n=8):
        items = []
        for i in range(n):
            t = self.es.enter_context(self.nc.psum_tensor("ps%d" % i, [128, 512], F32))
            items.append((t, Buf("ps%d" % i)))
        return Ring(items)

    def done(self):
        self.P.finish()
        self.es.close()
        return self.nc


EPS = 1e-6
C_AQ, C_BQ, C_CU, C_CV, C_DZ, C_DC = 0, 512, 1024, 1536, 2048, 2560
C_AK, C_AV, C_BK, C_BV, C_DX, C_DB, C_DT = 2816, 3072, 3328, 3840, 4352, 4864, 5120
IN_COLS = 5136
T1 = 1088
TBLK = ((0, 512), (512, 512), (1024, 64))


def build_k1():
    C = Ctx()
    nc, P = C.nc, C.P
    T = T1
    xT = C.din("xT", [128, 16, T])
    modv = C.din("modv", [128, 16, 5])
    w_in = C.din("w_in", [2048, IN_COLS])
    qkg = C.din("qkg", [128, 2])
    ropeC = C.din("ropeC", [128, 1024])
    ropeS = C.din("ropeS", [128, 1024])
    sgug = C.din("sgug", [128, 512])
    rmat = C.din("rmat", [128, 128])
    o_qkA = C.dout("qkA", [6, 128, T], BF16)
    o_bqk = C.dout("bqk", [8, 128, T], BF16)
    o_cu = C.dout("cu", [4, 128, T])
    o_dz = C.dout("dz", [4, 128, T])
    o_dcxb = C.dout("dcxb", [8, 128, T])
    o_av = C.dout("av", [T, 256], BF16)
    o_bv = C.dout("bv", [T, 512], BF16)
    o_vn = C.dout("vn", [T, 512])
    o_ddt = C.dout("ddt", [T, 16])

    x_sb = C.sb([128, 16, T]); Bx = Buf("x")
    h_sb = C.sb([128, 16, T], BF16); Bh = Buf("h")
    mod_sb = C.sb([128, 16, 5]); Bmod = Buf()
    gs_sb = C.sb([128, 16, 2]); Bgs = Buf()
    qkg_sb = C.sb([128, 2]); Bqkg = Buf()
    rc_sb = C.sb([128, 1024]); rs_sb = C.sb([128, 1024]); Brope = Buf()
    sg_sb = C.sb([128, 512]); Bsg = Buf()
    rm_sb = C.sb([128, 128]); Brm = Buf()
    ones_d = C.sb([128, 128]); ones_h = C.sb([128, 128]); Bones = Buf()
    ps = C.psum_ring(8)
    sq_ring = C.ring(2, [128, 4, 512])
    tmp = C.ring(6, [128, 512])
    small = C.ring(4, [128, 2])
    w_ring = C.ring(2, [128, 16, 512], BF16)
    of32 = C.ring(2, [128, T])
    obf = C.ring(2, [128, T], BF16)
    otm = C.ring(2, [128, 512])
    otb = C.ring(2, [128, 512], BF16)
    rs_ring = C.ring(1, [128, 512])

    for q4 in range(4):
        P.dma(x_sb[:, 4 * q4:4 * q4 + 4, :], xT[:, 4 * q4:4 * q4 + 4, :], writes=[Bx])
    P.dma(mod_sb[:], modv, writes=[Bmod])
    P.dma(qkg_sb[:], qkg, writes=[Bqkg])
    P.dma(rc_sb[:], ropeC, writes=[Brope])
    P.dma(rs_sb[:], ropeS, writes=[Brope])
    P.dma(sg_sb[:], sgug, writes=[Bsg])
    P.dma(rm_sb[:], rmat, writes=[Brm])
    P.add("pool", lambda e: e.memset(ones_d[:], 1.0 / 2048.0), [], [Bones])
    P.add("pool", lambda e: e.memset(ones_h[:], 1.0 / 128.0), [], [Bones])
    for j, col in enumerate((1, 3)):
        P.add("dve", lambda e, j=j, col=col: e.scalar_tensor_tensor(
            out=gs_sb[:, :, j], in0=mod_sb[:, :, col], scalar=1.0, in1=mod_sb[:, :, 0],
            op0=ALU.add, op1=ALU.mult), [Bmod], [Bgs])

    for bi, (n0, N) in enumerate(TBLK):
        seg = 0 if bi < 2 else 1
        pt, Bp = ps.next()
        for k4 in range(4):
            sq, Bsq = sq_ring.next()
            P.add("act", lambda e, sq=sq, n0=n0, N=N, k4=k4: e.activation(
                out=sq[:, :, :N], in_=x_sb[:, 4 * k4:4 * k4 + 4, n0:n0 + N], func=AF.Square), [Bx], [Bsq])
            for kk in range(4):
                k = 4 * k4 + kk
                P.mm(pt[:, :N], ones_d[:], sq[:, kk, :N], k == 0, k == 15, reads=[Bones, Bsq], writes=[Bp])
        rstd, Br = rs_ring.next()
        P.add("act", lambda e, rstd=rstd, pt=pt, N=N: e.activation(
            out=rstd[:, :N], in_=pt[:, :N], func=AF.Ln, bias=EPS), [Bp], [Br])
        P.add("act", lambda e, rstd=rstd, N=N: e.activation(
            out=rstd[:, :N], in_=rstd[:, :N], func=AF.Exp, scale=-0.5), [Br], [Br])
        for k in range(16):
            t, Bt = tmp.next()
            P.add("dve", lambda e, t=t, k=k, n0=n0, N=N, seg=seg, rstd=rstd: e.scalar_tensor_tensor(
                out=t[:, :N], in0=x_sb[:, k, n0:n0 + N], scalar=gs_sb[:, k, seg:seg + 1], in1=rstd[:, :N],
                op0=ALU.mult, op1=ALU.mult), [Bx, Bgs, Br], [Bt])
            P.add("act", lambda e, t=t, k=k, n0=n0, N=N, seg=seg: e.activation(
                out=h_sb[:, k, n0:n0 + N], in_=t[:, :N], func=AF.Identity,
                bias=mod_sb[:, k, 2 + 2 * seg:3 + 2 * seg], scale=1.0), [Bt, Bmod], [Bh])

    groups = [
        (C_AQ, 512, "fm", "qn", o_qkA, 0), (C_AK, 256, "fm", "kn", o_qkA, 4),
        (C_BQ, 512, "fm", "cbf", o_bqk, 0), (C_BK, 512, "fm", "cbf", o_bqk, 4),
        (C_CU, 512, "fm", "gelu", o_cu, 0), (C_DZ, 512, "fm", "silu", o_dz, 0),
        (C_DC, 256, "fm", "copy", o_dcxb, 0), (C_DX, 512, "fm", "copy", o_dcxb, 2),
        (C_DB, 256, "fm", "copy", o_dcxb, 6),
        (C_CV, 512, "tm", "vn", o_vn, 0), (C_AV, 256, "tm", "cbf", o_av, 0),
        (C_BV, 512, "tm", "cbf", o_bv, 0), (C_DT, 16, "tm", "copy", o_ddt, 0),
    ]
    wtiles = {}

    def load_w(gi):
        if gi >= len(groups):
            return
        c0, cw = groups[gi][0], groups[gi][1]
        wt, Bw = w_ring.next()
        P.dma(wt[:, :, :cw], w_in[:, c0:c0 + cw].rearrange("(k p) c -> p k c", p=128), writes=[Bw], q="pool")
        wtiles[gi] = (wt, Bw)

    load_w(0)
    for gi, (c0, cw, mode, kind, outT, oi0) in enumerate(groups):
        load_w(gi + 1)
        wt, Bw = wtiles[gi]
        if mode == "fm":
            for j in range(cw // 128):
                isbf = kind in ("qn", "kn", "cbf")
                o, Bo = (obf if isbf else of32).next()
                for bi, (n0, N) in enumerate(TBLK):
                    pt, Bp = ps.next()
                    for k in range(16):
                        P.mm(pt[:, :N], wt[:, k, j * 128:(j + 1) * 128], h_sb[:, k, n0:n0 + N], k == 0, k == 15,
                             reads=[Bw, Bh], writes=[Bp])
                    dst = o[:, n0:n0 + N]
                    if kind in ("cbf", "copy"):
                        P.add("dve", lambda e, dst=dst, pt=pt, N=N: e.tensor_copy(out=dst, in_=pt[:, :N]), [Bp], [Bo])
                    elif kind in ("gelu", "silu"):
                        f = AF.Gelu_apprx_tanh if kind == "gelu" else AF.Silu
                        P.add("act", lambda e, dst=dst, pt=pt, N=N, f=f: e.activation(out=dst, in_=pt[:, :N], func=f),
                              [Bp], [Bo])
                    else:
                        gcol = 0 if kind == "qn" else 1
                        qs, Bqs = tmp.next()
                        sq, Bsq = tmp.next()
                        P.add("act", lambda e, qs=qs, pt=pt, N=N: e.activation(out=qs[:, :N], in_=pt[:, :N], func=AF.Identity),
                              [Bp], [Bqs])
                        P.add("act", lambda e, sq=sq, pt=pt, N=N: e.activation(out=sq[:, :N], in_=pt[:, :N], func=AF.Square),
                              [Bp], [Bsq])
                        p2, Bp2 = ps.next()
                        P.mm(p2[:, :N], ones_h[:], sq[:, :N], True, True, reads=[Bones, Bsq], writes=[Bp2])
                        rstd, Br = tmp.next()
                        P.add("act", lambda e, rstd=rstd, p2=p2, N=N: e.activation(
                            out=rstd[:, :N], in_=p2[:, :N], func=AF.Ln, bias=EPS), [Bp2], [Br])
                        P.add("act", lambda e, rstd=rstd, N=N: e.activation(
                            out=rstd[:, :N], in_=rstd[:, :N], func=AF.Exp, scale=-0.5), [Br], [Br])
                        qn, Bqn = tmp.next()
                        P.add("dve", lambda e, qn=qn, qs=qs, rstd=rstd, N=N, gcol=gcol: e.scalar_tensor_tensor(
                            out=qn[:, :N], in0=qs[:, :N], scalar=qkg_sb[:, gcol:gcol + 1], in1=rstd[:, :N],
                            op0=ALU.mult, op1=ALU.mult), [Bqs, Br, Bqkg], [Bqn])
                        if bi < 2:
                            p3, Bp3 = ps.next()
                            P.mm(p3[:, :N], rm_sb[:], qn[:, :N], True, True, reads=[Brm, Bqn], writes=[Bp3])
                            t1, Bt1 = tmp.next()
                            t2, Bt2 = tmp.next()
                            P.add("dve", lambda e, t1=t1, qn=qn, n0=n0, N=N: e.tensor_tensor(
                                out=t1[:, :N], in0=qn[:, :N], in1=rc_sb[:, n0:n0 + N], op=ALU.mult), [Bqn, Brope], [Bt1])
                            P.add("dve", lambda e, t2=t2, p3=p3, n0=n0, N=N: e.tensor_tensor(
                                out=t2[:, :N], in0=p3[:, :N], in1=rs_sb[:, n0:n0 + N], op=ALU.mult), [Bp3, Brope], [Bt2])
                            P.add("pool", lambda e, dst=dst, t1=t1, t2=t2, N=N: e.tensor_tensor(
                                out=dst, in0=t1[:, :N], in1=t2[:, :N], op=ALU.add), [Bt1, Bt2], [Bo])
                        else:
                            P.add("dve", lambda e, dst=dst, qn=qn, N=N: e.tensor_copy(out=dst, in_=qn[:, :N]), [Bqn], [Bo])
                P.dma(outT[oi0 + j], o[:], reads=[Bo])
        else:
            for ti in range(9):
                t0 = ti * 128
                M = 128 if ti < 8 else 64
                pt, Bp = ps.next()
                for k in range(16):
                    P.mm(pt[:M, :cw], h_sb[:, k, t0:t0 + M], wt[:, k, :cw], k == 0, k == 15,
                         reads=[Bw, Bh], writes=[Bp])
                if kind == "vn":
                    g, Bg = tmp.next()
                    sq, Bsq = tmp.next()
                    ss, Bss = small.next()
                    o, Bo = otm.next()
                    P.add("act", lambda e, g=g, pt=pt, M=M: e.activation(out=g[:M, :], in_=pt[:M, :], func=AF.Gelu_apprx_tanh),
                          [Bp], [Bg])
                    P.add("act", lambda e, g=g, sq=sq, M=M: e.activation(out=sq[:M, :], in_=g[:M, :], func=AF.Square),
                          [Bg], [Bsq])
                    P.add("dve", lambda e, ss=ss, sq=sq, M=M: e.tensor_reduce(out=ss[:M, 0:1], in_=sq[:M, :], axis=AX.X, op=ALU.add),
                          [Bsq], [Bss])
                    P.add("act", lambda e, ss=ss, M=M: e.activation(out=ss[:M, 1:2], in_=ss[:M, 0:1], func=AF.Ln,
                                                                     bias=EPS, scale=1.0 / 512.0), [Bss], [Bss])
                    P.add("act", lambda e, ss=ss, M=M: e.activation(out=ss[:M, 0:1], in_=ss[:M, 1:2], func=AF.Exp, scale=-0.5),
                          [Bss], [Bss])
                    P.add("dve", lambda e, o=o, g=g, ss=ss, M=M: e.scalar_tensor_tensor(
                        out=o[:M, :], in0=g[:M, :], scalar=ss[:M, 0:1], in1=sg_sb[:M, :], op0=ALU.mult, op1=ALU.mult),
                          [Bg, Bss, Bsg], [Bo])
                else:
                    o, Bo = (otb if kind == "cbf" else otm).next()
                    P.add("dve", lambda e, o=o, pt=pt, M=M, cw=cw: e.tensor_copy(out=o[:M, :cw], in_=pt[:M, :cw]), [Bp], [Bo])
                P.dma(outT[t0:t0 + M, :], o[:M, :cw], reads=[Bo])
    return C.done()


def _fm(v):
    return np.ascontiguousarray(v.reshape(16, 128).T)


def _tok_to_fm(t):
    T = t.shape[0]
    return np.ascontiguousarray(t.T.reshape(16, 128, T).transpose(1, 0, 2))


def _fm_to_tok(f):
    T = f.shape[2]
    return np.ascontiguousarray(f.transpose(1, 0, 2).reshape(2048, T).T)


def _rope_tables():
    t = np.arange(4096)
    row = (t // 64).astype(np.float32)
    col = (t % 64).astype(np.float32)
    inv = np.power(np.float32(10000.0), -np.arange(32, dtype=np.float32) / np.float32(32)).astype(np.float32)
    ang = np.concatenate([row[:, None] * inv, col[:, None] * inv], axis=-1).astype(np.float32)
    cs = np.repeat(np.cos(ang), 2, axis=1).T.astype(np.float32)
    sn = np.repeat(np.sin(ang), 2, axis=1).T.astype(np.float32)
    return np.ascontiguousarray(cs), np.ascontiguousarray(sn)


def _rmat():
    r = np.zeros((128, 128), np.float32)
    for i in range(64):
        r[2 * i + 1, 2 * i] = -1.0
        r[2 * i, 2 * i + 1] = 1.0
    return r


_NC_CACHE = {}


def _get_nc(name, builder):
    if name not in _NC_CACHE:
        _NC_CACHE[name] = builder()
    return _NC_CACHE[name]


def run_k1(l, xfm, m, p):
    cs, sn = _rope_tables()
    rm = _rmat()
    in_maps = []
    w_in = np.ascontiguousarray(p["w_in"][l])
    qkg = np.ascontiguousarray(np.stack([p["q_norm"][l], p["k_norm"][l]], axis=1))
    sgug = np.ascontiguousarray(np.broadcast_to(p["sgu_norm"][l][None, :], (128, 512)))
    for i in range(NCORES):
        b, q = i // 4, i % 4
        modv = np.stack([_fm(p["norm_mix"][l]), _fm(m[l, b, 2048:4096]), _fm(m[l, b, 0:2048]),
                         _fm(m[l, 2, 2048:4096]), _fm(m[l, 2, 0:2048])], axis=2)
        in_maps.append({"xT": xfm[i], "modv": np.ascontiguousarray(modv), "w_in": w_in, "qkg": qkg,
                        "ropeC": np.ascontiguousarray(cs[:, q * 1024:(q + 1) * 1024]),
                        "ropeS": np.ascontiguousarray(sn[:, q * 1024:(q + 1) * 1024]),
                        "sgug": sgug, "rmat": rm})
    return _run(build_k1(), in_maps)


TK = 4352
NT = 34
SCALE = 128 ** -0.5


def build_k2():
    C = Ctx()
    nc, P = C.nc, C.P
    d_qa = C.din("qa", [128, TK], BF16); d_ka = C.din("ka", [128, TK], BF16); d_va = C.din("va", [TK, 128], BF16)
    d_qb = C.din("qb", [128, TK], BF16); d_kb = C.din("kb", [128, TK], BF16); d_vb = C.din("vb", [TK, 128], BF16)
    d_nab = C.din("nab", [5, 128, 7, 128])
    d_cu = C.din("cu", [128, TK]); d_vn = C.din("vn", [TK, 128])
    d_wsT = C.din("wsT", [128, 128]); d_bsb = C.din("bsb", [128, 512])
    d_dx = C.din("dx", [128, TK]); d_db = C.din("db", [128, TK]); d_dc = C.din("dc", [128, TK]); d_dz = C.din("dz", [128, TK])
    d_ddt = C.din("ddt", [TK, 4])
    d_cw = C.din("cw", [128, 3, 5]); d_cb = C.din("cb", [128, 3])
    d_dtb = C.din("dtb", [128, NT * 4]); d_alog = C.din("alog", [128, NT * 4]); d_dsk = C.din("dsk", [128, 2])
    d_tri = C.din("tri", [2, 128, 128]); d_ident = C.din("ident", [128, 128])
    o_a = C.dout("oa", [128, TK]); o_b = C.dout("ob", [128, TK]); o_c = C.dout("oc", [128, TK]); o_d = C.dout("od", [128, TK])

    banks = C.psum_ring(8).items
    C.push()
    psS = Ring(banks[0:3]); psO = Ring(banks[3:5]); psD = Ring(banks[5:7]); psX = Ring(banks[7:8])
    ones_bf = C.sb([128, 128], BF16); Bones = Buf()
    P.add("pool", lambda e: e.memset(ones_bf[:], 1.0), [], [Bones])
    e_ring = C.ring(3, [128, 896], BF16)
    tmp = C.ring(4, [128, 896])
    stage = C.ring(3, [128, 512])

    def load_qkv(dq, dk, dv):
        q = C.sb([128, TK], BF16); k = C.sb([128, TK], BF16); v = C.sb([128, NT, 128], BF16)
        Bq, Bk, Bv = Buf(), Buf(), Buf()
        P.dma(q[:], dq, writes=[Bq]); P.dma(k[:], dk, writes=[Bk])
        P.dma(v[:], dv.rearrange("(t p) c -> p t c", p=128), writes=[Bv])
        return (q, Bq), (k, Bk), (v, Bv)

    def finish_block(pO, BpO, pD, BpD, nq, dst):
        rd, Brd = tmp.next()
        P.add("dve", lambda e: e.reciprocal(out=rd[:, :nq], in_=pD[:, :nq]), [BpD], [Brd])
        st, Bst = stage.next()
        P.add("dve", lambda e: e.tensor_tensor(out=st[:, :nq], in0=pO[:, :nq], in1=rd[:, :nq], op=ALU.mult),
              [BpO, Brd], [Bst])
        P.dma(dst, st[:, :nq], reads=[Bst])

    def attn_dense(Q, K, V, q0, nq, ktiles, dst):
        (q, Bq), (k, Bk), (v, Bv) = Q, K, V
        pO, BpO = psO.next(); pD, BpD = psD.next()
        n = len(ktiles)

        def S(kt):
            pS, BpS = psS.next()
            P.mm(pS[:, :nq], k[:, kt * 128:(kt + 1) * 128], q[:, q0:q0 + nq], True, True, reads=[Bk, Bq], writes=[BpS])
            return pS, BpS
        cur = S(ktiles[0])
        for ii, kt in enumerate(ktiles):
            pS, BpS = cur
            if ii + 1 < n:
                cur = S(ktiles[ii + 1])
            E, BE = e_ring.next()
            P.add("act", lambda e, E=E, pS=pS: e.activation(out=E[:, :nq], in_=pS[:, :nq], func=AF.Exp, scale=SCALE),
                  [BpS], [BE])
            P.mm(pO[:, :nq], v[:, kt, :], E[:, :nq], ii == 0, ii == n - 1, reads=[Bv, BE], writes=[BpO])
            P.mm(pD[:, :nq], ones_bf[:], E[:, :nq], ii == 0, ii == n - 1, reads=[Bones, BE], writes=[BpD])
        finish_block(pO, BpO, pD, BpD, nq, dst)

    QA, KA, VA = load_qkv(d_qa, d_ka, d_va)
    attn_dense(QA, KA, VA, 0, 256, [0, 1], o_a[:, 0:256])
    for qb_ in range(8):
        q0 = 256 + qb_ * 512
        attn_dense(QA, KA, VA, q0, 512, list(range(NT)), o_a[:, q0:q0 + 512])

    QB, KB, VB = load_qkv(d_qb, d_kb, d_vb)
    nab = C.sb([128, 5, 7 * 128]); Bnab = Buf()
    for t5 in range(5):
        P.dma(nab[:, t5, :], d_nab[t5].rearrange("p t q -> p (t q)"), writes=[Bnab])
    attn_dense(QB, KB, VB, 0, 256, [0, 1], o_b[:, 0:256])
    (qb, Bqb), (kb, Bkb), (vb, Bvb) = QB, KB, VB
    for m in range(32):
        rs = min(max(2 * m - 4, 0), 54)
        tab = {0: 0, 1: 1, 30: 3, 31: 4}.get(m, 2)
        q0 = 256 + m * 128
        ktl = [0, 1] + [2 + rs // 2 + t for t in range(5)]
        pA, BpA = psS.next(); pB, BpB = psS.next()
        for ii, kt in enumerate(ktl):
            pt, Bpt, off = (pA, BpA, ii * 128) if ii < 4 else (pB, BpB, (ii - 4) * 128)
            P.mm(pt[:, off:off + 128], kb[:, kt * 128:(kt + 1) * 128], qb[:, q0:q0 + 128], True, True,
                 reads=[Bkb, Bqb], writes=[Bpt])
        sc, Bsc = tmp.next()
        P.add("dve", lambda e, sc=sc, pA=pA, tab=tab: e.scalar_tensor_tensor(
            out=sc[:, 0:512], in0=pA[:, 0:512], scalar=SCALE, in1=nab[:, tab, 0:512], op0=ALU.mult, op1=ALU.add),
              [BpA, Bnab], [Bsc])
        P.add("dve", lambda e, sc=sc, pB=pB, tab=tab: e.scalar_tensor_tensor(
            out=sc[:, 512:896], in0=pB[:, 0:384], scalar=SCALE, in1=nab[:, tab, 512:896], op0=ALU.mult, op1=ALU.add),
              [BpB, Bnab], [Bsc])
        E, BE = e_ring.next()
        P.add("act", lambda e, E=E, sc=sc: e.activation(out=E[:, :], in_=sc[:, :], func=AF.Exp), [Bsc], [BE])
        pO, BpO = psO.next(); pD, BpD = psD.next()
        for ii, kt in enumerate(ktl):
            P.mm(pO[:, :128], vb[:, kt, :], E[:, ii * 128:(ii + 1) * 128], ii == 0, ii == 6, reads=[Bvb, BE], writes=[BpO])
            P.mm(pD[:, :128], ones_bf[:], E[:, ii * 128:(ii + 1) * 128], ii == 0, ii == 6, reads=[Bones, BE], writes=[BpD])
        finish_block(pO, BpO, pD, BpD, 128, o_b[:, q0:q0 + 128])

    cu = C.sb([128, TK]); Bcu = Buf()
    vn = C.sb([128, NT, 128]); Bvn = Buf()
    wsT = C.sb([128, 128]); bsb = C.sb([128, 512]); Bws = Buf()
    P.dma(cu[:], d_cu, writes=[Bcu])
    P.dma(vn[:], d_vn.rearrange("(t p) c -> p t c", p=128), writes=[Bvn])
    P.dma(wsT[:], d_wsT, writes=[Bws]); P.dma(bsb[:], d_bsb, writes=[Bws])
    for g0 in range(0, NT, 4):
        ng = min(4, NT - g0)
        pt, Bpt = psX.next()
        for t in range(ng):
            P.mm(pt[:, t * 128:(t + 1) * 128], vn[:, g0 + t, :], wsT[:], True, True, reads=[Bvn, Bws], writes=[Bpt])
        w = ng * 128
        t1, Bt1 = tmp.next()
        P.add("dve", lambda e, t1=t1, pt=pt, w=w: e.tensor_tensor(out=t1[:, :w], in0=pt[:, :w], in1=bsb[:, :w], op=ALU.add),
              [Bpt, Bws], [Bt1])
        st, Bst = stage.next()
        P.add("pool", lambda e, st=st, t1=t1, w=w, g0=g0: e.tensor_tensor(
            out=st[:, :w], in0=t1[:, :w], in1=cu[:, g0 * 128:g0 * 128 + w], op=ALU.mult), [Bt1, Bcu], [Bst])
        P.dma(o_c[:, g0 * 128:g0 * 128 + w], st[:, :w], reads=[Bst])
    C.pop()

    C.push()
    psR = Ring(banks[0:8])
    raw = [C.sb([128, TK]) for _ in range(3)]; Braw = [Buf() for _ in range(3)]
    cv = [C.sb([128, TK]) for _ in range(3)]; Bcv = [Buf() for _ in range(3)]
    for i3, dd in enumerate((d_dx, d_db, d_dc)):
        P.dma(raw[i3][:], dd, writes=[Braw[i3]])
    dz = C.sb([128, TK]); Bdz = Buf()
    P.dma(dz[:], d_dz, writes=[Bdz])
    cw = C.sb([128, 3, 5]); cb = C.sb([128, 3]); Bcw = Buf()
    P.dma(cw[:], d_cw, writes=[Bcw]); P.dma(cb[:], d_cb, writes=[Bcw])
    ddt = C.sb([128, NT, 4]); Bddt = Buf()
    P.dma(ddt[:], d_ddt.rearrange("(t p) c -> p t c", p=128), writes=[Bddt])
    dtb = C.sb([128, NT * 4]); alog = C.sb([128, NT * 4]); dsk = C.sb([128, 2]); Bsm = Buf()
    P.dma(dtb[:], d_dtb, writes=[Bsm]); P.dma(alog[:], d_alog, writes=[Bsm]); P.dma(dsk[:], d_dsk, writes=[Bsm])
    tri = C.sb([128, 2, 128]); ident = C.sb([128, 128]); Btri = Buf()
    P.dma(tri[:], d_tri.rearrange("d p i -> p d i"), writes=[Btri]); P.dma(ident[:], d_ident, writes=[Btri])
    ones_f = C.sb([128, 128]); Bof = Buf()
    P.add("pool", lambda e: e.memset(ones_f[:], 1.0), [], [Bof])

    for i3 in range(3):
        for (s0, L) in ((0, 256), (256, 4096)):
            P.add("dve", lambda e, i3=i3, s0=s0, L=L: e.tensor_scalar(
                out=cv[i3][:, s0:s0 + L], in0=raw[i3][:, s0:s0 + L], scalar1=cw[:, i3, 2:3], scalar2=None, op0=ALU.mult),
                  [Braw[i3], Bcw], [Bcv[i3]])
            for kk in (0, 1, 3, 4):
                d = kk - 2
                lo, hi = max(0, -d), min(L, L - d)
                P.add("dve", lambda e, i3=i3, s0=s0, lo=lo, hi=hi, d=d, kk=kk: e.scalar_tensor_tensor(
                    out=cv[i3][:, s0 + lo:s0 + hi], in0=raw[i3][:, s0 + lo + d:s0 + hi + d], scalar=cw[:, i3, kk:kk + 1],
                    in1=cv[i3][:, s0 + lo:s0 + hi], op0=ALU.mult, op1=ALU.add), [Braw[i3], Bcw, Bcv[i3]], [Bcv[i3]])
        P.add("act", lambda e, i3=i3: e.activation(out=cv[i3][:, :], in_=cv[i3][:, :], func=AF.Silu, bias=cb[:, i3:i3 + 1]),
              [Bcv[i3], Bcw], [Bcv[i3]])
    xc, Bc_, Cc = cv
    Bxc, BBc, BCc = Bcv
    xtm = raw[0][:].rearrange("p (t c) -> p t c", c=128)
    btm = raw[1][:].rearrange("p (t c) -> p t c", c=128)
    Bxtm, Bbtm = Braw[0], Braw[1]
    for (src, Bsrc, dst, Bdst) in ((xc, Bxc, xtm, Bxtm), (Bc_, BBc, btm, Bbtm)):
        for g0 in range(0, NT, 4):
            ng = min(4, NT - g0)
            pt, Bpt = psR.next()
            for t in range(ng):
                P.add("pe", lambda e, pt=pt, t=t, src=src, g0=g0: e.transpose(
                    pt[:, t * 128:(t + 1) * 128], src[:, (g0 + t) * 128:(g0 + t + 1) * 128], ident[:]),
                      [Bsrc, Btri], [Bpt])
            P.add("dve", lambda e, pt=pt, dst=dst, g0=g0, ng=ng: e.tensor_copy(
                out=dst[:, g0:g0 + ng, :], in_=pt[:, :ng * 128].rearrange("p (t c) -> p t c", c=128)), [Bpt], [Bdst])
    sm = [C.sb([128, NT * 4]) for _ in range(5)]; Bs_ = [Buf() for _ in range(5)]
    ddf = ddt[:].rearrange("p t c -> p (t c)")
    P.add("dve", lambda e: e.tensor_tensor(out=sm[0][:], in0=ddf, in1=dtb[:], op=ALU.add), [Bddt, Bsm], [Bs_[0]])
    P.add("act", lambda e: e.activation(out=sm[1][:], in_=sm[0][:], func=AF.Abs), [Bs_[0]], [Bs_[1]])
    P.add("act", lambda e: e.activation(out=sm[1][:], in_=sm[1][:], func=AF.Exp, scale=-1.0), [Bs_[1]], [Bs_[1]])
    P.add("act", lambda e: e.activation(out=sm[1][:], in_=sm[1][:], func=AF.Ln, bias=1.0), [Bs_[1]], [Bs_[1]])
    P.add("dve", lambda e: e.tensor_scalar(out=sm[2][:], in0=sm[0][:], scalar1=0.0, scalar2=None, op0=ALU.max), [Bs_[0]], [Bs_[2]])
    dt_t, Bdt = sm[3], Bs_[3]
    P.add("dve", lambda e: e.tensor_tensor(out=dt_t[:], in0=sm[2][:], in1=sm[1][:], op=ALU.add), [Bs_[1], Bs_[2]], [Bdt])
    P.add("act", lambda e: e.activation(out=sm[0][:], in_=alog[:], func=AF.Exp), [Bsm, Bs_[0]], [Bs_[0]])
    a_t, Ba = sm[4], Bs_[4]
    P.add("dve", lambda e: e.scalar_tensor_tensor(out=a_t[:], in0=sm[0][:], scalar=-1.0, in1=dt_t[:], op0=ALU.mult, op1=ALU.mult),
          [Bs_[0], Bdt], [Ba])
    dt3 = dt_t[:].rearrange("p (t c) -> p t c", c=4)
    a3 = a_t[:].rearrange("p (t c) -> p t c", c=4)

    Y = C.sb([128, TK]); BY = Buf()
    hp = [C.sb([128, 128]) for _ in range(2)]; Bhp = [Buf(), Buf()]
    t128 = C.ring(12, [128, 128])
    t64 = C.ring(6, [128, 128])
    tsm = C.ring(8, [128, 4])
    for d in range(2):
        order = [0, 1] + list(range(2, NT)) if d == 0 else [1, 0] + list(range(NT - 1, 1, -1))
        for r in range(2):
            P.add("pool", lambda e, r=r: e.memset(hp[r][:], 0.0), [], [Bhp[r]])
        last = 127 if d == 0 else 0
        for c in order:
            cs = slice(c * 128, (c + 1) * 128)
            acol = a3[:, c, 2 * d:2 * d + 2]
            pc, Bpc = psR.next()
            P.mm(pc[:, 0:2], tri[:, d, :], acol, True, True, reads=[Btri, Ba], writes=[Bpc])
            cc, Bcc = tsm.next()
            P.add("dve", lambda e, cc=cc, pc=pc: e.tensor_copy(out=cc[:, 0:2], in_=pc[:, 0:2]), [Bpc], [Bcc])
            pr, Bpr = psR.next()
            for r in range(2):
                abc, Babc = t128.next()
                P.add("dve", lambda e, abc=abc, c=c, r=r, d=d: e.tensor_scalar(
                    out=abc[:], in0=ones_f[:], scalar1=a3[:, c, 2 * d + r:2 * d + r + 1], scalar2=None, op0=ALU.mult),
                      [Bof, Ba], [Babc])
                P.mm(pr[:, r * 128:(r + 1) * 128], abc[:], tri[:, d, :], True, True, reads=[Babc, Btri], writes=[Bpr])
            pss, Bpss = psR.next()
            P.mm(pss[:, 0:128], Bc_[:, cs], Cc[:, cs], True, True, reads=[BBc, BCc], writes=[Bpss])
            xw, Bxw = t64.next()
            py, Bpy = psR.next()
            tot, Btot = tsm.next()
            P.add("dve", lambda e, tot=tot, pr=pr, last=last: e.tensor_copy(
                out=tot[:, 0:2], in_=pr[:, 0:256].rearrange("p (r i) -> p r i", i=128)[:, :, last]), [Bpr], [Btot])
            dec, Bdec = tsm.next()
            for r in range(2):
                P.add("act", lambda e, dec=dec, cc=cc, tot=tot, r=r: e.activation(
                    out=dec[:, r:r + 1], in_=cc[:, r:r + 1], func=AF.Exp, scale=-1.0, bias=tot[:, r:r + 1]),
                      [Bcc, Btot], [Bdec])
            P.add("act", lambda e, dec=dec, tot=tot: e.activation(out=dec[:, 2:4], in_=tot[:, 0:2], func=AF.Exp),
                  [Btot, Bdec], [Bdec])
            first = True
            for r in range(2):
                hs = slice(r * 64, (r + 1) * 64)
                arg, Barg = t128.next()
                P.add("dve", lambda e, arg=arg, pr=pr, cc=cc, r=r: e.tensor_scalar(
                    out=arg[:], in0=pr[:, r * 128:(r + 1) * 128], scalar1=cc[:, r:r + 1], scalar2=0.0,
                    op0=ALU.subtract, op1=ALU.min), [Bpr, Bcc], [Barg])
                P.add("act", lambda e, arg=arg: e.activation(out=arg[:], in_=arg[:], func=AF.Exp), [Barg], [Barg])
                P.add("pool", lambda e, arg=arg, d=d: e.tensor_tensor(out=arg[:], in0=arg[:], in1=tri[:, d, :], op=ALU.mult),
                      [Barg, Btri], [Barg])
                mt, Bmt = t128.next()
                P.add("dve", lambda e, mt=mt, pss=pss, arg=arg: e.tensor_tensor(
                    out=mt[:], in0=pss[:, 0:128], in1=arg[:], op=ALU.mult), [Bpss, Barg], [Bmt])
                xd, Bxd = t64.next()
                P.add("pool", lambda e, xd=xd: e.memset(xd[:], 0.0), [], [Bxd])
                P.add("dve", lambda e, xd=xd, c=c, r=r, hs=hs, d=d: e.tensor_scalar(
                    out=xd[:, hs], in0=xtm[:, c, hs], scalar1=dt3[:, c, 2 * d + r:2 * d + r + 1], scalar2=None, op0=ALU.mult),
                      [Bxtm, Bdt, Bxd], [Bxd])
                P.add("dve", lambda e, xw=xw, xd=xd, dec=dec, r=r, hs=hs: e.tensor_scalar(
                    out=xw[:, hs], in0=xd[:, hs], scalar1=dec[:, r:r + 1], scalar2=None, op0=ALU.mult), [Bxd, Bdec], [Bxw])
                ecr, Becr = t128.next()
                P.add("act", lambda e, ecr=ecr, pr=pr, r=r: e.activation(
                    out=ecr[:], in_=pr[:, r * 128:(r + 1) * 128], func=AF.Exp), [Bpr], [Becr])
                P.add("pool", lambda e, ecr=ecr, cs=cs: e.tensor_tensor(out=ecr[:], in0=ecr[:], in1=Cc[:, cs], op=ALU.mult),
                      [Becr, BCc], [Becr])
                P.mm(py[:, 0:128], xd[:], mt[:], first, False, reads=[Bxd, Bmt], writes=[Bpy])
                first = False
                P.mm(py[:, 0:128], hp[r][:], ecr[:], False, r == 1, reads=[Bhp[r], Becr], writes=[Bpy])
            if d == 0:
                P.add("dve", lambda e, py=py, cs=cs: e.tensor_copy(out=Y[:, cs], in_=py[:, 0:128]), [Bpy], [BY])
            else:
                P.add("dve", lambda e, py=py, cs=cs: e.tensor_tensor(out=Y[:, cs], in0=py[:, 0:128], in1=Y[:, cs], op=ALU.add),
                      [Bpy, BY], [BY])
            pst, Bpst = psR.next()
            P.mm(pst[:, 0:128], btm[:, c, :], xw[:], True, True, reads=[Bbtm, Bxw], writes=[Bpst])
            for r in range(2):
                hs = slice(r * 64, (r + 1) * 64)
                P.add("dve", lambda e, r=r, hs=hs, dec=dec, pst=pst: e.scalar_tensor_tensor(
                    out=hp[r][:, hs], in0=hp[r][:, hs], scalar=dec[:, 2 + r:3 + r], in1=pst[:, hs], op0=ALU.mult, op1=ALU.add),
                      [Bhp[r], Bdec, Bpst], [Bhp[r]])
    dsum = C.sb([128, 1]); Bds = Buf()
    P.add("dve", lambda e: e.tensor_tensor(out=dsum[:], in0=dsk[:, 0:1], in1=dsk[:, 1:2], op=ALU.add), [Bsm], [Bds])
    for c0 in range(0, TK, 1088):
        sl = slice(c0, c0 + 1088)
        P.add("dve", lambda e, sl=sl: e.scalar_tensor_tensor(out=Y[:, sl], in0=xc[:, sl], scalar=dsum[:, 0:1], in1=Y[:, sl],
                                                              op0=ALU.mult, op1=ALU.add), [Bxc, Bds, BY], [BY])
        P.add("pool", lambda e, sl=sl: e.tensor_tensor(out=Y[:, sl], in0=Y[:, sl], in1=dz[:, sl], op=ALU.mult), [BY, Bdz], [BY])
        P.dma(o_d[:, sl], Y[:, sl], reads=[BY])
    C.pop()
    return C.done()


def _na_tables(rpb_l):
    out = np.zeros((4, 5, 7, 128, 128), np.float32)
    specs = [(0, 0), (2, 0), (8, 4), (60, 54), (62, 54)]
    kk = np.arange(640)
    qq = np.arange(128)
    for ti, (r0, rs) in enumerate(specs):
        krow = rs + kk // 64
        kcol = kk % 64
        qrow = r0 + qq // 64
        qcol = qq % 64
        kr0 = np.clip(qrow - 4, 0, 56)
        wc0 = np.clip(qcol - 8, 0, 48)
        ok = ((krow[:, None] >= kr0[None, :]) & (krow[:, None] < kr0[None, :] + 8) &
              (kcol[:, None] >= wc0[None, :]) & (kcol[:, None] < wc0[None, :] + 16))
        drow = np.clip(krow[:, None] - qrow[None, :] + 7, 0, 14)
        dcol = np.clip(kcol[:, None] - qcol[None, :], -15, 15) + 15
        for h in range(4):
            g = rpb_l[h][drow, dcol]
            g = np.where(ok, g, np.float32(-30000.0))
            out[h, ti, 2:7] = g.reshape(5, 128, 128)
    return np.ascontiguousarray(out.transpose(0, 1, 3, 2, 4))


def _tri_consts():
    j = np.arange(128)[:, None]
    i = np.arange(128)[None, :]
    return np.ascontiguousarray(np.stack([(j <= i), (j >= i)]).astype(np.float32))


def _gather_tok(res, key, b, fm):
    parts = [np.asarray(res[4 * b + q][key]) for q in range(4)]
    if fm:
        return np.concatenate([p_[:, :, 1024:] for p_ in parts] + [p_[:, :, :1024] for p_ in parts], axis=2)
    return np.concatenate([p_[1024:] for p_ in parts] + [p_[:1024] for p_ in parts], axis=0)


def run_k2(l, r1, p):
    nab = _na_tables(p["rpb"][l])
    tri = _tri_consts()
    ident = np.eye(128, dtype=np.float32)
    in_maps = []
    g_ = {}
    for b in range(2):
        for key, fm in (("qkA", 1), ("bqk", 1), ("cu", 1), ("dz", 1), ("dcxb", 1), ("av", 0), ("bv", 0), ("vn", 0), ("ddt", 0)):
            g_[(b, key)] = _gather_tok(r1, key, b, fm)
    for i in range(NCORES):
        b, hh = i // 4, i % 4
        g = hh // 2
        ch = slice(hh * 128, (hh + 1) * 128)
        dcxb = g_[(b, "dcxb")]
        cwf = p["conv_w"][l]
        cbf = p["conv_b"][l]
        xs = slice(hh * 128, (hh + 1) * 128)
        bs = slice(512 + g * 128, 512 + (g + 1) * 128)
        cs_ = slice(768 + g * 128, 768 + (g + 1) * 128)
        cw = np.stack([cwf[:, xs].T, cwf[:, bs].T, cwf[:, cs_].T], axis=1)
        cb = np.stack([cbf[xs], cbf[bs], cbf[cs_]], axis=1)
        hsel = [2 * hh, 2 * hh + 1, 8 + 2 * hh, 8 + 2 * hh + 1]
        dtb4 = p["dt_bias"][l].reshape(16)[hsel]
        al4 = p["a_log"][l].reshape(16)[hsel]
        heads_p = np.repeat([2 * hh, 2 * hh + 1], 64)
        dsk = np.stack([p["d_skip"][l][0][heads_p], p["d_skip"][l][1][heads_p]], axis=1)
        in_maps.append({
            "qa": np.ascontiguousarray(g_[(b, "qkA")][hh]), "ka": np.ascontiguousarray(g_[(b, "qkA")][4 + hh // 2]),
            "va": np.ascontiguousarray(g_[(b, "av")][:, (hh // 2) * 128:(hh // 2 + 1) * 128]),
            "qb": np.ascontiguousarray(g_[(b, "bqk")][hh]), "kb": np.ascontiguousarray(g_[(b, "bqk")][4 + hh]),
            "vb": np.ascontiguousarray(g_[(b, "bv")][:, ch]),
            "nab": nab[hh],
            "cu": np.ascontiguousarray(g_[(b, "cu")][hh]), "vn": np.ascontiguousarray(g_[(b, "vn")][:, ch]),
            "wsT": np.ascontiguousarray(p["sgu_w"][l][hh].T),
            "bsb": np.ascontiguousarray(np.broadcast_to(np.tile(p["sgu_b"][l][hh], 4)[None, :], (128, 512))),
            "dx": np.ascontiguousarray(dcxb[2 + hh]), "db": np.ascontiguousarray(dcxb[6 + g]),
            "dc": np.ascontiguousarray(dcxb[g]), "dz": np.ascontiguousarray(g_[(b, "dz")][hh]),
            "ddt": np.ascontiguousarray(g_[(b, "ddt")][:, hsel]),
            "cw": np.ascontiguousarray(cw.astype(np.float32)), "cb": np.ascontiguousarray(cb.astype(np.float32)),
            "dtb": np.ascontiguousarray(np.broadcast_to(np.tile(dtb4, NT)[None, :], (128, NT * 4))),
            "alog": np.ascontiguousarray(np.broadcast_to(np.tile(al4, NT)[None, :], (128, NT * 4))),
            "dsk": np.ascontiguousarray(dsk.astype(np.float32)),
            "tri": tri, "ident": ident,
        })
    return _run(build_k2(), in_maps)


D_FF = 5632
D_FFE = 7168


def build_k3(moe):
    C = Ctx()
    nc, P = C.nc, C.P
    T = 1024 if moe else T1
    blks = TBLK[:2] if moe else TBLK
    nsp = 4 if moe else 2
    xT = C.din("xT", [128, 16, T])
    mixT = C.din("mixT", [128, 16, T])
    modv = C.din("modv", [128, 16, 11])
    w_out = C.din("w_out", [2048, 2048])
    if moe:
        router = C.din("router", [128, 16, 8]); d_ident = C.din("ident", [128, 128])
        d_sel = C.din("sel", [8, 8, 128])
        nff = D_FFE // 128
        xo = C.dout("xo", [128, 16, 1024])
        go = C.dout("gates", [1024, 8])
        h2o = C.dout("h2o", [128, 16, 1024], BF16)
    else:
        w13 = C.din("w13", [2048, 2 * D_FF]); w2 = C.din("w2", [D_FF, 2048])
        nff = D_FF // 128
        xo = C.dout("xo", [128, 16, T])
    nh = nff // nsp

    banks = C.psum_ring(8).items
    ps = Ring(banks)
    x_sb = C.sb([128, 16, T]); Bx = Buf("x")
    h_sb = C.sb([128, 16, T], BF16); Bh = Buf("h")
    mod_sb = C.sb([128, 16, 11]); Bmod = Buf()
    gs_sb = C.sb([128, 16, 2]); Bgs = Buf()
    ones_d = C.sb([128, 128]); ones_g = C.sb([128, 128]); Bones = Buf()
    tmp = C.ring(6, [128, 512])
    rs_ring = C.ring(1, [128, 512])
    sq_ring = C.ring(1, [128, 4, 512])
    for q4 in range(4):
        P.dma(x_sb[:, 4 * q4:4 * q4 + 4, :], xT[:, 4 * q4:4 * q4 + 4, :], writes=[Bx])
    P.dma(mod_sb[:], modv, writes=[Bmod])
    P.add("pool", lambda e: e.memset(ones_d[:], 1.0 / 2048.0), [], [Bones])
    P.add("pool", lambda e: e.memset(ones_g[:], 1.0 / 512.0), [], [Bones])
    for j, col in enumerate((4, 6)):
        P.add("dve", lambda e, j=j, col=col: e.scalar_tensor_tensor(
            out=gs_sb[:, :, j], in0=mod_sb[:, :, col], scalar=1.0, in1=mod_sb[:, :, 3],
            op0=ALU.add, op1=ALU.mult), [Bmod], [Bgs])

    def rstd_from(pt, Bp, N):
        rstd, Br = rs_ring.next()
        P.add("act", lambda e: e.activation(out=rstd[:, :N], in_=pt[:, :N], func=AF.Ln, bias=EPS), [Bp], [Br])
        P.add("act", lambda e: e.activation(out=rstd[:, :N], in_=rstd[:, :N], func=AF.Exp, scale=-0.5), [Br], [Br])
        return rstd, Br

    C.push()
    mix_ring = C.ring(2, [128, 4, T])
    w_ring = C.ring(2, [128, 16, 256], BF16)
    for mg in range(4):
        mix, Bmix = mix_ring.next()
        P.dma(mix[:], mixT[:, 4 * mg:4 * mg + 4, :], writes=[Bmix])
        for bi, (n0, N) in enumerate(blks):
            sq, Bsq = sq_ring.next()
            P.add("act", lambda e, sq=sq, n0=n0, N=N, mix=mix: e.activation(
                out=sq[:, :, :N], in_=mix[:, :, n0:n0 + N], func=AF.Square), [Bmix], [Bsq])
            pt, Bp = ps.next()
            for kk in range(4):
                P.mm(pt[:, :N], ones_g[:], sq[:, kk, :N], kk == 0, kk == 3, reads=[Bones, Bsq], writes=[Bp])
            rstd, Br = rstd_from(pt, Bp, N)
            for kk in range(4):
                k = 4 * mg + kk
                P.add("dve", lambda e, k=k, kk=kk, n0=n0, N=N, rstd=rstd, mix=mix: e.scalar_tensor_tensor(
                    out=h_sb[:, k, n0:n0 + N], in0=mix[:, kk, n0:n0 + N], scalar=mod_sb[:, k, 0:1], in1=rstd[:, :N],
                    op0=ALU.mult, op1=ALU.mult), [Bmix, Bmod, Br], [Bh])
    wt_next = None

    def load_wo(jj):
        wt, Bw = w_ring.next()
        P.dma(wt[:], w_out[:, jj * 256:(jj + 1) * 256].rearrange("(k p) c -> p k c", p=128), writes=[Bw], q="pool")
        return wt, Bw
    wt_next = load_wo(0)
    for jj in range(8):
        wt, Bw = wt_next
        if jj + 1 < 8:
            wt_next = load_wo(jj + 1)
        for j2 in range(2):
            j = 2 * jj + j2
            for bi, (n0, N) in enumerate(blks):
                seg = 0 if bi < 2 else 1
                pt, Bp = ps.next()
                for k in range(16):
                    P.mm(pt[:, :N], wt[:, k, j2 * 128:(j2 + 1) * 128], h_sb[:, k, n0:n0 + N], k == 0, k == 15,
                         reads=[Bw, Bh], writes=[Bp])
                P.add("dve", lambda e, pt=pt, j=j, n0=n0, N=N, seg=seg: e.scalar_tensor_tensor(
                    out=x_sb[:, j, n0:n0 + N], in0=pt[:, :N], scalar=mod_sb[:, j, 1 + seg:2 + seg], in1=x_sb[:, j, n0:n0 + N],
                    op0=ALU.mult, op1=ALU.add), [Bp, Bmod, Bx], [Bx])
    C.pop()

    C.push()
    if moe:
        rt_sb = C.sb([128, 16, 8]); ident = C.sb([128, 128]); sel = C.sb([8, 8, 128]); Brt = Buf()
        P.dma(rt_sb[:], router, writes=[Brt]); P.dma(ident[:], d_ident, writes=[Brt]); P.dma(sel[:], d_sel, writes=[Brt])
        lg = C.sb([8, 1024]); Blg = Buf()
        gate_tm = C.sb([128, 8, 8]); Bgtm = Buf()
        hf_ring = C.ring(2, [128, 512])
        sm8 = C.ring(8, [128, 8])
        sm1 = C.ring(8, [128, 1])
    for bi, (n0, N) in enumerate(blks):
        seg = 0 if bi < 2 else 1
        pt, Bp = ps.next()
        for k4 in range(4):
            sq, Bsq = sq_ring.next()
            P.add("act", lambda e, sq=sq, n0=n0, N=N, k4=k4: e.activation(
                out=sq[:, :, :N], in_=x_sb[:, 4 * k4:4 * k4 + 4, n0:n0 + N], func=AF.Square), [Bx], [Bsq])
            for kk in range(4):
                k = 4 * k4 + kk
                P.mm(pt[:, :N], ones_d[:], sq[:, kk, :N], k == 0, k == 15, reads=[Bones, Bsq], writes=[Bp])
        rstd, Br = rstd_from(pt, Bp, N)
        if moe:
            pl, Bpl = ps.next()
        for k in range(16):
            t, Bt = tmp.next()
            P.add("dve", lambda e, t=t, k=k, n0=n0, N=N, seg=seg, rstd=rstd: e.scalar_tensor_tensor(
                out=t[:, :N], in0=x_sb[:, k, n0:n0 + N], scalar=gs_sb[:, k, seg:seg + 1], in1=rstd[:, :N],
                op0=ALU.mult, op1=ALU.mult), [Bx, Bgs, Br], [Bt])
            P.add("act", lambda e, t=t, k=k, n0=n0, N=N, seg=seg: e.activation(
                out=h_sb[:, k, n0:n0 + N], in_=t[:, :N], func=AF.Identity,
                bias=mod_sb[:, k, 5 + 2 * seg:6 + 2 * seg], scale=1.0), [Bt, Bmod], [Bh])
            if moe:
                hf, Bhf = hf_ring.next()
                P.add("pool", lambda e, hf=hf, t=t, k=k, N=N: e.tensor_scalar(
                    out=hf[:, :N], in0=t[:, :N], scalar1=mod_sb[:, k, 5:6], scalar2=None, op0=ALU.add), [Bt, Bmod], [Bhf])
                P.mm(pl[0:8, :N], rt_sb[:, k, :], hf[:, :N], k == 0, k == 15, reads=[Brt, Bhf], writes=[Bpl])
        if moe:
            P.add("dve", lambda e, pl=pl, n0=n0, N=N: e.tensor_copy(out=lg[:, n0:n0 + N], in_=pl[0:8, :N]), [Bpl], [Blg])
    if moe:
        for ti in range(8):
            pt, Bp = ps.next()
            P.add("pe", lambda e, pt=pt, ti=ti: e.transpose(pt[:, 0:8], lg[0:8, ti * 128:(ti + 1) * 128], ident[0:8, 0:8]),
                  [Blg, Brt], [Bp])
            l_, Bl = sm8.next(); m1, Bm1 = sm1.next(); e1, Be1 = sm8.next(); l2, Bl2 = sm8.next(); m2, Bm2 = sm1.next()
            P.add("dve", lambda e, l_=l_, pt=pt: e.tensor_copy(out=l_[:], in_=pt[:, 0:8]), [Bp], [Bl])
            P.add("dve", lambda e, l_=l_, m1=m1: e.tensor_reduce(out=m1[:], in_=l_[:], axis=AX.X, op=ALU.max), [Bl], [Bm1])
            P.add("dve", lambda e, l_=l_, m1=m1, e1=e1: e.tensor_scalar(
                out=e1[:], in0=l_[:], scalar1=m1[:, 0:1], scalar2=None, op0=ALU.is_equal), [Bl, Bm1], [Be1])
            P.add("dve", lambda e, l_=l_, e1=e1, l2=l2: e.scalar_tensor_tensor(
                out=l2[:], in0=e1[:], scalar=-1e30, in1=l_[:], op0=ALU.mult, op1=ALU.add), [Bl, Be1], [Bl2])
            P.add("dve", lambda e, l2=l2, m2=m2: e.tensor_reduce(out=m2[:], in_=l2[:], axis=AX.X, op=ALU.max), [Bl2], [Bm2])
            s2, Bs2 = sm8.next(); ex, Bex = sm8.next(); nm1, Bnm1 = sm1.next(); ssum, Bss = sm1.next()
            P.add("dve", lambda e, l_=l_, m2=m2, s2=s2: e.tensor_scalar(
                out=s2[:], in0=l_[:], scalar1=m2[:, 0:1], scalar2=None, op0=ALU.is_ge), [Bl, Bm2], [Bs2])
            P.add("dve", lambda e, m1=m1, nm1=nm1: e.tensor_scalar(
                out=nm1[:], in0=m1[:], scalar1=-1.0, scalar2=None, op0=ALU.mult), [Bm1], [Bnm1])
            P.add("act", lambda e, l_=l_, ex=ex, nm1=nm1: e.activation(out=ex[:], in_=l_[:], func=AF.Exp, bias=nm1[:, 0:1]),
                  [Bl, Bnm1], [Bex])
            P.add("dve", lambda e, ex=ex, s2=s2: e.tensor_tensor(out=ex[:], in0=ex[:], in1=s2[:], op=ALU.mult), [Bex, Bs2], [Bex])
            P.add("dve", lambda e, ex=ex, ssum=ssum: e.tensor_reduce(out=ssum[:], in_=ex[:], axis=AX.X, op=ALU.add), [Bex], [Bss])
            P.add("dve", lambda e, ssum=ssum: e.reciprocal(out=ssum[:], in_=ssum[:]), [Bss], [Bss])
            P.add("dve", lambda e, ex=ex, ssum=ssum, ti=ti: e.tensor_scalar(
                out=gate_tm[:, ti, :], in0=ex[:], scalar1=ssum[:, 0:1], scalar2=None, op0=ALU.mult), [Bex, Bss], [Bgtm])
            p2, Bp2 = ps.next()
            P.add("pe", lambda e, p2=p2, ti=ti: e.transpose(p2[0:8, 0:128], gate_tm[:, ti, :], ident[:]), [Bgtm, Brt], [Bp2])
            P.add("dve", lambda e, p2=p2, ti=ti: e.tensor_copy(out=lg[:, ti * 128:(ti + 1) * 128], in_=p2[0:8, 0:128]),
                  [Bp2, Blg], [Blg])
        P.dma(go.rearrange("(t p) e -> p t e", p=128), gate_tm[:], reads=[Bgtm])
        for q4 in range(4):
            P.dma(xo[:, 4 * q4:4 * q4 + 4, :], x_sb[:, 4 * q4:4 * q4 + 4, :], reads=[Bx])
            P.dma(h2o[:, 4 * q4:4 * q4 + 4, :], h_sb[:, 4 * q4:4 * q4 + 4, :], reads=[Bh])
        C.pop()
        return C.done()

    act = C.sb([128, nh, T], BF16); Bact = Buf()
    w13_ring = C.ring(2, [128, 16, 256], BF16)
    w2_ring = C.ring(2, [128, nh, 128], BF16)
    gbc_ring = C.ring(1, [128, 1024]) if moe else None
    ffw = D_FFE if moe else D_FF
    for ex_i in range(8 if moe else 1):
        w13e = w13[ex_i] if moe else w13
        w2e = w2[ex_i] if moe else w2
        if moe:
            gbc, Bgbc = gbc_ring.next()
            for bi, (n0, N) in enumerate(blks):
                pt, Bp = ps.next()
                P.mm(pt[:, :N], sel[:, ex_i, :], lg[:, n0:n0 + N], True, True, reads=[Brt, Blg], writes=[Bp])
                P.add("dve", lambda e, gbc=gbc, pt=pt, n0=n0, N=N: e.tensor_copy(out=gbc[:, n0:n0 + N], in_=pt[:, :N]),
                      [Bp], [Bgbc])
        for half in range(nsp):
            def load13(f):
                wt, Bw = w13_ring.next()
                c0 = (half * nh + f) * 128
                P.dma(wt[:, :, 0:128], w13e[:, c0:c0 + 128].rearrange("(k p) c -> p k c", p=128), writes=[Bw], q="pool")
                P.dma(wt[:, :, 128:256], w13e[:, ffw + c0:ffw + c0 + 128].rearrange("(k p) c -> p k c", p=128),
                      writes=[Bw], q="pool")
                return wt, Bw
            nxt = load13(0)
            for f in range(nh):
                wt, Bw = nxt
                if f + 1 < nh:
                    nxt = load13(f + 1)
                for bi, (n0, N) in enumerate(blks):
                    pg, Bpg = ps.next(); pu, Bpu = ps.next()
                    for k in range(16):
                        P.mm(pg[:, :N], wt[:, k, 0:128], h_sb[:, k, n0:n0 + N], k == 0, k == 15, reads=[Bw, Bh], writes=[Bpg])
                    for k in range(16):
                        P.mm(pu[:, :N], wt[:, k, 128:256], h_sb[:, k, n0:n0 + N], k == 0, k == 15, reads=[Bw, Bh], writes=[Bpu])
                    t, Bt = tmp.next()
                    P.add("act", lambda e, t=t, pg=pg, N=N: e.activation(out=t[:, :N], in_=pg[:, :N], func=AF.Silu), [Bpg], [Bt])
                    P.add("dve", lambda e, t=t, pu=pu, f=f, n0=n0, N=N: e.tensor_tensor(
                        out=act[:, f, n0:n0 + N], in0=pu[:, :N], in1=t[:, :N], op=ALU.mult), [Bpu, Bt], [Bact])

            def load2(j):
                wt, Bw = w2_ring.next()
                r0 = half * nh * 128
                P.dma(wt[:], w2e[r0:r0 + nh * 128, j * 128:(j + 1) * 128].rearrange("(f p) c -> p f c", p=128),
                      writes=[Bw], q="pool")
                return wt, Bw
            nxt = load2(0)
            for j in range(16):
                wt, Bw = nxt
                if j + 1 < 16:
                    nxt = load2(j + 1)
                for bi, (n0, N) in enumerate(blks):
                    seg = 0 if bi < 2 else 1
                    pt, Bp = ps.next()
                    for f in range(nh):
                        P.mm(pt[:, :N], wt[:, f, :], act[:, f, n0:n0 + N], f == 0, f == nh - 1, reads=[Bw, Bact], writes=[Bp])
                    if moe:
                        t, Bt = tmp.next()
                        P.add("dve", lambda e, t=t, pt=pt, j=j, n0=n0, N=N, gbc=gbc: e.scalar_tensor_tensor(
                            out=t[:, :N], in0=pt[:, :N], scalar=mod_sb[:, j, 8:9], in1=gbc[:, n0:n0 + N],
                            op0=ALU.mult, op1=ALU.mult), [Bp, Bmod, Bgbc], [Bt])
                        P.add("pool", lambda e, t=t, j=j, n0=n0, N=N: e.tensor_tensor(
                            out=x_sb[:, j, n0:n0 + N], in0=x_sb[:, j, n0:n0 + N], in1=t[:, :N], op=ALU.add), [Bt, Bx], [Bx])
                    else:
                        P.add("dve", lambda e, pt=pt, j=j, n0=n0, N=N, seg=seg: e.scalar_tensor_tensor(
                            out=x_sb[:, j, n0:n0 + N], in0=pt[:, :N], scalar=mod_sb[:, j, 8 + seg:9 + seg],
                            in1=x_sb[:, j, n0:n0 + N], op0=ALU.mult, op1=ALU.add), [Bp, Bmod, Bx], [Bx])

    if moe:
        for bi, (n0, N) in enumerate(blks):
            pt, Bp = ps.next()
            for k4 in range(4):
                sq, Bsq = sq_ring.next()
                P.add("act", lambda e, sq=sq, n0=n0, N=N, k4=k4: e.activation(
                    out=sq[:, :, :N], in_=x_sb[:, 4 * k4:4 * k4 + 4, n0:n0 + N], func=AF.Square), [Bx], [Bsq])
                for kk in range(4):
                    k = 4 * k4 + kk
                    P.mm(pt[:, :N], ones_d[:], sq[:, kk, :N], k == 0, k == 15, reads=[Bones, Bsq], writes=[Bp])
            rstd, Br = rstd_from(pt, Bp, N)
            for k in range(16):
                P.add("dve", lambda e, k=k, n0=n0, N=N, rstd=rstd: e.scalar_tensor_tensor(
                    out=x_sb[:, k, n0:n0 + N], in0=x_sb[:, k, n0:n0 + N], scalar=mod_sb[:, k, 10:11], in1=rstd[:, :N],
                    op0=ALU.mult, op1=ALU.mult), [Bx, Bmod, Br], [Bx])
        for q4 in range(4):
            P.dma(xo[:, 4 * q4:4 * q4 + 4, :], x_sb[:, 4 * q4:4 * q4 + 4, :], reads=[Bx])
    else:
        for q4 in range(4):
            P.dma(xo[:, 4 * q4:4 * q4 + 4, :], x_sb[:, 4 * q4:4 * q4 + 4, :], reads=[Bx])
    C.pop()
    return C.done()


def build_k4():
    C = Ctx()
    nc, P = C.nc, C.P
    NTT = 8
    h2 = C.din("h2", [NTT, 128, 16, 1024], BF16)
    gate = C.din("gate", [128, NTT * 1024])
    w13 = C.din("w13", [2048, 2 * D_FFE]); w2 = C.din("w2", [D_FFE, 2048])
    yo = C.dout("y", [NTT, 128, 16, 1024])
    nsp = 4
    nh = (D_FFE // 128) // nsp
    ps = Ring(C.psum_ring(8).items)
    h_ring = C.ring(2, [128, 16, 1024], BF16)
    acc_ring = C.ring(1, [128, 16, 1024])
    g_ring = C.ring(2, [128, 1024])
    act = C.sb([128, nh, 1024], BF16); Bact = Buf()
    w13_ring = C.ring(3, [128, 16, 256], BF16)
    w2_ring = C.ring(3, [128, nh, 128], BF16)
    tmp = C.ring(4, [128, 512])
    blks = TBLK[:2]
    nxt_h = None

    def load_h(tt):
        h_sb, Bh = h_ring.next(); g_sb, Bg = g_ring.next()
        for q4 in range(4):
            P.dma(h_sb[:, 4 * q4:4 * q4 + 4, :], h2[tt, :, 4 * q4:4 * q4 + 4, :], writes=[Bh])
        P.dma(g_sb[:], gate[:, tt * 1024:(tt + 1) * 1024], writes=[Bg])
        return h_sb, Bh, g_sb, Bg
    nxt_h = load_h(0)
    for tt in range(NTT):
        h_sb, Bh, g_sb, Bg = nxt_h
        if tt + 1 < NTT:
            nxt_h = load_h(tt + 1)
        acc, Bacc = acc_ring.next()
        for sp in range(nsp):
            def load13(f):
                wt, Bw = w13_ring.next()
                c0 = (sp * nh + f) * 128
                P.dma(wt[:, :, 0:128], w13[:, c0:c0 + 128].rearrange("(k p) c -> p k c", p=128), writes=[Bw], q="pool")
                P.dma(wt[:, :, 128:256], w13[:, D_FFE + c0:D_FFE + c0 + 128].rearrange("(k p) c -> p k c", p=128),
                      writes=[Bw], q="pool")
                return wt, Bw
            q13 = [load13(0), load13(1)]
            for f in range(nh):
                wt, Bw = q13.pop(0)
                if f + 2 < nh:
                    q13.append(load13(f + 2))
                for bi, (n0, N) in enumerate(blks):
                    pg, Bpg = ps.next(); pu, Bpu = ps.next()
                    for k in range(16):
                        P.mm(pg[:, :N], wt[:, k, 0:128], h_sb[:, k, n0:n0 + N], k == 0, k == 15, reads=[Bw, Bh], writes=[Bpg])
                    for k in range(16):
                        P.mm(pu[:, :N], wt[:, k, 128:256], h_sb[:, k, n0:n0 + N], k == 0, k == 15, reads=[Bw, Bh], writes=[Bpu])
                    t, Bt = tmp.next()
                    P.add("act", lambda e, t=t, pg=pg, N=N: e.activation(out=t[:, :N], in_=pg[:, :N], func=AF.Silu), [Bpg], [Bt])
                    P.add("dve", lambda e, t=t, pu=pu, f=f, n0=n0, N=N: e.tensor_tensor(
                        out=act[:, f, n0:n0 + N], in0=pu[:, :N], in1=t[:, :N], op=ALU.mult), [Bpu, Bt], [Bact])

            def load2(j):
                wt, Bw = w2_ring.next()
                r0 = sp * nh * 128
                P.dma(wt[:], w2[r0:r0 + nh * 128, j * 128:(j + 1) * 128].rearrange("(f p) c -> p f c", p=128),
                      writes=[Bw], q="pool")
                return wt, Bw
            q2 = [load2(0), load2(1)]
            for j in range(16):
                wt, Bw = q2.pop(0)
                if j + 2 < 16:
                    q2.append(load2(j + 2))
                for bi, (n0, N) in enumerate(blks):
                    pt, Bp = ps.next()
                    for f in range(nh):
                        P.mm(pt[:, :N], wt[:, f, :], act[:, f, n0:n0 + N], f == 0, f == nh - 1, reads=[Bw, Bact], writes=[Bp])
                    if sp == 0:
                        P.add("dve", lambda e, pt=pt, j=j, n0=n0, N=N, acc=acc, g_sb=g_sb: e.tensor_tensor(
                            out=acc[:, j, n0:n0 + N], in0=pt[:, :N], in1=g_sb[:, n0:n0 + N], op=ALU.mult), [Bp, Bg], [Bacc])
                    else:
                        t, Bt = tmp.next()
                        P.add("dve", lambda e, t=t, pt=pt, n0=n0, N=N, g_sb=g_sb: e.tensor_tensor(
                            out=t[:, :N], in0=pt[:, :N], in1=g_sb[:, n0:n0 + N], op=ALU.mult), [Bp, Bg], [Bt])
                        P.add("pool", lambda e, t=t, j=j, n0=n0, N=N, acc=acc: e.tensor_tensor(
                            out=acc[:, j, n0:n0 + N], in0=acc[:, j, n0:n0 + N], in1=t[:, :N], op=ALU.add), [Bt, Bacc], [Bacc])
        for q4 in range(4):
            P.dma(yo[tt, :, 4 * q4:4 * q4 + 4, :], acc[:, 4 * q4:4 * q4 + 4, :], reads=[Bacc])
    return C.done()


def build_k5():
    C = Ctx()
    nc, P = C.nc, C.P
    xm = C.din("xm", [128, 16, 1024])
    ys = C.din("ys", [8, 128, 16, 1024])
    gv = C.din("gv", [128, 16, 2])
    out = C.dout("out", [128, 16, 1024])
    ps = Ring(C.psum_ring(8).items)
    x_sb = C.sb([128, 16, 1024]); Bx = Buf()
    g_sb = C.sb([128, 16, 2]); Bg = Buf()
    ones_d = C.sb([128, 128]); Bones = Buf()
    y_ring = C.ring(3, [128, 4, 1024])
    acc_ring = C.ring(2, [128, 4, 1024])
    sq_ring = C.ring(1, [128, 4, 512])
    rs_ring = C.ring(1, [128, 512])
    P.dma(g_sb[:], gv, writes=[Bg])
    P.add("pool", lambda e: e.memset(ones_d[:], 1.0 / 2048.0), [], [Bones])
    for k4 in range(4):
        ks = slice(4 * k4, 4 * k4 + 4)
        P.dma(x_sb[:, ks, :], xm[:, ks, :], writes=[Bx])
        acc, Bacc = acc_ring.next()
        for e_ in range(8):
            y, By = y_ring.next()
            P.dma(y[:], ys[e_, :, ks, :], writes=[By])
            if e_ == 0:
                continue_first = (y, By)
                continue
            eng = "dve" if e_ % 2 else "pool"
            if e_ == 1:
                y0, By0 = continue_first
                P.add(eng, lambda e, acc=acc, y=y, y0=y0: e.tensor_tensor(out=acc[:], in0=y0[:], in1=y[:], op=ALU.add),
                      [By, By0], [Bacc])
            else:
                P.add(eng, lambda e, acc=acc, y=y: e.tensor_tensor(out=acc[:], in0=acc[:], in1=y[:], op=ALU.add),
                      [By, Bacc], [Bacc])
        for kk in range(4):
            k = 4 * k4 + kk
            P.add("dve", lambda e, acc=acc, kk=kk, k=k: e.scalar_tensor_tensor(
                out=x_sb[:, k, :], in0=acc[:, kk, :], scalar=g_sb[:, k, 0:1], in1=x_sb[:, k, :], op0=ALU.mult, op1=ALU.add),
                  [Bacc, Bg, Bx], [Bx])
    for bi, (n0, N) in enumerate(TBLK[:2]):
        pt, Bp = ps.next()
        for k4 in range(4):
            sq, Bsq = sq_ring.next()
            P.add("act", lambda e, sq=sq, n0=n0, N=N, k4=k4: e.activation(
                out=sq[:, :, :N], in_=x_sb[:, 4 * k4:4 * k4 + 4, n0:n0 + N], func=AF.Square), [Bx], [Bsq])
            for kk in range(4):
                k = 4 * k4 + kk
                P.mm(pt[:, :N], ones_d[:], sq[:, kk, :N], k == 0, k == 15, reads=[Bones, Bsq], writes=[Bp])
        rstd, Br = rs_ring.next()
        P.add("act", lambda e, rstd=rstd, pt=pt, N=N: e.activation(out=rstd[:, :N], in_=pt[:, :N], func=AF.Ln, bias=EPS), [Bp], [Br])
        P.add("act", lambda e, rstd=rstd, N=N: e.activation(out=rstd[:, :N], in_=rstd[:, :N], func=AF.Exp, scale=-0.5), [Br], [Br])
        for k in range(16):
            P.add("dve", lambda e, k=k, n0=n0, N=N, rstd=rstd: e.scalar_tensor_tensor(
                out=x_sb[:, k, n0:n0 + N], in0=x_sb[:, k, n0:n0 + N], scalar=g_sb[:, k, 1:2], in1=rstd[:, :N],
                op0=ALU.mult, op1=ALU.mult), [Bx, Bg, Br], [Bx])
    for q4 in range(4):
        P.dma(out[:, 4 * q4:4 * q4 + 4, :], x_sb[:, 4 * q4:4 * q4 + 4, :], reads=[Bx])
    return C.done()


def _mix_for_core(r2, b, q, T):
    mix = np.empty((128, 16, T), np.float32)
    for mi, key in enumerate(("oa", "ob", "oc", "od")):
        for hh in range(4):
            o = np.asarray(r2[4 * b + hh][key])
            mix[:, mi * 4 + hh, :1024] = o[:, 256 + q * 1024:256 + (q + 1) * 1024]
            if T > 1024:
                mix[:, mi * 4 + hh, 1024:] = o[:, q * 64:(q + 1) * 64]
    return mix


def _modv3(l, b, m, p):
    ch = lambda r, c: _fm(m[l, r, c * 2048:(c + 1) * 2048])
    cols = [_fm(p["out_norm"][l]), ch(b, 2), ch(2, 2), _fm(p["norm_ffn"][l]), ch(b, 4), ch(b, 3), ch(2, 4), ch(2, 3),
            ch(b, 5), ch(2, 5), _fm(p["final_norm"])]
    return np.ascontiguousarray(np.stack(cols, axis=2).astype(np.float32))


def run_k3_dense(l, xfm, r2, m, p):
    in_maps = []
    w_out = np.ascontiguousarray(p["w_out"][l]); w13 = np.ascontiguousarray(p["ffn_w13"][l // 2])
    w2 = np.ascontiguousarray(p["ffn_w2"][l // 2])
    for i in range(NCORES):
        b, q = i // 4, i % 4
        in_maps.append({"xT": xfm[i], "mixT": _mix_for_core(r2, b, q, T1), "modv": _modv3(l, b, m, p),
                        "w_out": w_out, "w13": w13, "w2": w2})
    res = _run(build_k3(False), in_maps)
    return [np.asarray(r["xo"]) for r in res]


def run_moe_pre(l, xfm, r2, m, p):
    w_out = np.ascontiguousarray(p["w_out"][l])
    router = np.ascontiguousarray(p["router"][l // 2].reshape(16, 128, 8).transpose(1, 0, 2))
    ident = np.eye(128, dtype=np.float32)
    sel = np.zeros((8, 8, 128), np.float32)
    for e in range(8):
        sel[e, e, :] = 1.0
    in_maps = []
    for i in range(NCORES):
        b, q = i // 4, i % 4
        in_maps.append({"xT": np.ascontiguousarray(xfm[i][:, :, :1024]), "mixT": _mix_for_core(r2, b, q, 1024),
                        "modv": _modv3(l, b, m, p), "w_out": w_out, "router": router, "ident": ident, "sel": sel})
    return _run(build_k3(True), in_maps)


def run_moe_dense(l, r3, m, p):
    h2_all = np.ascontiguousarray(np.stack([np.asarray(r["h2o"]) for r in r3], axis=0))
    gates = np.concatenate([np.asarray(r["gates"]) for r in r3], axis=0)
    in_maps = []
    for e in range(NCORES):
        in_maps.append({"h2": h2_all, "gate": np.ascontiguousarray(np.broadcast_to(gates[None, :, e], (128, 8192))),
                        "w13": np.ascontiguousarray(p["moe_w13"][l // 2][e]), "w2": np.ascontiguousarray(p["moe_w2"][l // 2][e])})
    r4 = _run(build_k4(), in_maps)
    in_maps = []
    for i in range(NCORES):
        b = i // 4
        ys = np.ascontiguousarray(np.stack([np.asarray(r4[e]["y"][i]) for e in range(8)], axis=0))
        gv = np.ascontiguousarray(np.stack([_fm(m[l, b, 5 * 2048:6 * 2048]), _fm(p["final_norm"])], axis=2).astype(np.float32))
        in_maps.append({"xm": np.asarray(r3[i]["xo"]), "ys": ys, "gv": gv})
    r5 = _run(build_k5(), in_maps)
    return [np.asarray(r["out"]) for r in r5]


def run_moe_sparse(l, r3, m, p, nb=2):
    cap = nb * SB
    h2tm = np.ascontiguousarray(np.concatenate(
        [np.asarray(r["h2o"]).transpose(1, 0, 2).reshape(2048, 1024).T for r in r3], axis=0))
    gates = np.concatenate([np.asarray(r["gates"]) for r in r3], axis=0)
    pi = np.arange(128)
    lstrict = (pi[:, None] < pi[None, :]).astype(np.float32)
    ti_ = np.arange(64)
    ustrict = (ti_[:, None] < ti_[None, :]).astype(np.float32)
    ident = np.eye(128, dtype=np.float32)
    identb = np.eye(128, dtype=np.float32).astype(h2tm.dtype)
    in_maps = []
    for e in range(NCORES):
        in_maps.append({"h2tm": h2tm, "gcol": np.ascontiguousarray(gates[:, e].reshape(64, 128).T),
                        "w13": np.ascontiguousarray(p["moe_w13"][l // 2][e]), "w2": np.ascontiguousarray(p["moe_w2"][l // 2][e]),
                        "lstrict": lstrict, "ustrict": ustrict, "ident": ident, "identb": identb})
    r4 = _run(build_k4s(nb), in_maps)
    for e in range(NCORES):
        if float(np.asarray(r4[e]["cnt"])[0, 0]) > cap:
            return None
    yc_all = np.ascontiguousarray(np.stack([np.asarray(r4[e]["yc"]) for e in range(8)], axis=0))
    pos_all = np.stack([np.asarray(r4[e]["pos"]) for e in range(8)], axis=2)
    gat_all = gates.reshape(64, 128, 8).transpose(1, 0, 2)
    in_maps = []
    for i in range(NCORES):
        b = i // 4
        xm = np.ascontiguousarray(np.asarray(r3[i]["xo"]).transpose(1, 0, 2).reshape(2048, 1024).T)
        gb = np.stack([np.broadcast_to(m[l, b, 5 * 2048:6 * 2048][None, :], (128, 2048)),
                       np.broadcast_to(p["final_norm"][None, :], (128, 2048))], axis=1).astype(np.float32)
        in_maps.append({"xm": xm, "yc": yc_all, "pos": np.ascontiguousarray(pos_all[:, 8 * i:8 * i + 8, :]),
                        "gat": np.ascontiguousarray(gat_all[:, 8 * i:8 * i + 8, :]).astype(np.float32),
                        "gb": np.ascontiguousarray(gb)})
    r5 = _run(build_k5s(nb), in_maps)
    return [np.asarray(r["out"]) for r in r5]


def run_moe_layer(l, xfm, r2, m, p):
    r3 = run_moe_pre(l, xfm, r2, m, p)
    gates = np.concatenate([np.asarray(r["gates"]) for r in r3], axis=0)
    nb = int(max(1, -(-int((gates > 0).sum(axis=0).max()) // SB)))
    outs = run_moe_sparse(l, r3, m, p, nb=nb)
    if outs is not None:
        return outs
    return [_fm_to_tok(o) for o in run_moe_dense(l, r3, m, p)]


def kernel(**inputs):
    p = {k: np.asarray(v) for k, v in inputs.items()}
    x, ctx = p["x"], p["ctx"]
    m = run_mod(p["c"], p["c_ctx"], p["w_mod"], p["b_mod"])
    xfm = []
    for i in range(NCORES):
        b, q = i // 4, i % 4
        tok = np.concatenate([x[b, q * 1024:(q + 1) * 1024], ctx[b, q * 64:(q + 1) * 64]], axis=0)
        xfm.append(_tok_to_fm(tok))
    r1 = run_k1(0, xfm, m, p)
    r2 = run_k2(0, r1, p)
    xfm = run_k3_dense(0, xfm, r2, m, p)
    r1 = run_k1(1, xfm, m, p)
    r2 = run_k2(1, r1, p)
    outs = run_moe_layer(1, xfm, r2, m, p)
    out = np.empty((2, 4096, 2048), np.float32)
    for i in range(NCORES):
        b, q = i // 4, i % 4
        out[b, q * 1024:(q + 1) * 1024] = outs[i]
    return out


SB = 1280
I32 = mybir.dt.int32


def _bc_reg(e, cache, val):
    if "r" not in cache:
        cache["r"] = e.to_reg(val)
    return cache["r"]


def build_k4s(nb):
    C = Ctx()
    nc, P = C.nc, C.P
    CAP = nb * SB
    bcc = {}
    h2tm = C.din("h2tm", [8192, 2048], BF16)
    gcol_d = C.din("gcol", [128, 64])
    w13 = C.din("w13", [2048, 2 * D_FFE]); w2 = C.din("w2", [D_FFE, 2048])
    d_ls = C.din("lstrict", [128, 128]); d_us = C.din("ustrict", [64, 64]); d_id = C.din("ident", [128, 128])
    d_idb = C.din("identb", [128, 128], BF16)
    yo = C.dout("yc", [CAP, 2048])
    poso = C.dout("pos", [128, 64], I32)
    cnto = C.dout("cnt", [128, 1])
    Hc = nc.dram_tensor("Hc", [CAP, 2048], BF16).ap(); BHc = Buf()
    banks = []
    for i in range(6):
        banks.append((C.es.enter_context(nc.psum_tensor("ps%d" % i, [128, 512], F32)), Buf()))
    bbanks = []
    for i in range(2):
        bbanks.append((C.es.enter_context(nc.psum_tensor("pb%d" % i, [128, 1024], BF16)), Buf()))
    ps = Ring(banks); pb = Ring(bbanks)
    nsp = 8
    nh = (D_FFE // 128) // nsp

    gcol = C.sb([128, 64]); Bg = Buf()
    ls = C.sb([128, 128]); us = C.sb([64, 64]); ident = C.sb([128, 128]); identb = C.sb([128, 128], BF16); Bc = Buf()
    ones = C.sb([128, 128]); zer = C.sb([128, 2048], BF16)
    P.dma(gcol[:], gcol_d, writes=[Bg])
    P.dma(ls[:], d_ls, writes=[Bc]); P.dma(us[:], d_us, writes=[Bc]); P.dma(ident[:], d_id, writes=[Bc])
    P.dma(identb[:], d_idb, writes=[Bc])
    P.add("pool", lambda e: e.memset(ones[:], 1.0), [], [Bc])
    P.add("pool", lambda e: e.memset(zer[:], 0.0), [], [Bc])
    for t in range(CAP // 128):
        P.dma(Hc[t * 128:(t + 1) * 128, :], zer[:], reads=[Bc], writes=[BHc])
    mask = C.sb([128, 64]); Bm = Buf()
    P.add("dve", lambda e: e.tensor_single_scalar(out=mask[:], in_=gcol[:], scalar=0.0, op=ALU.is_gt), [Bg], [Bm])
    pt, Bp = ps.next()
    P.add("pe", lambda e, pt=pt: e.transpose(pt[0:64, 0:128], mask[:], ident[:]), [Bm, Bc], [Bp])
    maskT = C.sb([64, 128]); BmT = Buf()
    P.add("dve", lambda e, pt=pt: e.tensor_copy(out=maskT[:], in_=pt[0:64, 0:128]), [Bp], [BmT])
    p2, Bp2 = ps.next()
    P.mm(p2[:, 0:64], maskT[:], us[:], True, True, reads=[BmT, Bc], writes=[Bp2])
    mu = C.sb([128, 64]); Bmu = Buf()
    P.add("dve", lambda e, p2=p2: e.tensor_copy(out=mu[:], in_=p2[:, 0:64]), [Bp2], [Bmu])
    p3, Bp3 = ps.next()
    P.mm(p3[:, 0:64], ones[:], mu[:], True, False, reads=[Bc, Bmu], writes=[Bp3])
    P.mm(p3[:, 0:64], ls[:], mask[:], False, True, reads=[Bc, Bm], writes=[Bp3])
    big = C.sb([128, 64]); posf = C.sb([128, 64]); posi = C.sb([128, 64], I32); Bpos = Buf()
    P.add("dve", lambda e: e.tensor_scalar(out=big[:], in0=mask[:], scalar1=-1.0e6, scalar2=1.0e6, op0=ALU.mult, op1=ALU.add),
          [Bm], [Bpos])
    P.add("dve", lambda e, p3=p3: e.tensor_tensor(out=posf[:], in0=p3[:, 0:64], in1=big[:], op=ALU.add), [Bp3, Bpos], [Bpos])
    P.add("dve", lambda e: e.tensor_copy(out=posi[:], in_=posf[:]), [Bpos], [Bpos])
    P.dma(poso, posi[:], reads=[Bpos])
    p4, Bp4 = ps.next()
    P.mm(p4[:, 0:64], ones[:], mask[:], True, True, reads=[Bc, Bm], writes=[Bp4])
    cnt = C.sb([128, 1]); Bcnt = Buf()
    P.add("dve", lambda e, p4=p4: e.tensor_reduce(out=cnt[:], in_=p4[:, 0:64], axis=AX.X, op=ALU.add), [Bp4], [Bcnt])
    P.dma(cnto, cnt[:], reads=[Bcnt])

    ld_ring = C.ring(3, [128, 2048], BF16)
    for t in range(64):
        ht, Bht = ld_ring.next()
        P.dma(ht[:], h2tm[t * 128:(t + 1) * 128, :], writes=[Bht])
        P.add("pool", lambda e, ht=ht, t=t: e.indirect_dma_start(
            out=Hc[:, :], out_offset=bass.IndirectOffsetOnAxis(ap=posi[:, t:t + 1], axis=0), in_=ht[:, :], in_offset=None,
            bounds_check=_bc_reg(e, bcc, CAP - 1), oob_is_err=False), [Bht, Bpos, BHc], [BHc], dma=True)

    hfm = C.sb([128, 16, SB], BF16); Bh = Buf()
    act = C.sb([128, nh, SB], BF16); Bact = Buf()
    acc = C.sb([128, SB // 128, 2048]); Bacc = Buf()
    w13_ring = C.ring(3, [128, 16, 256], BF16)
    w2_ring = C.ring(2, [128, nh, 512], BF16)
    tmp = C.ring(4, [128, 512])
    nblks = [(0, 512), (512, 512), (1024, 256)]
    for b in range(nb):
        s0 = b * SB
        for st in range(SB // 128):
            ht, Bht = ld_ring.next()
            P.dma(ht[:], Hc[s0 + st * 128:s0 + (st + 1) * 128, :], reads=[BHc], writes=[Bht])
            for half in range(2):
                pbt, Bpb = pb.next()
                for kk in range(8):
                    k = half * 8 + kk
                    P.add("pe", lambda e, pbt=pbt, kk=kk, k=k, ht=ht: e.transpose(
                        pbt[:, kk * 128:(kk + 1) * 128], ht[:, k * 128:(k + 1) * 128], identb[:]), [Bht, Bc], [Bpb])
                P.add("dve", lambda e, pbt=pbt, half=half, st=st: e.tensor_copy(
                    out=hfm[:, half * 8:half * 8 + 8, st * 128:(st + 1) * 128],
                    in_=pbt[:, :].rearrange("p (k s) -> p k s", s=128)), [Bpb], [Bh])
        for sp in range(nsp):
            def load13(f):
                wt, Bw = w13_ring.next()
                c0 = (sp * nh + f) * 128
                P.dma(wt[:, :, 0:128], w13[:, c0:c0 + 128].rearrange("(k p) c -> p k c", p=128), writes=[Bw], q="pool")
                P.dma(wt[:, :, 128:256], w13[:, D_FFE + c0:D_FFE + c0 + 128].rearrange("(k p) c -> p k c", p=128),
                      writes=[Bw], q="pool")
                return wt, Bw
            q13 = [load13(0), load13(1)]
            for f in range(nh):
                wt, Bw = q13.pop(0)
                if f + 2 < nh:
                    q13.append(load13(f + 2))
                for (n0, N) in nblks:
                    pg, Bpg = ps.next(); pu, Bpu = ps.next()
                    for k in range(16):
                        P.mm(pg[:, :N], wt[:, k, 0:128], hfm[:, k, n0:n0 + N], k == 0, k == 15, reads=[Bw, Bh], writes=[Bpg])
                    for k in range(16):
                        P.mm(pu[:, :N], wt[:, k, 128:256], hfm[:, k, n0:n0 + N], k == 0, k == 15, reads=[Bw, Bh], writes=[Bpu])
                    t_, Bt = tmp.next()
                    P.add("act", lambda e, t_=t_, pg=pg, N=N: e.activation(out=t_[:, :N], in_=pg[:, :N], func=AF.Silu), [Bpg], [Bt])
                    P.add("dve", lambda e, t_=t_, pu=pu, f=f, n0=n0, N=N: e.tensor_tensor(
                        out=act[:, f, n0:n0 + N], in0=pu[:, :N], in1=t_[:, :N], op=ALU.mult), [Bpu, Bt], [Bact])

            def load2(db):
                wt, Bw = w2_ring.next()
                r0 = sp * nh * 128
                P.dma(wt[:], w2[r0:r0 + nh * 128, db * 512:(db + 1) * 512].rearrange("(f p) c -> p f c", p=128),
                      writes=[Bw], q="pool")
                return wt, Bw
            nxt = load2(0)
            for db in range(4):
                wt, Bw = nxt
                if db + 1 < 4:
                    nxt = load2(db + 1)
                for st in range(SB // 128):
                    pt, Bp = ps.next()
                    for f in range(nh):
                        P.mm(pt[:, :], act[:, f, st * 128:(st + 1) * 128], wt[:, f, :], f == 0, f == nh - 1,
                             reads=[Bw, Bact], writes=[Bp])
                    dst = acc[:, st, db * 512:(db + 1) * 512]
                    if sp == 0:
                        P.add("act", lambda e, dst=dst, pt=pt: e.activation(out=dst, in_=pt[:, :], func=AF.Identity), [Bp], [Bacc])
                    else:
                        P.add("dve", lambda e, dst=dst, pt=pt: e.tensor_tensor(out=dst, in0=pt[:, :], in1=dst, op=ALU.add),
                              [Bp, Bacc], [Bacc])
        for st in range(SB // 128):
            P.dma(yo[s0 + st * 128:s0 + (st + 1) * 128, :], acc[:, st, :], reads=[Bacc])
    return C.done()


def build_k5s(nb):
    C = Ctx()
    nc, P = C.nc, C.P
    CAP = nb * SB
    bcc = {}
    xm = C.din("xm", [1024, 2048])
    yc = C.din("yc", [8, CAP, 2048])
    pos = C.din("pos", [128, 8, 8], I32)
    gat = C.din("gat", [128, 8, 8])
    gb = C.din("gb", [128, 2, 2048])
    out = C.dout("out", [1024, 2048])
    pos_sb = C.sb([128, 8, 8], I32); gat_sb = C.sb([128, 8, 8]); gb_sb = C.sb([128, 2, 2048]); Bin = Buf()
    P.dma(pos_sb[:], pos, writes=[Bin]); P.dma(gat_sb[:], gat, writes=[Bin]); P.dma(gb_sb[:], gb, writes=[Bin])
    g_ring = C.ring(4, [128, 2048])
    x_ring = C.ring(2, [128, 2048])
    a_ring = C.ring(2, [128, 2048])
    sq_ring = C.ring(2, [128, 2048])
    sm = C.ring(4, [128, 2])
    for ti in range(8):
        x, Bx = x_ring.next()
        P.dma(x[:], xm[ti * 128:(ti + 1) * 128, :], writes=[Bx])
        acc, Bacc = a_ring.next()
        for e_ in range(8):
            g, Bgt = g_ring.next()
            P.add("pool", lambda e, g=g: e.memset(g[:], 0.0), [], [Bgt])
            P.add("pool", lambda e, g=g, ti=ti, e_=e_: e.indirect_dma_start(
                out=g[:, :], out_offset=None, in_=yc.rearrange("e c d -> (e c) d"),
                in_offset=bass.IndirectOffsetOnAxis(ap=pos_sb[:, ti, e_:e_ + 1], axis=0),
                element_offset=e_ * CAP * 2048,
                bounds_check=_bc_reg(e, bcc, CAP - 1), oob_is_err=False), [Bin, Bgt], [Bgt], dma=True)
            if e_ == 0:
                P.add("dve", lambda e, acc=acc, g=g, ti=ti: e.tensor_scalar(
                    out=acc[:], in0=g[:], scalar1=gat_sb[:, ti, 0:1], scalar2=None, op0=ALU.mult), [Bgt, Bin], [Bacc])
            else:
                P.add("dve", lambda e, acc=acc, g=g, ti=ti, e_=e_: e.scalar_tensor_tensor(
                    out=acc[:], in0=g[:], scalar=gat_sb[:, ti, e_:e_ + 1], in1=acc[:], op0=ALU.mult, op1=ALU.add),
                      [Bgt, Bin, Bacc], [Bacc])
        P.add("pool", lambda e, acc=acc: e.tensor_tensor(out=acc[:], in0=acc[:], in1=gb_sb[:, 0, :], op=ALU.mult), [Bacc, Bin], [Bacc])
        P.add("dve", lambda e, acc=acc, x=x: e.tensor_tensor(out=x[:], in0=x[:], in1=acc[:], op=ALU.add), [Bacc, Bx], [Bx])
        sq, Bsq = sq_ring.next(); s_, Bs = sm.next()
        P.add("act", lambda e, sq=sq, x=x: e.activation(out=sq[:], in_=x[:], func=AF.Square), [Bx], [Bsq])
        P.add("dve", lambda e, sq=sq, s_=s_: e.tensor_reduce(out=s_[:, 0:1], in_=sq[:], axis=AX.X, op=ALU.add), [Bsq], [Bs])
        P.add("act", lambda e, s_=s_: e.activation(out=s_[:, 1:2], in_=s_[:, 0:1], func=AF.Ln, bias=EPS, scale=1.0 / 2048.0), [Bs], [Bs])
        P.add("act", lambda e, s_=s_: e.activation(out=s_[:, 0:1], in_=s_[:, 1:2], func=AF.Exp, scale=-0.5), [Bs], [Bs])
        P.add("dve", lambda e, x=x, s_=s_: e.scalar_tensor_tensor(
            out=x[:], in0=x[:], scalar=s_[:, 0:1], in1=gb_sb[:, 1, :], op0=ALU.mult, op1=ALU.mult), [Bx, Bs, Bin], [Bx])
        P.dma(out[ti * 128:(ti + 1) * 128, :], x[:], reads=[Bx])
    return C.done()
```

```python
import numpy as np
from contextlib import ExitStack
import concourse.bass as bass
import concourse.mybir as mybir
from concourse.bass_utils import run_bass_kernel_spmd

F32 = mybir.dt.float32
BF16 = mybir.dt.bfloat16
AF = mybir.ActivationFunctionType
ALU = mybir.AluOpType
AX = mybir.AxisListType
NCORES = 8


class Buf:
    __slots__ = ("name", "w", "r")

    def __init__(self, name=""):
        self.name = name
        self.w = None
        self.r = {}


class _Op:
    __slots__ = ("eng", "fn", "deps", "sig", "val", "dma", "key", "cc")


class Prog:
    ENGS = ("pe", "act", "dve", "pool", "sp")

    def __init__(self, nc):
        self.nc = nc
        self.ops = []
        self.last = {}
        self.dmas = []
        self.bar = {}

    def barrier(self):
        deps = list(self.last.values()) + list(self.dmas)
        for d in deps:
            d.sig = True
        self.dmas = []
        for e in self.ENGS:
            self.bar[e] = list(self.bar.get(e, [])) + deps

    def add(self, eng, fn, reads=(), writes=(), dma=False, cc=False):
        op = _Op()
        op.eng, op.fn, op.dma, op.sig, op.val = eng, fn, dma, dma, 0
        op.cc = cc
        deps = set()
        for b in reads:
            if b.w is not None:
                deps.add(b.w)
        for b in writes:
            if b.w is not None:
                deps.add(b.w)
            for r in b.r.values():
                deps.add(r)
        key = (eng, dma)
        for b in reads:
            b.r[key] = op
        for b in writes:
            b.w = op
            b.r = {}
        deps.discard(op)
        if eng == "pe" and not dma:
            deps = {d for d in deps if not (d.eng == "pe" and not d.dma)}
        if self.bar.get(eng):
            deps.update(d for d in self.bar[eng] if not (d.eng == eng and not d.dma and eng == "pe"))
            self.bar[eng] = []
        if dma:
            self.dmas.append(op)
        else:
            self.last[eng] = op
        op.deps = deps
        for d in deps:
            d.sig = True
        self.ops.append(op)
        return op

    def dma(self, out, in_, reads=(), writes=(), q="sp"):
        return self.add(q, lambda e: e.dma_start(out=out, in_=in_), reads, writes, dma=True)

    def mm(self, out, lhsT, rhs, start, stop, reads=(), writes=()):
        return self.add("pe", lambda e: e.matmul(out, lhsT, rhs, start=start, stop=stop), reads, writes)

    NDS = 24

    def finish(self):
        nc = self.nc
        cnt = {}
        dma_hist = {}
        ncc = 0
        for op in self.ops:
            if op.cc:
                op.key = ("cc", True, ncc)
                ncc += 1
                op.val = 1
                cnt[op.key] = 1
            elif op.dma:
                hist = dma_hist.setdefault(op.eng, [])
                j = len(hist)
                if j >= self.NDS:
                    op.deps.add(hist[j - self.NDS])
                hist.append(op)
                op.key = (op.eng, True, j % self.NDS)
                op.val = 16 * (j // self.NDS + 1)
                cnt[op.key] = op.val
            elif op.sig:
                op.key = (op.eng, False, 0)
                cnt[op.key] = cnt.get(op.key, 0) + 1
                op.val = cnt[op.key]
        with ExitStack() as es:
            sems = {}
            for k in cnt:
                sems[k] = es.enter_context(nc.semaphore("s_%s_%d_%d" % (k[0], int(k[1]), k[2])))
            block = es.enter_context(nc.Block())
            reg = {"pe": block.tensor, "act": block.scalar, "dve": block.vector,
                   "pool": block.gpsimd, "sp": block.sync}
            for eng in self.ENGS:
                ops_e = [op for op in self.ops if op.eng == eng]
                if not ops_e:
                    continue

                def body(e, ops_e=ops_e, eng=eng):
                    waited = {}
                    for op in ops_e:
                        need = {}
                        for d in op.deps:
                            if d.val > need.get(d.key, 0):
                                need[d.key] = d.val
                        for k, v in need.items():
                            if waited.get(k, 0) < v:
                                e.wait_ge(sems[k], v)
                                waited[k] = v
                        ins = op.fn(e)
                        if op.cc:
                            ins.then_inc(sems[op.key])
                        elif op.sig:
                            ins.then_inc(sems[op.key], 16 if op.dma else 1)
                    for k, v in cnt.items():
                        if (k[0] == eng or (k[0] == "cc" and eng == "pool")) and k[1] and waited.get(k, 0) < v:
                            e.wait_ge(sems[k], v)

                reg[eng](body)


def _run(nc, in_maps):
    res = run_bass_kernel_spmd(nc, in_maps, core_ids=list(range(NCORES)))
    return res.results


MOD_COLS = 12288 // NCORES


def build_mod():
    nc = bass.Bass("TRN2", target_bir_lowering=False)
    cT = nc.dram_tensor("cT", [128, 16, 3], F32, kind="ExternalInput").ap()
    w = nc.dram_tensor("w", [2, 2048, MOD_COLS], F32, kind="ExternalInput").ap()
    b = nc.dram_tensor("b", [2, 3, MOD_COLS], F32, kind="ExternalInput").ap()
    m = nc.dram_tensor("m", [2, 3, MOD_COLS], F32, kind="ExternalOutput").ap()
    P = Prog(nc)
    with ExitStack() as es:
        sb = lambda name, shape, dt=F32: es.enter_context(nc.sbuf_tensor(name, shape, dt))
        c_sb = sb("c_sb", [128, 16, 3])
        s_sb = sb("s_sb", [128, 16, 3])
        b_sb = sb("b_sb", [3, 2, MOD_COLS])
        o_sb = sb("o_sb", [3, 2, MOD_COLS])
        wt = [sb("wt%d" % i, [128, 16, 512]) for i in range(2)]
        ps = [es.enter_context(nc.psum_tensor("ps%d" % i, [128, 512], F32)) for i in range(2)]
        Bc, Bs, Bb, Bo = Buf("c"), Buf("s"), Buf("b"), Buf("o")
        Bw = [Buf("w0"), Buf("w1")]
        Bp = [Buf("p0"), Buf("p1")]
        P.dma(c_sb[:], cT, writes=[Bc])
        P.dma(b_sb[:], b.rearrange("l r c -> r l c"), writes=[Bb])
        P.add("act", lambda e: e.activation(out=s_sb[:], in_=c_sb[:], func=AF.Silu), [Bc], [Bs])
        it = 0
        for l in range(2):
            for n in range(MOD_COLS // 512):
                j = it % 2
                P.dma(wt[j][:], w[l, :, n * 512:(n + 1) * 512].rearrange("(k p) c -> p k c", p=128),
                      writes=[Bw[j]])
                for k in range(16):
                    P.mm(ps[j][0:3, :], s_sb[:, k, :], wt[j][:, k, :], k == 0, k == 15,
                         reads=[Bs, Bw[j]], writes=[Bp[j]])
                P.add("dve", lambda e, j=j, l=l, n=n: e.tensor_tensor(
                    out=o_sb[:, l, n * 512:(n + 1) * 512], in0=ps[j][0:3, :],
                    in1=b_sb[:, l, n * 512:(n + 1) * 512], op=ALU.add), [Bp[j], Bb], [Bo])
                it += 1
        P.dma(m.rearrange("l r c -> r l c"), o_sb[:], reads=[Bo])
        P.finish()
    return nc


def run_mod(c, c_ctx, w_mod, b_mod):
    call = np.concatenate([c, c_ctx[None]], 0).astype(np.float32)
    cT = np.ascontiguousarray(call.T.reshape(16, 128, 3).transpose(1, 0, 2))
    nc = build_mod()
    in_maps = []
    for i in range(NCORES):
        sl = slice(i * MOD_COLS, (i + 1) * MOD_COLS)
        in_maps.append({"cT": cT, "w": np.ascontiguousarray(w_mod[:, :, sl]),
                        "b": np.ascontiguousarray(np.broadcast_to(b_mod[:, None, sl], (2, 3, MOD_COLS)))})
    res = _run(nc, in_maps)
    return np.concatenate([r["m"] for r in res], axis=2)


class Ring:
    def __init__(self, items):
        self.items = items
        self.i = 0

    def next(self):
        it = self.items[self.i % len(self.items)]
        self.i += 1
        return it


class Ctx:
    def __init__(self):
        self.nc = bass.Bass("TRN2", target_bir_lowering=False)
        self.P = Prog(self.nc)
        self.es = ExitStack()
        self.stacks = [self.es]
        self.n = 0

    def push(self):
        self.stacks.append(ExitStack())

    def pop(self):
        self.P.barrier()
        self.stacks.pop().close()

    def din(self, name, shape, dt=F32):
        return self.nc.dram_tensor(name, list(shape), dt, kind="ExternalInput").ap()

    def dout(self, name, shape, dt=F32):
        return self.nc.dram_tensor(name, list(shape), dt, kind="ExternalOutput").ap()

    def sb(self, shape, dt=F32, name=None):
        self.n += 1
        return self.stacks[-1].enter_context(self.nc.sbuf_tensor(name or "t%d" % self.n, list(shape), dt))

    def ring(self, n, shape, dt=F32):
        return Ring([(self.sb(shape, dt), Buf()) for _ in range(n)])

    def psum_ring(self, n=8):
        items = []
        for i in range(n):
            t = self.es.enter_context(self.nc.psum_tensor("ps%d" % i, [128, 512], F32))
            items.append((t, Buf("ps%d" % i)))
        return Ring(items)

    def done(self):
        self.P.finish()
        self.es.close()
        return self.nc


EPS = 1e-6
C_AQ, C_BQ, C_CU, C_CV, C_DZ, C_DC = 0, 512, 1024, 1536, 2048, 2560
C_AK, C_AV, C_BK, C_BV, C_DX, C_DB, C_DT = 2816, 3072, 3328, 3840, 4352, 4864, 5120
IN_COLS = 5136
T1 = 1088
TBLK = ((0, 512), (512, 512), (1024, 64))


def build_k1():
    C = Ctx()
    nc, P = C.nc, C.P
    T = T1
    xT = C.din("xT", [128, 16, T])
    modv = C.din("modv", [128, 16, 5])
    w_in = C.din("w_in", [2048, IN_COLS])
    qkg = C.din("qkg", [128, 2])
    ropeC = C.din("ropeC", [128, 1024])
    ropeS = C.din("ropeS", [128, 1024])
    sgug = C.din("sgug", [128, 512])
    rmat = C.din("rmat", [128, 128])
    o_qkA = C.dout("qkA", [6, 128, T], BF16)
    o_bqk = C.dout("bqk", [8, 128, T], BF16)
    o_cu = C.dout("cu", [4, 128, T])
    o_dz = C.dout("dz", [4, 128, T])
    o_dcxb = C.dout("dcxb", [8, 128, T])
    o_av = C.dout("av", [T, 256], BF16)
    o_bv = C.dout("bv", [T, 512], BF16)
    o_vn = C.dout("vn", [T, 512])
    o_ddt = C.dout("ddt", [T, 16])

    x_sb = C.sb([128, 16, T]); Bx = Buf("x")
    h_sb = C.sb([128, 16, T], BF16); Bh = Buf("h")
    mod_sb = C.sb([128, 16, 5]); Bmod = Buf()
    gs_sb = C.sb([128, 16, 2]); Bgs = Buf()
    qkg_sb = C.sb([128, 2]); Bqkg = Buf()
    rc_sb = C.sb([128, 1024]); rs_sb = C.sb([128, 1024]); Brope = Buf()
    sg_sb = C.sb([128, 512]); Bsg = Buf()
    rm_sb = C.sb([128, 128]); Brm = Buf()
    ones_d = C.sb([128, 128]); ones_h = C.sb([128, 128]); Bones = Buf()
    ps = C.psum_ring(8)
    sq_ring = C.ring(2, [128, 4, 512])
    tmp = C.ring(6, [128, 512])
    small = C.ring(4, [128, 2])
    w_ring = C.ring(2, [128, 16, 512], BF16)
    of32 = C.ring(2, [128, T])
    obf = C.ring(2, [128, T], BF16)
    otm = C.ring(2, [128, 512])
    otb = C.ring(2, [128, 512], BF16)
    rs_ring = C.ring(1, [128, 512])

    for q4 in range(4):
        P.dma(x_sb[:, 4 * q4:4 * q4 + 4, :], xT[:, 4 * q4:4 * q4 + 4, :], writes=[Bx])
    P.dma(mod_sb[:], modv, writes=[Bmod])
    P.dma(qkg_sb[:], qkg, writes=[Bqkg])
    P.dma(rc_sb[:], ropeC, writes=[Brope])
    P.dma(rs_sb[:], ropeS, writes=[Brope])
    P.dma(sg_sb[:], sgug, writes=[Bsg])
    P.dma(rm_sb[:], rmat, writes=[Brm])
    P.add("pool", lambda e: e.memset(ones_d[:], 1.0 / 2048.0), [], [Bones])
    P.add("pool", lambda e: e.memset(ones_h[:], 1.0 / 128.0), [], [Bones])
    for j, col in enumerate((1, 3)):
        P.add("dve", lambda e, j=j, col=col: e.scalar_tensor_tensor(
            out=gs_sb[:, :, j], in0=mod_sb[:, :, col], scalar=1.0, in1=mod_sb[:, :, 0],
            op0=ALU.add, op1=ALU.mult), [Bmod], [Bgs])

    for bi, (n0, N) in enumerate(TBLK):
        seg = 0 if bi < 2 else 1
        pt, Bp = ps.next()
        for k4 in range(4):
            sq, Bsq = sq_ring.next()
            P.add("act", lambda e, sq=sq, n0=n0, N=N, k4=k4: e.activation(
                out=sq[:, :, :N], in_=x_sb[:, 4 * k4:4 * k4 + 4, n0:n0 + N], func=AF.Square), [Bx], [Bsq])
            for kk in range(4):
                k = 4 * k4 + kk
                P.mm(pt[:, :N], ones_d[:], sq[:, kk, :N], k == 0, k == 15, reads=[Bones, Bsq], writes=[Bp])
        rstd, Br = rs_ring.next()
        P.add("act", lambda e, rstd=rstd, pt=pt, N=N: e.activation(
            out=rstd[:, :N], in_=pt[:, :N], func=AF.Ln, bias=EPS), [Bp], [Br])
        P.add("act", lambda e, rstd=rstd, N=N: e.activation(
            out=rstd[:, :N], in_=rstd[:, :N], func=AF.Exp, scale=-0.5), [Br], [Br])
        for k in range(16):
            t, Bt = tmp.next()
            P.add("dve", lambda e, t=t, k=k, n0=n0, N=N, seg=seg, rstd=rstd: e.scalar_tensor_tensor(
                out=t[:, :N], in0=x_sb[:, k, n0:n0 + N], scalar=gs_sb[:, k, seg:seg + 1], in1=rstd[:, :N],
                op0=ALU.mult, op1=ALU.mult), [Bx, Bgs, Br], [Bt])
            P.add("act", lambda e, t=t, k=k, n0=n0, N=N, seg=seg: e.activation(
                out=h_sb[:, k, n0:n0 + N], in_=t[:, :N], func=AF.Identity,
                bias=mod_sb[:, k, 2 + 2 * seg:3 + 2 * seg], scale=1.0), [Bt, Bmod], [Bh])

    groups = [
        (C_AQ, 512, "fm", "qn", o_qkA, 0), (C_AK, 256, "fm", "kn", o_qkA, 4),
        (C_BQ, 512, "fm", "cbf", o_bqk, 0), (C_BK, 512, "fm", "cbf", o_bqk, 4),
        (C_CU, 512, "fm", "gelu", o_cu, 0), (C_DZ, 512, "fm", "silu", o_dz, 0),
        (C_DC, 256, "fm", "copy", o_dcxb, 0), (C_DX, 512, "fm", "copy", o_dcxb, 2),
        (C_DB, 256, "fm", "copy", o_dcxb, 6),
        (C_CV, 512, "tm", "vn", o_vn, 0), (C_AV, 256, "tm", "cbf", o_av, 0),
        (C_BV, 512, "tm", "cbf", o_bv, 0), (C_DT, 16, "tm", "copy", o_ddt, 0),
    ]
    wtiles = {}

    def load_w(gi):
        if gi >= len(groups):
            return
        c0, cw = groups[gi][0], groups[gi][1]
        wt, Bw = w_ring.next()
        P.dma(wt[:, :, :cw], w_in[:, c0:c0 + cw].rearrange("(k p) c -> p k c", p=128), writes=[Bw], q="pool")
        wtiles[gi] = (wt, Bw)

    load_w(0)
    for gi, (c0, cw, mode, kind, outT, oi0) in enumerate(groups):
        load_w(gi + 1)
        wt, Bw = wtiles[gi]
        if mode == "fm":
            for j in range(cw // 128):
                isbf = kind in ("qn", "kn", "cbf")
                o, Bo = (obf if isbf else of32).next()
                for bi, (n0, N) in enumerate(TBLK):
                    pt, Bp = ps.next()
                    for k in range(16):
                        P.mm(pt[:, :N], wt[:, k, j * 128:(j + 1) * 128], h_sb[:, k, n0:n0 + N], k == 0, k == 15,
                             reads=[Bw, Bh], writes=[Bp])
                    dst = o[:, n0:n0 + N]
                    if kind in ("cbf", "copy"):
                        P.add("dve", lambda e, dst=dst, pt=pt, N=N: e.tensor_copy(out=dst, in_=pt[:, :N]), [Bp], [Bo])
                    elif kind in ("gelu", "silu"):
                        f = AF.Gelu_apprx_tanh if kind == "gelu" else AF.Silu
                        P.add("act", lambda e, dst=dst, pt=pt, N=N, f=f: e.activation(out=dst, in_=pt[:, :N], func=f),
                              [Bp], [Bo])
                    else:
                        gcol = 0 if kind == "qn" else 1
                        qs, Bqs = tmp.next()
                        sq, Bsq = tmp.next()
                        P.add("act", lambda e, qs=qs, pt=pt, N=N: e.activation(out=qs[:, :N], in_=pt[:, :N], func=AF.Identity),
                              [Bp], [Bqs])
                        P.add("act", lambda e, sq=sq, pt=pt, N=N: e.activation(out=sq[:, :N], in_=pt[:, :N], func=AF.Square),
                              [Bp], [Bsq])
                        p2, Bp2 = ps.next()
                        P.mm(p2[:, :N], ones_h[:], sq[:, :N], True, True, reads=[Bones, Bsq], writes=[Bp2])
                        rstd, Br = tmp.next()
                        P.add("act", lambda e, rstd=rstd, p2=p2, N=N: e.activation(
                            out=rstd[:, :N], in_=p2[:, :N], func=AF.Ln, bias=EPS), [Bp2], [Br])
                        P.add("act", lambda e, rstd=rstd, N=N: e.activation(
                            out=rstd[:, :N], in_=rstd[:, :N], func=AF.Exp, scale=-0.5), [Br], [Br])
                        qn, Bqn = tmp.next()
                        P.add("dve", lambda e, qn=qn, qs=qs, rstd=rstd, N=N, gcol=gcol: e.scalar_tensor_tensor(
                            out=qn[:, :N], in0=qs[:, :N], scalar=qkg_sb[:, gcol:gcol + 1], in1=rstd[:, :N],
                            op0=ALU.mult, op1=ALU.mult), [Bqs, Br, Bqkg], [Bqn])
                        if bi < 2:
                            p3, Bp3 = ps.next()
                            P.mm(p3[:, :N], rm_sb[:], qn[:, :N], True, True, reads=[Brm, Bqn], writes=[Bp3])
                            t1, Bt1 = tmp.next()
                            t2, Bt2 = tmp.next()
                            P.add("dve", lambda e, t1=t1, qn=qn, n0=n0, N=N: e.tensor_tensor(
                                out=t1[:, :N], in0=qn[:, :N], in1=rc_sb[:, n0:n0 + N], op=ALU.mult), [Bqn, Brope], [Bt1])
                            P.add("dve", lambda e, t2=t2, p3=p3, n0=n0, N=N: e.tensor_tensor(
                                out=t2[:, :N], in0=p3[:, :N], in1=rs_sb[:, n0:n0 + N], op=ALU.mult), [Bp3, Brope], [Bt2])
                            P.add("pool", lambda e, dst=dst, t1=t1, t2=t2, N=N: e.tensor_tensor(
                                out=dst, in0=t1[:, :N], in1=t2[:, :N], op=ALU.add), [Bt1, Bt2], [Bo])
                        else:
                            P.add("dve", lambda e, dst=dst, qn=qn, N=N: e.tensor_copy(out=dst, in_=qn[:, :N]), [Bqn], [Bo])
                P.dma(outT[oi0 + j], o[:], reads=[Bo])
        else:
            for ti in range(9):
                t0 = ti * 128
                M = 128 if ti < 8 else 64
                pt, Bp = ps.next()
                for k in range(16):
                    P.mm(pt[:M, :cw], h_sb[:, k, t0:t0 + M], wt[:, k, :cw], k == 0, k == 15,
                         reads=[Bw, Bh], writes=[Bp])
                if kind == "vn":
                    g, Bg = tmp.next()
                    sq, Bsq = tmp.next()
                    ss, Bss = small.next()
                    o, Bo = otm.next()
                    P.add("act", lambda e, g=g, pt=pt, M=M: e.activation(out=g[:M, :], in_=pt[:M, :], func=AF.Gelu_apprx_tanh),
                          [Bp], [Bg])
                    P.add("act", lambda e, g=g, sq=sq, M=M: e.activation(out=sq[:M, :], in_=g[:M, :], func=AF.Square),
                          [Bg], [Bsq])
                    P.add("dve", lambda e, ss=ss, sq=sq, M=M: e.tensor_reduce(out=ss[:M, 0:1], in_=sq[:M, :], axis=AX.X, op=ALU.add),
                          [Bsq], [Bss])
                    P.add("act", lambda e, ss=ss, M=M: e.activation(out=ss[:M, 1:2], in_=ss[:M, 0:1], func=AF.Ln,
                                                                     bias=EPS, scale=1.0 / 512.0), [Bss], [Bss])
                    P.add("act", lambda e, ss=ss, M=M: e.activation(out=ss[:M, 0:1], in_=ss[:M, 1:2], func=AF.Exp, scale=-0.5),
                          [Bss], [Bss])
                    P.add("dve", lambda e, o=o, g=g, ss=ss, M=M: e.scalar_tensor_tensor(
                        out=o[:M, :], in0=g[:M, :], scalar=ss[:M, 0:1], in1=sg_sb[:M, :], op0=ALU.mult, op1=ALU.mult),
                          [Bg, Bss, Bsg], [Bo])
                else:
                    o, Bo = (otb if kind == "cbf" else otm).next()
                    P.add("dve", lambda e, o=o, pt=pt, M=M, cw=cw: e.tensor_copy(out=o[:M, :cw], in_=pt[:M, :cw]), [Bp], [Bo])
                P.dma(outT[t0:t0 + M, :], o[:M, :cw], reads=[Bo])
    return C.done()


def _fm(v):
    return np.ascontiguousarray(v.reshape(16, 128).T)


def _tok_to_fm(t):
    T = t.shape[0]
    return np.ascontiguousarray(t.T.reshape(16, 128, T).transpose(1, 0, 2))


def _fm_to_tok(f):
    T = f.shape[2]
    return np.ascontiguousarray(f.transpose(1, 0, 2).reshape(2048, T).T)


def _rope_tables():
    t = np.arange(4096)
    row = (t // 64).astype(np.float32)
    col = (t % 64).astype(np.float32)
    inv = np.power(np.float32(10000.0), -np.arange(32, dtype=np.float32) / np.float32(32)).astype(np.float32)
    ang = np.concatenate([row[:, None] * inv, col[:, None] * inv], axis=-1).astype(np.float32)
    cs = np.repeat(np.cos(ang), 2, axis=1).T.astype(np.float32)
    sn = np.repeat(np.sin(ang), 2, axis=1).T.astype(np.float32)
    return np.ascontiguousarray(cs), np.ascontiguousarray(sn)


def _rmat():
    r = np.zeros((128, 128), np.float32)
    for i in range(64):
        r[2 * i + 1, 2 * i] = -1.0
        r[2 * i, 2 * i + 1] = 1.0
    return r


_NC_CACHE = {}


def _get_nc(name, builder):
    if name not in _NC_CACHE:
        _NC_CACHE[name] = builder()
    return _NC_CACHE[name]


def run_k1(l, xfm, m, p):
    cs, sn = _rope_tables()
    rm = _rmat()
    in_maps = []
    w_in = np.ascontiguousarray(p["w_in"][l])
    qkg = np.ascontiguousarray(np.stack([p["q_norm"][l], p["k_norm"][l]], axis=1))
    sgug = np.ascontiguousarray(np.broadcast_to(p["sgu_norm"][l][None, :], (128, 512)))
    for i in range(NCORES):
        b, q = i // 4, i % 4
        modv = np.stack([_fm(p["norm_mix"][l]), _fm(m[l, b, 2048:4096]), _fm(m[l, b, 0:2048]),
                         _fm(m[l, 2, 2048:4096]), _fm(m[l, 2, 0:2048])], axis=2)
        in_maps.append({"xT": xfm[i], "modv": np.ascontiguousarray(modv), "w_in": w_in, "qkg": qkg,
                        "ropeC": np.ascontiguousarray(cs[:, q * 1024:(q + 1) * 1024]),
                        "ropeS": np.ascontiguousarray(sn[:, q * 1024:(q + 1) * 1024]),
                        "sgug": sgug, "rmat": rm})
    return _run(build_k1(), in_maps)


TK = 4352
NT = 34
SCALE = 128 ** -0.5


def build_k2():
    C = Ctx()
    nc, P = C.nc, C.P
    d_qa = C.din("qa", [128, TK], BF16); d_ka = C.din("ka", [128, TK], BF16); d_va = C.din("va", [TK, 128], BF16)
    d_qb = C.din("qb", [128, TK], BF16); d_kb = C.din("kb", [128, TK], BF16); d_vb = C.din("vb", [TK, 128], BF16)
    d_nab = C.din("nab", [5, 128, 7, 128])
    d_cu = C.din("cu", [128, TK]); d_vn = C.din("vn", [TK, 128])
    d_wsT = C.din("wsT", [128, 128]); d_bsb = C.din("bsb", [128, 512])
    d_dx = C.din("dx", [128, TK]); d_db = C.din("db", [128, TK]); d_dc = C.din("dc", [128, TK]); d_dz = C.din("dz", [128, TK])
    d_ddt = C.din("ddt", [TK, 4])
    d_cw = C.din("cw", [128, 3, 5]); d_cb = C.din("cb", [128, 3])
    d_dtb = C.din("dtb", [128, NT * 4]); d_alog = C.din("alog", [128, NT * 4]); d_dsk = C.din("dsk", [128, 2])
    d_tri = C.din("tri", [2, 128, 128]); d_ident = C.din("ident", [128, 128])
    o_a = C.dout("oa", [128, TK]); o_b = C.dout("ob", [128, TK]); o_c = C.dout("oc", [128, TK]); o_d = C.dout("od", [128, TK])

    banks = C.psum_ring(8).items
    C.push()
    psS = Ring(banks[0:3]); psO = Ring(banks[3:5]); psD = Ring(banks[5:7]); psX = Ring(banks[7:8])
    ones_bf = C.sb([128, 128], BF16); Bones = Buf()
    P.add("pool", lambda e: e.memset(ones_bf[:], 1.0), [], [Bones])
    e_ring = C.ring(3, [128, 896], BF16)
    tmp = C.ring(4, [128, 896])
    stage = C.ring(3, [128, 512])

    def load_qkv(dq, dk, dv):
        q = C.sb([128, TK], BF16); k = C.sb([128, TK], BF16); v = C.sb([128, NT, 128], BF16)
        Bq, Bk, Bv = Buf(), Buf(), Buf()
        P.dma(q[:], dq, writes=[Bq]); P.dma(k[:], dk, writes=[Bk])
        P.dma(v[:], dv.rearrange("(t p) c -> p t c", p=128), writes=[Bv])
        return (q, Bq), (k, Bk), (v, Bv)

    def finish_block(pO, BpO, pD, BpD, nq, dst):
        rd, Brd = tmp.next()
        P.add("dve", lambda e: e.reciprocal(out=rd[:, :nq], in_=pD[:, :nq]), [BpD], [Brd])
        st, Bst = stage.next()
        P.add("dve", lambda e: e.tensor_tensor(out=st[:, :nq], in0=pO[:, :nq], in1=rd[:, :nq], op=ALU.mult),
              [BpO, Brd], [Bst])
        P.dma(dst, st[:, :nq], reads=[Bst])

    def attn_dense(Q, K, V, q0, nq, ktiles, dst):
        (q, Bq), (k, Bk), (v, Bv) = Q, K, V
        pO, BpO = psO.next(); pD, BpD = psD.next()
        n = len(ktiles)

        def S(kt):
            pS, BpS = psS.next()
            P.mm(pS[:, :nq], k[:, kt * 128:(kt + 1) * 128], q[:, q0:q0 + nq], True, True, reads=[Bk, Bq], writes=[BpS])
            return pS, BpS
        cur = S(ktiles[0])
        for ii, kt in enumerate(ktiles):
            pS, BpS = cur
            if ii + 1 < n:
                cur = S(ktiles[ii + 1])
            E, BE = e_ring.next()
            P.add("act", lambda e, E=E, pS=pS: e.activation(out=E[:, :nq], in_=pS[:, :nq], func=AF.Exp, scale=SCALE),
                  [BpS], [BE])
            P.mm(pO[:, :nq], v[:, kt, :], E[:, :nq], ii == 0, ii == n - 1, reads=[Bv, BE], writes=[BpO])
            P.mm(pD[:, :nq], ones_bf[:], E[:, :nq], ii == 0, ii == n - 1, reads=[Bones, BE], writes=[BpD])
        finish_block(pO, BpO, pD, BpD, nq, dst)

    QA, KA, VA = load_qkv(d_qa, d_ka, d_va)
    attn_dense(QA, KA, VA, 0, 256, [0, 1], o_a[:, 0:256])
    for qb_ in range(8):
        q0 = 256 + qb_ * 512
        attn_dense(QA, KA, VA, q0, 512, list(range(NT)), o_a[:, q0:q0 + 512])

    QB, KB, VB = load_qkv(d_qb, d_kb, d_vb)
    nab = C.sb([128, 5, 7 * 128]); Bnab = Buf()
    for t5 in range(5):
        P.dma(nab[:, t5, :], d_nab[t5].rearrange("p t q -> p (t q)"), writes=[Bnab])
    attn_dense(QB, KB, VB, 0, 256, [0, 1], o_b[:, 0:256])
    (qb, Bqb), (kb, Bkb), (vb, Bvb) = QB, KB, VB
    for m in range(32):
        rs = min(max(2 * m - 4, 0), 54)
        tab = {0: 0, 1: 1, 30: 3, 31: 4}.get(m, 2)
        q0 = 256 + m * 128
        ktl = [0, 1] + [2 + rs // 2 + t for t in range(5)]
        pA, BpA = psS.next(); pB, BpB = psS.next()
        for ii, kt in enumerate(ktl):
            pt, Bpt, off = (pA, BpA, ii * 128) if ii < 4 else (pB, BpB, (ii - 4) * 128)
            P.mm(pt[:, off:off + 128], kb[:, kt * 128:(kt + 1) * 128], qb[:, q0:q0 + 128], True, True,
                 reads=[Bkb, Bqb], writes=[Bpt])
        sc, Bsc = tmp.next()
        P.add("dve", lambda e, sc=sc, pA=pA, tab=tab: e.scalar_tensor_tensor(
            out=sc[:, 0:512], in0=pA[:, 0:512], scalar=SCALE, in1=nab[:, tab, 0:512], op0=ALU.mult, op1=ALU.add),
              [BpA, Bnab], [Bsc])
        P.add("dve", lambda e, sc=sc, pB=pB, tab=tab: e.scalar_tensor_tensor(
            out=sc[:, 512:896], in0=pB[:, 0:384], scalar=SCALE, in1=nab[:, tab, 512:896], op0=ALU.mult, op1=ALU.add),
              [BpB, Bnab], [Bsc])
        E, BE = e_ring.next()
        P.add("act", lambda e, E=E, sc=sc: e.activation(out=E[:, :], in_=sc[:, :], func=AF.Exp), [Bsc], [BE])
        pO, BpO = psO.next(); pD, BpD = psD.next()
        for ii, kt in enumerate(ktl):
            P.mm(pO[:, :128], vb[:, kt, :], E[:, ii * 128:(ii + 1) * 128], ii == 0, ii == 6, reads=[Bvb, BE], writes=[BpO])
            P.mm(pD[:, :128], ones_bf[:], E[:, ii * 128:(ii + 1) * 128], ii == 0, ii == 6, reads=[Bones, BE], writes=[BpD])
        finish_block(pO, BpO, pD, BpD, 128, o_b[:, q0:q0 + 128])

    cu = C.sb([128, TK]); Bcu = Buf()
    vn = C.sb([128, NT, 128]); Bvn = Buf()
    wsT = C.sb([128, 128]); bsb = C.sb([128, 512]); Bws = Buf()
    P.dma(cu[:], d_cu, writes=[Bcu])
    P.dma(vn[:], d_vn.rearrange("(t p) c -> p t c", p=128), writes=[Bvn])
    P.dma(wsT[:], d_wsT, writes=[Bws]); P.dma(bsb[:], d_bsb, writes=[Bws])
    for g0 in range(0, NT, 4):
        ng = min(4, NT - g0)
        pt, Bpt = psX.next()
        for t in range(ng):
            P.mm(pt[:, t * 128:(t + 1) * 128], vn[:, g0 + t, :], wsT[:], True, True, reads=[Bvn, Bws], writes=[Bpt])
        w = ng * 128
        t1, Bt1 = tmp.next()
        P.add("dve", lambda e, t1=t1, pt=pt, w=w: e.tensor_tensor(out=t1[:, :w], in0=pt[:, :w], in1=bsb[:, :w], op=ALU.add),
              [Bpt, Bws], [Bt1])
        st, Bst = stage.next()
        P.add("pool", lambda e, st=st, t1=t1, w=w, g0=g0: e.tensor_tensor(
            out=st[:, :w], in0=t1[:, :w], in1=cu[:, g0 * 128:g0 * 128 + w], op=ALU.mult), [Bt1, Bcu], [Bst])
        P.dma(o_c[:, g0 * 128:g0 * 128 + w], st[:, :w], reads=[Bst])
    C.pop()

    C.push()
    psR = Ring(banks[0:8])
    raw = [C.sb([128, TK]) for _ in range(3)]; Braw = [Buf() for _ in range(3)]
    cv = [C.sb([128, TK]) for _ in range(3)]; Bcv = [Buf() for _ in range(3)]
    for i3, dd in enumerate((d_dx, d_db, d_dc)):
        P.dma(raw[i3][:], dd, writes=[Braw[i3]])
    dz = C.sb([128, TK]); Bdz = Buf()
    P.dma(dz[:], d_dz, writes=[Bdz])
    cw = C.sb([128, 3, 5]); cb = C.sb([128, 3]); Bcw = Buf()
    P.dma(cw[:], d_cw, writes=[Bcw]); P.dma(cb[:], d_cb, writes=[Bcw])
    ddt = C.sb([128, NT, 4]); Bddt = Buf()
    P.dma(ddt[:], d_ddt.rearrange("(t p) c -> p t c", p=128), writes=[Bddt])
    dtb = C.sb([128, NT * 4]); alog = C.sb([128, NT * 4]); dsk = C.sb([128, 2]); Bsm = Buf()
    P.dma(dtb[:], d_dtb, writes=[Bsm]); P.dma(alog[:], d_alog, writes=[Bsm]); P.dma(dsk[:], d_dsk, writes=[Bsm])
    tri = C.sb([128, 2, 128]); ident = C.sb([128, 128]); Btri = Buf()
    P.dma(tri[:], d_tri.rearrange("d p i -> p d i"), writes=[Btri]); P.dma(ident[:], d_ident, writes=[Btri])
    ones_f = C.sb([128, 128]); Bof = Buf()
    P.add("pool", lambda e: e.memset(ones_f[:], 1.0), [], [Bof])

    for i3 in range(3):
        for (s0, L) in ((0, 256), (256, 4096)):
            P.add("dve", lambda e, i3=i3, s0=s0, L=L: e.tensor_scalar(
                out=cv[i3][:, s0:s0 + L], in0=raw[i3][:, s0:s0 + L], scalar1=cw[:, i3, 2:3], scalar2=None, op0=ALU.mult),
                  [Braw[i3], Bcw], [Bcv[i3]])
            for kk in (0, 1, 3, 4):
                d = kk - 2
                lo, hi = max(0, -d), min(L, L - d)
                P.add("dve", lambda e, i3=i3, s0=s0, lo=lo, hi=hi, d=d, kk=kk: e.scalar_tensor_tensor(
                    out=cv[i3][:, s0 + lo:s0 + hi], in0=raw[i3][:, s0 + lo + d:s0 + hi + d], scalar=cw[:, i3, kk:kk + 1],
                    in1=cv[i3][:, s0 + lo:s0 + hi], op0=ALU.mult, op1=ALU.add), [Braw[i3], Bcw, Bcv[i3]], [Bcv[i3]])
        P.add("act", lambda e, i3=i3: e.activation(out=cv[i3][:, :], in_=cv[i3][:, :], func=AF.Silu, bias=cb[:, i3:i3 + 1]),
              [Bcv[i3], Bcw], [Bcv[i3]])
    xc, Bc_, Cc = cv
    Bxc, BBc, BCc = Bcv
    xtm = raw[0][:].rearrange("p (t c) -> p t c", c=128)
    btm = raw[1][:].rearrange("p (t c) -> p t c", c=128)
    Bxtm, Bbtm = Braw[0], Braw[1]
    for (src, Bsrc, dst, Bdst) in ((xc, Bxc, xtm, Bxtm), (Bc_, BBc, btm, Bbtm)):
        for g0 in range(0, NT, 4):
            ng = min(4, NT - g0)
            pt, Bpt = psR.next()
            for t in range(ng):
                P.add("pe", lambda e, pt=pt, t=t, src=src, g0=g0: e.transpose(
                    pt[:, t * 128:(t + 1) * 128], src[:, (g0 + t) * 128:(g0 + t + 1) * 128], ident[:]),
                      [Bsrc, Btri], [Bpt])
            P.add("dve", lambda e, pt=pt, dst=dst, g0=g0, ng=ng: e.tensor_copy(
                out=dst[:, g0:g0 + ng, :], in_=pt[:, :ng * 128].rearrange("p (t c) -> p t c", c=128)), [Bpt], [Bdst])
    sm = [C.sb([128, NT * 4]) for _ in range(5)]; Bs_ = [Buf() for _ in range(5)]
    ddf = ddt[:].rearrange("p t c -> p (t c)")
    P.add("dve", lambda e: e.tensor_tensor(out=sm[0][:], in0=ddf, in1=dtb[:], op=ALU.add), [Bddt, Bsm], [Bs_[0]])
    P.add("act", lambda e: e.activation(out=sm[1][:], in_=sm[0][:], func=AF.Abs), [Bs_[0]], [Bs_[1]])
    P.add("act", lambda e: e.activation(out=sm[1][:], in_=sm[1][:], func=AF.Exp, scale=-1.0), [Bs_[1]], [Bs_[1]])
    P.add("act", lambda e: e.activation(out=sm[1][:], in_=sm[1][:], func=AF.Ln, bias=1.0), [Bs_[1]], [Bs_[1]])
    P.add("dve", lambda e: e.tensor_scalar(out=sm[2][:], in0=sm[0][:], scalar1=0.0, scalar2=None, op0=ALU.max), [Bs_[0]], [Bs_[2]])
    dt_t, Bdt = sm[3], Bs_[3]
    P.add("dve", lambda e: e.tensor_tensor(out=dt_t[:], in0=sm[2][:], in1=sm[1][:], op=ALU.add), [Bs_[1], Bs_[2]], [Bdt])
    P.add("act", lambda e: e.activation(out=sm[0][:], in_=alog[:], func=AF.Exp), [Bsm, Bs_[0]], [Bs_[0]])
    a_t, Ba = sm[4], Bs_[4]
    P.add("dve", lambda e: e.scalar_tensor_tensor(out=a_t[:], in0=sm[0][:], scalar=-1.0, in1=dt_t[:], op0=ALU.mult, op1=ALU.mult),
          [Bs_[0], Bdt], [Ba])
    dt3 = dt_t[:].rearrange("p (t c) -> p t c", c=4)
    a3 = a_t[:].rearrange("p (t c) -> p t c", c=4)

    Y = C.sb([128, TK]); BY = Buf()
    hp = [C.sb([128, 128]) for _ in range(2)]; Bhp = [Buf(), Buf()]
    t128 = C.ring(12, [128, 128])
    t64 = C.ring(6, [128, 128])
    tsm = C.ring(8, [128, 4])
    for d in range(2):
        order = [0, 1] + list(range(2, NT)) if d == 0 else [1, 0] + list(range(NT - 1, 1, -1))
        for r in range(2):
            P.add("pool", lambda e, r=r: e.memset(hp[r][:], 0.0), [], [Bhp[r]])
        last = 127 if d == 0 else 0
        for c in order:
            cs = slice(c * 128, (c + 1) * 128)
            acol = a3[:, c, 2 * d:2 * d + 2]
            pc, Bpc = psR.next()
            P.mm(pc[:, 0:2], tri[:, d, :], acol, True, True, reads=[Btri, Ba], writes=[Bpc])
            cc, Bcc = tsm.next()
            P.add("dve", lambda e, cc=cc, pc=pc: e.tensor_copy(out=cc[:, 0:2], in_=pc[:, 0:2]), [Bpc], [Bcc])
            pr, Bpr = psR.next()
            for r in range(2):
                abc, Babc = t128.next()
                P.add("dve", lambda e, abc=abc, c=c, r=r, d=d: e.tensor_scalar(
                    out=abc[:], in0=ones_f[:], scalar1=a3[:, c, 2 * d + r:2 * d + r + 1], scalar2=None, op0=ALU.mult),
                      [Bof, Ba], [Babc])
                P.mm(pr[:, r * 128:(r + 1) * 128], abc[:], tri[:, d, :], True, True, reads=[Babc, Btri], writes=[Bpr])
            pss, Bpss = psR.next()
            P.mm(pss[:, 0:128], Bc_[:, cs], Cc[:, cs], True, True, reads=[BBc, BCc], writes=[Bpss])
            xw, Bxw = t64.next()
            py, Bpy = psR.next()
            tot, Btot = tsm.next()
            P.add("dve", lambda e, tot=tot, pr=pr, last=last: e.tensor_copy(
                out=tot[:, 0:2], in_=pr[:, 0:256].rearrange("p (r i) -> p r i", i=128)[:, :, last]), [Bpr], [Btot])
            dec, Bdec = tsm.next()
            for r in range(2):
                P.add("act", lambda e, dec=dec, cc=cc, tot=tot, r=r: e.activation(
                    out=dec[:, r:r + 1], in_=cc[:, r:r + 1], func=AF.Exp, scale=-1.0, bias=tot[:, r:r + 1]),
                      [Bcc, Btot], [Bdec])
            P.add("act", lambda e, dec=dec, tot=tot: e.activation(out=dec[:, 2:4], in_=tot[:, 0:2], func=AF.Exp),
                  [Btot, Bdec], [Bdec])
            first = True
            for r in range(2):
                hs = slice(r * 64, (r + 1) * 64)
                arg, Barg = t128.next()
                P.add("dve", lambda e, arg=arg, pr=pr, cc=cc, r=r: e.tensor_scalar(
                    out=arg[:], in0=pr[:, r * 128:(r + 1) * 128], scalar1=cc[:, r:r + 1], scalar2=0.0,
                    op0=ALU.subtract, op1=ALU.min), [Bpr, Bcc], [Barg])
                P.add("act", lambda e, arg=arg: e.activation(out=arg[:], in_=arg[:], func=AF.Exp), [Barg], [Barg])
                P.add("pool", lambda e, arg=arg, d=d: e.tensor_tensor(out=arg[:], in0=arg[:], in1=tri[:, d, :], op=ALU.mult),
                      [Barg, Btri], [Barg])
                mt, Bmt = t128.next()
                P.add("dve", lambda e, mt=mt, pss=pss, arg=arg: e.tensor_tensor(
                    out=mt[:], in0=pss[:, 0:128], in1=arg[:], op=ALU.mult), [Bpss, Barg], [Bmt])
                xd, Bxd = t64.next()
                P.add("pool", lambda e, xd=xd: e.memset(xd[:], 0.0), [], [Bxd])
                P.add("dve", lambda e, xd=xd, c=c, r=r, hs=hs, d=d: e.tensor_scalar(
                    out=xd[:, hs], in0=xtm[:, c, hs], scalar1=dt3[:, c, 2 * d + r:2 * d + r + 1], scalar2=None, op0=ALU.mult),
                      [Bxtm, Bdt, Bxd], [Bxd])
                P.add("dve", lambda e, xw=xw, xd=xd, dec=dec, r=r, hs=hs: e.tensor_scalar(
                    out=xw[:, hs], in0=xd[:, hs], scalar1=dec[:, r:r + 1], scalar2=None, op0=ALU.mult), [Bxd, Bdec], [Bxw])
                ecr, Becr = t128.next()
                P.add("act", lambda e, ecr=ecr, pr=pr, r=r: e.activation(
                    out=ecr[:], in_=pr[:, r * 128:(r + 1) * 128], func=AF.Exp), [Bpr], [Becr])
                P.add("pool", lambda e, ecr=ecr, cs=cs: e.tensor_tensor(out=ecr[:], in0=ecr[:], in1=Cc[:, cs], op=ALU.mult),
                      [Becr, BCc], [Becr])
                P.mm(py[:, 0:128], xd[:], mt[:], first, False, reads=[Bxd, Bmt], writes=[Bpy])
                first = False
                P.mm(py[:, 0:128], hp[r][:], ecr[:], False, r == 1, reads=[Bhp[r], Becr], writes=[Bpy])
            if d == 0:
                P.add("dve", lambda e, py=py, cs=cs: e.tensor_copy(out=Y[:, cs], in_=py[:, 0:128]), [Bpy], [BY])
            else:
                P.add("dve", lambda e, py=py, cs=cs: e.tensor_tensor(out=Y[:, cs], in0=py[:, 0:128], in1=Y[:, cs], op=ALU.add),
                      [Bpy, BY], [BY])
            pst, Bpst = psR.next()
            P.mm(pst[:, 0:128], btm[:, c, :], xw[:], True, True, reads=[Bbtm, Bxw], writes=[Bpst])
            for r in range(2):
                hs = slice(r * 64, (r + 1) * 64)
                P.add("dve", lambda e, r=r, hs=hs, dec=dec, pst=pst: e.scalar_tensor_tensor(
                    out=hp[r][:, hs], in0=hp[r][:, hs], scalar=dec[:, 2 + r:3 + r], in1=pst[:, hs], op0=ALU.mult, op1=ALU.add),
                      [Bhp[r], Bdec, Bpst], [Bhp[r]])
    dsum = C.sb([128, 1]); Bds = Buf()
    P.add("dve", lambda e: e.tensor_tensor(out=dsum[:], in0=dsk[:, 0:1], in1=dsk[:, 1:2], op=ALU.add), [Bsm], [Bds])
    for c0 in range(0, TK, 1088):
        sl = slice(c0, c0 + 1088)
        P.add("dve", lambda e, sl=sl: e.scalar_tensor_tensor(out=Y[:, sl], in0=xc[:, sl], scalar=dsum[:, 0:1], in1=Y[:, sl],
                                                              op0=ALU.mult, op1=ALU.add), [Bxc, Bds, BY], [BY])
        P.add("pool", lambda e, sl=sl: e.tensor_tensor(out=Y[:, sl], in0=Y[:, sl], in1=dz[:, sl], op=ALU.mult), [BY, Bdz], [BY])
        P.dma(o_d[:, sl], Y[:, sl], reads=[BY])
    C.pop()
    return C.done()


def _na_tables(rpb_l):
    out = np.zeros((4, 5, 7, 128, 128), np.float32)
    specs = [(0, 0), (2, 0), (8, 4), (60, 54), (62, 54)]
    kk = np.arange(640)
    qq = np.arange(128)
    for ti, (r0, rs) in enumerate(specs):
        krow = rs + kk // 64
        kcol = kk % 64
        qrow = r0 + qq // 64
        qcol = qq % 64
        kr0 = np.clip(qrow - 4, 0, 56)
        wc0 = np.clip(qcol - 8, 0, 48)
        ok = ((krow[:, None] >= kr0[None, :]) & (krow[:, None] < kr0[None, :] + 8) &
              (kcol[:, None] >= wc0[None, :]) & (kcol[:, None] < wc0[None, :] + 16))
        drow = np.clip(krow[:, None] - qrow[None, :] + 7, 0, 14)
        dcol = np.clip(kcol[:, None] - qcol[None, :], -15, 15) + 15
        for h in range(4):
            g = rpb_l[h][drow, dcol]
            g = np.where(ok, g, np.float32(-30000.0))
            out[h, ti, 2:7] = g.reshape(5, 128, 128)
    return np.ascontiguousarray(out.transpose(0, 1, 3, 2, 4))


def _tri_consts():
    j = np.arange(128)[:, None]
    i = np.arange(128)[None, :]
    return np.ascontiguousarray(np.stack([(j <= i), (j >= i)]).astype(np.float32))


def _gather_tok(res, key, b, fm):
    parts = [np.asarray(res[4 * b + q][key]) for q in range(4)]
    if fm:
        return np.concatenate([p_[:, :, 1024:] for p_ in parts] + [p_[:, :, :1024] for p_ in parts], axis=2)
    return np.concatenate([p_[1024:] for p_ in parts] + [p_[:1024] for p_ in parts], axis=0)


def run_k2(l, r1, p):
    nab = _na_tables(p["rpb"][l])
    tri = _tri_consts()
    ident = np.eye(128, dtype=np.float32)
    in_maps = []
    g_ = {}
    for b in range(2):
        for key, fm in (("qkA", 1), ("bqk", 1), ("cu", 1), ("dz", 1), ("dcxb", 1), ("av", 0), ("bv", 0), ("vn", 0), ("ddt", 0)):
            g_[(b, key)] = _gather_tok(r1, key, b, fm)
    for i in range(NCORES):
        b, hh = i // 4, i % 4
        g = hh // 2
        ch = slice(hh * 128, (hh + 1) * 128)
        dcxb = g_[(b, "dcxb")]
        cwf = p["conv_w"][l]
        cbf = p["conv_b"][l]
        xs = slice(hh * 128, (hh + 1) * 128)
        bs = slice(512 + g * 128, 512 + (g + 1) * 128)
        cs_ = slice(768 + g * 128, 768 + (g + 1) * 128)
        cw = np.stack([cwf[:, xs].T, cwf[:, bs].T, cwf[:, cs_].T], axis=1)
        cb = np.stack([cbf[xs], cbf[bs], cbf[cs_]], axis=1)
        hsel = [2 * hh, 2 * hh + 1, 8 + 2 * hh, 8 + 2 * hh + 1]
        dtb4 = p["dt_bias"][l].reshape(16)[hsel]
        al4 = p["a_log"][l].reshape(16)[hsel]
        heads_p = np.repeat([2 * hh, 2 * hh + 1], 64)
        dsk = np.stack([p["d_skip"][l][0][heads_p], p["d_skip"][l][1][heads_p]], axis=1)
        in_maps.append({
            "qa": np.ascontiguousarray(g_[(b, "qkA")][hh]), "ka": np.ascontiguousarray(g_[(b, "qkA")][4 + hh // 2]),
            "va": np.ascontiguousarray(g_[(b, "av")][:, (hh // 2) * 128:(hh // 2 + 1) * 128]),
            "qb": np.ascontiguousarray(g_[(b, "bqk")][hh]), "kb": np.ascontiguousarray(g_[(b, "bqk")][4 + hh]),
            "vb": np.ascontiguousarray(g_[(b, "bv")][:, ch]),
            "nab": nab[hh],
            "cu": np.ascontiguousarray(g_[(b, "cu")][hh]), "vn": np.ascontiguousarray(g_[(b, "vn")][:, ch]),
            "wsT": np.ascontiguousarray(p["sgu_w"][l][hh].T),
            "bsb": np.ascontiguousarray(np.broadcast_to(np.tile(p["sgu_b"][l][hh], 4)[None, :], (128, 512))),
            "dx": np.ascontiguousarray(dcxb[2 + hh]), "db": np.ascontiguousarray(dcxb[6 + g]),
            "dc": np.ascontiguousarray(dcxb[g]), "dz": np.ascontiguousarray(g_[(b, "dz")][hh]),
            "ddt": np.ascontiguousarray(g_[(b, "ddt")][:, hsel]),
            "cw": np.ascontiguousarray(cw.astype(np.float32)), "cb": np.ascontiguousarray(cb.astype(np.float32)),
            "dtb": np.ascontiguousarray(np.broadcast_to(np.tile(dtb4, NT)[None, :], (128, NT * 4))),
            "alog": np.ascontiguousarray(np.broadcast_to(np.tile(al4, NT)[None, :], (128, NT * 4))),
            "dsk": np.ascontiguousarray(dsk.astype(np.float32)),
            "tri": tri, "ident": ident,
        })
    return _run(build_k2(), in_maps)


D_FF = 5632
D_FFE = 7168


def build_k3(moe):
    C = Ctx()
    nc, P = C.nc, C.P
    T = 1024 if moe else T1
    blks = TBLK[:2] if moe else TBLK
    nsp = 4 if moe else 2
    xT = C.din("xT", [128, 16, T])
    mixT = C.din("mixT", [128, 16, T])
    modv = C.din("modv", [128, 16, 11])
    w_out = C.din("w_out", [2048, 2048])
    if moe:
        router = C.din("router", [128, 16, 8]); d_ident = C.din("ident", [128, 128])
        d_sel = C.din("sel", [8, 8, 128])
        nff = D_FFE // 128
        xo = C.dout("xo", [128, 16, 1024])
        go = C.dout("gates", [1024, 8])
        h2o = C.dout("h2o", [128, 16, 1024], BF16)
    else:
        w13 = C.din("w13", [2048, 2 * D_FF]); w2 = C.din("w2", [D_FF, 2048])
        nff = D_FF // 128
        xo = C.dout("xo", [128, 16, T])
    nh = nff // nsp

    banks = C.psum_ring(8).items
    ps = Ring(banks)
    x_sb = C.sb([128, 16, T]); Bx = Buf("x")
    h_sb = C.sb([128, 16, T], BF16); Bh = Buf("h")
    mod_sb = C.sb([128, 16, 11]); Bmod = Buf()
    gs_sb = C.sb([128, 16, 2]); Bgs = Buf()
    ones_d = C.sb([128, 128]); ones_g = C.sb([128, 128]); Bones = Buf()
    tmp = C.ring(6, [128, 512])
    rs_ring = C.ring(1, [128, 512])
    sq_ring = C.ring(1, [128, 4, 512])
    for q4 in range(4):
        P.dma(x_sb[:, 4 * q4:4 * q4 + 4, :], xT[:, 4 * q4:4 * q4 + 4, :], writes=[Bx])
    P.dma(mod_sb[:], modv, writes=[Bmod])
    P.add("pool", lambda e: e.memset(ones_d[:], 1.0 / 2048.0), [], [Bones])
    P.add("pool", lambda e: e.memset(ones_g[:], 1.0 / 512.0), [], [Bones])
    for j, col in enumerate((4, 6)):
        P.add("dve", lambda e, j=j, col=col: e.scalar_tensor_tensor(
            out=gs_sb[:, :, j], in0=mod_sb[:, :, col], scalar=1.0, in1=mod_sb[:, :, 3],
            op0=ALU.add, op1=ALU.mult), [Bmod], [Bgs])

    def rstd_from(pt, Bp, N):
        rstd, Br = rs_ring.next()
        P.add("act", lambda e: e.activation(out=rstd[:, :N], in_=pt[:, :N], func=AF.Ln, bias=EPS), [Bp], [Br])
        P.add("act", lambda e: e.activation(out=rstd[:, :N], in_=rstd[:, :N], func=AF.Exp, scale=-0.5), [Br], [Br])
        return rstd, Br

    C.push()
    mix_ring = C.ring(2, [128, 4, T])
    w_ring = C.ring(2, [128, 16, 256], BF16)
    for mg in range(4):
        mix, Bmix = mix_ring.next()
        P.dma(mix[:], mixT[:, 4 * mg:4 * mg + 4, :], writes=[Bmix])
        for bi, (n0, N) in enumerate(blks):
            sq, Bsq = sq_ring.next()
            P.add("act", lambda e, sq=sq, n0=n0, N=N, mix=mix: e.activation(
                out=sq[:, :, :N], in_=mix[:, :, n0:n0 + N], func=AF.Square), [Bmix], [Bsq])
            pt, Bp = ps.next()
            for kk in range(4):
                P.mm(pt[:, :N], ones_g[:], sq[:, kk, :N], kk == 0, kk == 3, reads=[Bones, Bsq], writes=[Bp])
            rstd, Br = rstd_from(pt, Bp, N)
            for kk in range(4):
                k = 4 * mg + kk
                P.add("dve", lambda e, k=k, kk=kk, n0=n0, N=N, rstd=rstd, mix=mix: e.scalar_tensor_tensor(
                    out=h_sb[:, k, n0:n0 + N], in0=mix[:, kk, n0:n0 + N], scalar=mod_sb[:, k, 0:1], in1=rstd[:, :N],
                    op0=ALU.mult, op1=ALU.mult), [Bmix, Bmod, Br], [Bh])
    wt_next = None

    def load_wo(jj):
        wt, Bw = w_ring.next()
        P.dma(wt[:], w_out[:, jj * 256:(jj + 1) * 256].rearrange("(k p) c -> p k c", p=128), writes=[Bw], q="pool")
        return wt, Bw
    wt_next = load_wo(0)
    for jj in range(8):
        wt, Bw = wt_next
        if jj + 1 < 8:
            wt_next = load_wo(jj + 1)
        for j2 in range(2):
            j = 2 * jj + j2
            for bi, (n0, N) in enumerate(blks):
                seg = 0 if bi < 2 else 1
                pt, Bp = ps.next()
                for k in range(16):
                    P.mm(pt[:, :N], wt[:, k, j2 * 128:(j2 + 1) * 128], h_sb[:, k, n0:n0 + N], k == 0, k == 15,
                         reads=[Bw, Bh], writes=[Bp])
                P.add("dve", lambda e, pt=pt, j=j, n0=n0, N=N, seg=seg: e.scalar_tensor_tensor(
                    out=x_sb[:, j, n0:n0 + N], in0=pt[:, :N], scalar=mod_sb[:, j, 1 + seg:2 + seg], in1=x_sb[:, j, n0:n0 + N],
                    op0=ALU.mult, op1=ALU.add), [Bp, Bmod, Bx], [Bx])
    C.pop()

    C.push()
    if moe:
        rt_sb = C.sb([128, 16, 8]); ident = C.sb([128, 128]); sel = C.sb([8, 8, 128]); Brt = Buf()
        P.dma(rt_sb[:], router, writes=[Brt]); P.dma(ident[:], d_ident, writes=[Brt]); P.dma(sel[:], d_sel, writes=[Brt])
        lg = C.sb([8, 1024]); Blg = Buf()
        gate_tm = C.sb([128, 8, 8]); Bgtm = Buf()
        hf_ring = C.ring(2, [128, 512])
        sm8 = C.ring(8, [128, 8])
        sm1 = C.ring(8, [128, 1])
    for bi, (n0, N) in enumerate(blks):
        seg = 0 if bi < 2 else 1
        pt, Bp = ps.next()
        for k4 in range(4):
            sq, Bsq = sq_ring.next()
            P.add("act", lambda e, sq=sq, n0=n0, N=N, k4=k4: e.activation(
                out=sq[:, :, :N], in_=x_sb[:, 4 * k4:4 * k4 + 4, n0:n0 + N], func=AF.Square), [Bx], [Bsq])
            for kk in range(4):
                k = 4 * k4 + kk
                P.mm(pt[:, :N], ones_d[:], sq[:, kk, :N], k == 0, k == 15, reads=[Bones, Bsq], writes=[Bp])
        rstd, Br = rstd_from(pt, Bp, N)
        if moe:
            pl, Bpl = ps.next()
        for k in range(16):
            t, Bt = tmp.next()
            P.add("dve", lambda e, t=t, k=k, n0=n0, N=N, seg=seg, rstd=rstd: e.scalar_tensor_tensor(
                out=t[:, :N], in0=x_sb[:, k, n0:n0 + N], scalar=gs_sb[:, k, seg:seg + 1], in1=rstd[:, :N],
                op0=ALU.mult, op1=ALU.mult), [Bx, Bgs, Br], [Bt])
            P.add("act", lambda e, t=t, k=k, n0=n0, N=N, seg=seg: e.activation(
                out=h_sb[:, k, n0:n0 + N], in_=t[:, :N], func=AF.Identity,
                bias=mod_sb[:, k, 5 + 2 * seg:6 + 2 * seg], scale=1.0), [Bt, Bmod], [Bh])
            if moe:
                hf, Bhf = hf_ring.next()
                P.add("pool", lambda e, hf=hf, t=t, k=k, N=N: e.tensor_scalar(
                    out=hf[:, :N], in0=t[:, :N], scalar1=mod_sb[:, k, 5:6], scalar2=None, op0=ALU.add), [Bt, Bmod], [Bhf])
                P.mm(pl[0:8, :N], rt_sb[:, k, :], hf[:, :N], k == 0, k == 15, reads=[Brt, Bhf], writes=[Bpl])
        if moe:
            P.add("dve", lambda e, pl=pl, n0=n0, N=N: e.tensor_copy(out=lg[:, n0:n0 + N], in_=pl[0:8, :N]), [Bpl], [Blg])
    if moe:
        for ti in range(8):
            pt, Bp = ps.next()
            P.add("pe", lambda e, pt=pt, ti=ti: e.transpose(pt[:, 0:8], lg[0:8, ti * 128:(ti + 1) * 128], ident[0:8, 0:8]),
                  [Blg, Brt], [Bp])
            l_, Bl = sm8.next(); m1, Bm1 = sm1.next(); e1, Be1 = sm8.next(); l2, Bl2 = sm8.next(); m2, Bm2 = sm1.next()
            P.add("dve", lambda e, l_=l_, pt=pt: e.tensor_copy(out=l_[:], in_=pt[:, 0:8]), [Bp], [Bl])
            P.add("dve", lambda e, l_=l_, m1=m1: e.tensor_reduce(out=m1[:], in_=l_[:], axis=AX.X, op=ALU.max), [Bl], [Bm1])
            P.add("dve", lambda e, l_=l_, m1=m1, e1=e1: e.tensor_scalar(
                out=e1[:], in0=l_[:], scalar1=m1[:, 0:1], scalar2=None, op0=ALU.is_equal), [Bl, Bm1], [Be1])
            P.add("dve", lambda e, l_=l_, e1=e1, l2=l2: e.scalar_tensor_tensor(
                out=l2[:], in0=e1[:], scalar=-1e30, in1=l_[:], op0=ALU.mult, op1=ALU.add), [Bl, Be1], [Bl2])
            P.add("dve", lambda e, l2=l2, m2=m2: e.tensor_reduce(out=m2[:], in_=l2[:], axis=AX.X, op=ALU.max), [Bl2], [Bm2])
            s2, Bs2 = sm8.next(); ex, Bex = sm8.next(); nm1, Bnm1 = sm1.next(); ssum, Bss = sm1.next()
            P.add("dve", lambda e, l_=l_, m2=m2, s2=s2: e.tensor_scalar(
                out=s2[:], in0=l_[:], scalar1=m2[:, 0:1], scalar2=None, op0=ALU.is_ge), [Bl, Bm2], [Bs2])
            P.add("dve", lambda e, m1=m1, nm1=nm1: e.tensor_scalar(
                out=nm1[:], in0=m1[:], scalar1=-1.0, scalar2=None, op0=ALU.mult), [Bm1], [Bnm1])
            P.add("act", lambda e, l_=l_, ex=ex, nm1=nm1: e.activation(out=ex[:], in_=l_[:], func=AF.Exp, bias=nm1[:, 0:1]),
                  [Bl, Bnm1], [Bex])
            P.add("dve", lambda e, ex=ex, s2=s2: e.tensor_tensor(out=ex[:], in0=ex[:], in1=s2[:], op=ALU.mult), [Bex, Bs2], [Bex])
            P.add("dve", lambda e, ex=ex, ssum=ssum: e.tensor_reduce(out=ssum[:], in_=ex[:], axis=AX.X, op=ALU.add), [Bex], [Bss])
            P.add("dve", lambda e, ssum=ssum: e.reciprocal(out=ssum[:], in_=ssum[:]), [Bss], [Bss])
            P.add("dve", lambda e, ex=ex, ssum=ssum, ti=ti: e.tensor_scalar(
                out=gate_tm[:, ti, :], in0=ex[:], scalar1=ssum[:, 0:1], scalar2=None, op0=ALU.mult), [Bex, Bss], [Bgtm])
            p2, Bp2 = ps.next()
            P.add("pe", lambda e, p2=p2, ti=ti: e.transpose(p2[0:8, 0:128], gate_tm[:, ti, :], ident[:]), [Bgtm, Brt], [Bp2])
            P.add("dve", lambda e, p2=p2, ti=ti: e.tensor_copy(out=lg[:, ti * 128:(ti + 1) * 128], in_=p2[0:8, 0:128]),
                  [Bp2, Blg], [Blg])
        P.dma(go.rearrange("(t p) e -> p t e", p=128), gate_tm[:], reads=[Bgtm])
        for q4 in range(4):
            P.dma(xo[:, 4 * q4:4 * q4 + 4, :], x_sb[:, 4 * q4:4 * q4 + 4, :], reads=[Bx])
            P.dma(h2o[:, 4 * q4:4 * q4 + 4, :], h_sb[:, 4 * q4:4 * q4 + 4, :], reads=[Bh])
        C.pop()
        return C.done()

    act = C.sb([128, nh, T], BF16); Bact = Buf()
    w13_ring = C.ring(2, [128, 16, 256], BF16)
    w2_ring = C.ring(2, [128, nh, 128], BF16)
    gbc_ring = C.ring(1, [128, 1024]) if moe else None
    ffw = D_FFE if moe else D_FF
    for ex_i in range(8 if moe else 1):
        w13e = w13[ex_i] if moe else w13
        w2e = w2[ex_i] if moe else w2
        if moe:
            gbc, Bgbc = gbc_ring.next()
            for bi, (n0, N) in enumerate(blks):
                pt, Bp = ps.next()
                P.mm(pt[:, :N], sel[:, ex_i, :], lg[:, n0:n0 + N], True, True, reads=[Brt, Blg], writes=[Bp])
                P.add("dve", lambda e, gbc=gbc, pt=pt, n0=n0, N=N: e.tensor_copy(out=gbc[:, n0:n0 + N], in_=pt[:, :N]),
                      [Bp], [Bgbc])
        for half in range(nsp):
            def load13(f):
                wt, Bw = w13_ring.next()
                c0 = (half * nh + f) * 128
                P.dma(wt[:, :, 0:128], w13e[:, c0:c0 + 128].rearrange("(k p) c -> p k c", p=128), writes=[Bw], q="pool")
                P.dma(wt[:, :, 128:256], w13e[:, ffw + c0:ffw + c0 + 128].rearrange("(k p) c -> p k c", p=128),
                      writes=[Bw], q="pool")
                return wt, Bw
            nxt = load13(0)
            for f in range(nh):
                wt, Bw = nxt
                if f + 1 < nh:
                    nxt = load13(f + 1)
                for bi, (n0, N) in enumerate(blks):
                    pg, Bpg = ps.next(); pu, Bpu = ps.next()
                    for k in range(16):
                        P.mm(pg[:, :N], wt[:, k, 0:128], h_sb[:, k, n0:n0 + N], k == 0, k == 15, reads=[Bw, Bh], writes=[Bpg])
                    for k in range(16):
                        P.mm(pu[:, :N], wt[:, k, 128:256], h_sb[:, k, n0:n0 + N], k == 0, k == 15, reads=[Bw, Bh], writes=[Bpu])
                    t, Bt = tmp.next()
                    P.add("act", lambda e, t=t, pg=pg, N=N: e.activation(out=t[:, :N], in_=pg[:, :N], func=AF.Silu), [Bpg], [Bt])
                    P.add("dve", lambda e, t=t, pu=pu, f=f, n0=n0, N=N: e.tensor_tensor(
                        out=act[:, f, n0:n0 + N], in0=pu[:, :N], in1=t[:, :N], op=ALU.mult), [Bpu, Bt], [Bact])

            def load2(j):
                wt, Bw = w2_ring.next()
                r0 = half * nh * 128
                P.dma(wt[:], w2e[r0:r0 + nh * 128, j * 128:(j + 1) * 128].rearrange("(f p) c -> p f c", p=128),
                      writes=[Bw], q="pool")
                return wt, Bw
            nxt = load2(0)
            for j in range(16):
                wt, Bw = nxt
                if j + 1 < 16:
                    nxt = load2(j + 1)
                for bi, (n0, N) in enumerate(blks):
                    seg = 0 if bi < 2 else 1
                    pt, Bp = ps.next()
                    for f in range(nh):
                        P.mm(pt[:, :N], wt[:, f, :], act[:, f, n0:n0 + N], f == 0, f == nh - 1, reads=[Bw, Bact], writes=[Bp])
                    if moe:
                        t, Bt = tmp.next()
                        P.add("dve", lambda e, t=t, pt=pt, j=j, n0=n0, N=N, gbc=gbc: e.scalar_tensor_tensor(
                            out=t[:, :N], in0=pt[:, :N], scalar=mod_sb[:, j, 8:9], in1=gbc[:, n0:n0 + N],
                            op0=ALU.mult, op1=ALU.mult), [Bp, Bmod, Bgbc], [Bt])
                        P.add("pool", lambda e, t=t, j=j, n0=n0, N=N: e.tensor_tensor(
                            out=x_sb[:, j, n0:n0 + N], in0=x_sb[:, j, n0:n0 + N], in1=t[:, :N], op=ALU.add), [Bt, Bx], [Bx])
                    else:
                        P.add("dve", lambda e, pt=pt, j=j, n0=n0, N=N, seg=seg: e.scalar_tensor_tensor(
                            out=x_sb[:, j, n0:n0 + N], in0=pt[:, :N], scalar=mod_sb[:, j, 8 + seg:9 + seg],
                            in1=x_sb[:, j, n0:n0 + N], op0=ALU.mult, op1=ALU.add), [Bp, Bmod, Bx], [Bx])

    if moe:
        for bi, (n0, N) in enumerate(blks):
            pt, Bp = ps.next()
            for k4 in range(4):
                sq, Bsq = sq_ring.next()
                P.add("act", lambda e, sq=sq, n0=n0, N=N, k4=k4: e.activation(
                    out=sq[:, :, :N], in_=x_sb[:, 4 * k4:4 * k4 + 4, n0:n0 + N], func=AF.Square), [Bx], [Bsq])
                for kk in range(4):
                    k = 4 * k4 + kk
                    P.mm(pt[:, :N], ones_d[:], sq[:, kk, :N], k == 0, k == 15, reads=[Bones, Bsq], writes=[Bp])
            rstd, Br = rstd_from(pt, Bp, N)
            for k in range(16):
                P.add("dve", lambda e, k=k, n0=n0, N=N, rstd=rstd: e.scalar_tensor_tensor(
                    out=x_sb[:, k, n0:n0 + N], in0=x_sb[:, k, n0:n0 + N], scalar=mod_sb[:, k, 10:11], in1=rstd[:, :N],
                    op0=ALU.mult, op1=ALU.mult), [Bx, Bmod, Br], [Bx])
        for q4 in range(4):
            P.dma(xo[:, 4 * q4:4 * q4 + 4, :], x_sb[:, 4 * q4:4 * q4 + 4, :], reads=[Bx])
    else:
        for q4 in range(4):
            P.dma(xo[:, 4 * q4:4 * q4 + 4, :], x_sb[:, 4 * q4:4 * q4 + 4, :], reads=[Bx])
    C.pop()
    return C.done()


def build_k4():
    C = Ctx()
    nc, P = C.nc, C.P
    NTT = 8
    h2 = C.din("h2", [NTT, 128, 16, 1024], BF16)
    gate = C.din("gate", [128, NTT * 1024])
    w13 = C.din("w13", [2048, 2 * D_FFE]); w2 = C.din("w2", [D_FFE, 2048])
    yo = C.dout("y", [NTT, 128, 16, 1024])
    nsp = 4
    nh = (D_FFE // 128) // nsp
    ps = Ring(C.psum_ring(8).items)
    h_ring = C.ring(2, [128, 16, 1024], BF16)
    acc_ring = C.ring(1, [128, 16, 1024])
    g_ring = C.ring(2, [128, 1024])
    act = C.sb([128, nh, 1024], BF16); Bact = Buf()
    w13_ring = C.ring(3, [128, 16, 256], BF16)
    w2_ring = C.ring(3, [128, nh, 128], BF16)
    tmp = C.ring(4, [128, 512])
    blks = TBLK[:2]
    nxt_h = None

    def load_h(tt):
        h_sb, Bh = h_ring.next(); g_sb, Bg = g_ring.next()
        for q4 in range(4):
            P.dma(h_sb[:, 4 * q4:4 * q4 + 4, :], h2[tt, :, 4 * q4:4 * q4 + 4, :], writes=[Bh])
        P.dma(g_sb[:], gate[:, tt * 1024:(tt + 1) * 1024], writes=[Bg])
        return h_sb, Bh, g_sb, Bg
    nxt_h = load_h(0)
    for tt in range(NTT):
        h_sb, Bh, g_sb, Bg = nxt_h
        if tt + 1 < NTT:
            nxt_h = load_h(tt + 1)
        acc, Bacc = acc_ring.next()
        for sp in range(nsp):
            def load13(f):
                wt, Bw = w13_ring.next()
                c0 = (sp * nh + f) * 128
                P.dma(wt[:, :, 0:128], w13[:, c0:c0 + 128].rearrange("(k p) c -> p k c", p=128), writes=[Bw], q="pool")
                P.dma(wt[:, :, 128:256], w13[:, D_FFE + c0:D_FFE + c0 + 128].rearrange("(k p) c -> p k c", p=128),
                      writes=[Bw], q="pool")
                return wt, Bw
            q13 = [load13(0), load13(1)]
            for f in range(nh):
                wt, Bw = q13.pop(0)
                if f + 2 < nh:
                    q13.append(load13(f + 2))
                for bi, (n0, N) in enumerate(blks):
                    pg, Bpg = ps.next(); pu, Bpu = ps.next()
                    for k in range(16):
                        P.mm(pg[:, :N], wt[:, k, 0:128], h_sb[:, k, n0:n0 + N], k == 0, k == 15, reads=[Bw, Bh], writes=[Bpg])
                    for k in range(16):
                        P.mm(pu[:, :N], wt[:, k, 128:256], h_sb[:, k, n0:n0 + N], k == 0, k == 15, reads=[Bw, Bh], writes=[Bpu])
                    t, Bt = tmp.next()
                    P.add("act", lambda e, t=t, pg=pg, N=N: e.activation(out=t[:, :N], in_=pg[:, :N], func=AF.Silu), [Bpg], [Bt])
                    P.add("dve", lambda e, t=t, pu=pu, f=f, n0=n0, N=N: e.tensor_tensor(
                        out=act[:, f, n0:n0 + N], in0=pu[:, :N], in1=t[:, :N], op=ALU.mult), [Bpu, Bt], [Bact])

            def load2(j):
                wt, Bw = w2_ring.next()
                r0 = sp * nh * 128
                P.dma(wt[:], w2[r0:r0 + nh * 128, j * 128:(j + 1) * 128].rearrange("(f p) c -> p f c", p=128),
                      writes=[Bw], q="pool")
                return wt, Bw
            q2 = [load2(0), load2(1)]
            for j in range(16):
                wt, Bw = q2.pop(0)
                if j + 2 < 16:
                    q2.append(load2(j + 2))
                for bi, (n0, N) in enumerate(blks):
                    pt, Bp = ps.next()
                    for f in range(nh):
                        P.mm(pt[:, :N], wt[:, f, :], act[:, f, n0:n0 + N], f == 0, f == nh - 1, reads=[Bw, Bact], writes=[Bp])
                    if sp == 0:
                        P.add("dve", lambda e, pt=pt, j=j, n0=n0, N=N, acc=acc, g_sb=g_sb: e.tensor_tensor(
                            out=acc[:, j, n0:n0 + N], in0=pt[:, :N], in1=g_sb[:, n0:n0 + N], op=ALU.mult), [Bp, Bg], [Bacc])
                    else:
                        t, Bt = tmp.next()
                        P.add("dve", lambda e, t=t, pt=pt, n0=n0, N=N, g_sb=g_sb: e.tensor_tensor(
                            out=t[:, :N], in0=pt[:, :N], in1=g_sb[:, n0:n0 + N], op=ALU.mult), [Bp, Bg], [Bt])
                        P.add("pool", lambda e, t=t, j=j, n0=n0, N=N, acc=acc: e.tensor_tensor(
                            out=acc[:, j, n0:n0 + N], in0=acc[:, j, n0:n0 + N], in1=t[:, :N], op=ALU.add), [Bt, Bacc], [Bacc])
        for q4 in range(4):
            P.dma(yo[tt, :, 4 * q4:4 * q4 + 4, :], acc[:, 4 * q4:4 * q4 + 4, :], reads=[Bacc])
    return C.done()


def build_k5():
    C = Ctx()
    nc, P = C.nc, C.P
    xm = C.din("xm", [128, 16, 1024])
    ys = C.din("ys", [8, 128, 16, 1024])
    gv = C.din("gv", [128, 16, 2])
    out = C.dout("out", [128, 16, 1024])
    ps = Ring(C.psum_ring(8).items)
    x_sb = C.sb([128, 16, 1024]); Bx = Buf()
    g_sb = C.sb([128, 16, 2]); Bg = Buf()
    ones_d = C.sb([128, 128]); Bones = Buf()
    y_ring = C.ring(3, [128, 4, 1024])
    acc_ring = C.ring(2, [128, 4, 1024])
    sq_ring = C.ring(1, [128, 4, 512])
    rs_ring = C.ring(1, [128, 512])
    P.dma(g_sb[:], gv, writes=[Bg])
    P.add("pool", lambda e: e.memset(ones_d[:], 1.0 / 2048.0), [], [Bones])
    for k4 in range(4):
        ks = slice(4 * k4, 4 * k4 + 4)
        P.dma(x_sb[:, ks, :], xm[:, ks, :], writes=[Bx])
        acc, Bacc = acc_ring.next()
        for e_ in range(8):
            y, By = y_ring.next()
            P.dma(y[:], ys[e_, :, ks, :], writes=[By])
            if e_ == 0:
                continue_first = (y, By)
                continue
            eng = "dve" if e_ % 2 else "pool"
            if e_ == 1:
                y0, By0 = continue_first
                P.add(eng, lambda e, acc=acc, y=y, y0=y0: e.tensor_tensor(out=acc[:], in0=y0[:], in1=y[:], op=ALU.add),
                      [By, By0], [Bacc])
            else:
                P.add(eng, lambda e, acc=acc, y=y: e.tensor_tensor(out=acc[:], in0=acc[:], in1=y[:], op=ALU.add),
                      [By, Bacc], [Bacc])
        for kk in range(4):
            k = 4 * k4 + kk
            P.add("dve", lambda e, acc=acc, kk=kk, k=k: e.scalar_tensor_tensor(
                out=x_sb[:, k, :], in0=acc[:, kk, :], scalar=g_sb[:, k, 0:1], in1=x_sb[:, k, :], op0=ALU.mult, op1=ALU.add),
                  [Bacc, Bg, Bx], [Bx])
    for bi, (n0, N) in enumerate(TBLK[:2]):
        pt, Bp = ps.next()
        for k4 in range(4):
            sq, Bsq = sq_ring.next()
            P.add("act", lambda e, sq=sq, n0=n0, N=N, k4=k4: e.activation(
                out=sq[:, :, :N], in_=x_sb[:, 4 * k4:4 * k4 + 4, n0:n0 + N], func=AF.Square), [Bx], [Bsq])
            for kk in range(4):
                k = 4 * k4 + kk
                P.mm(pt[:, :N], ones_d[:], sq[:, kk, :N], k == 0, k == 15, reads=[Bones, Bsq], writes=[Bp])
        rstd, Br = rs_ring.next()
        P.add("act", lambda e, rstd=rstd, pt=pt, N=N: e.activation(out=rstd[:, :N], in_=pt[:, :N], func=AF.Ln, bias=EPS), [Bp], [Br])
        P.add("act", lambda e, rstd=rstd, N=N: e.activation(out=rstd[:, :N], in_=rstd[:, :N], func=AF.Exp, scale=-0.5), [Br], [Br])
        for k in range(16):
            P.add("dve", lambda e, k=k, n0=n0, N=N, rstd=rstd: e.scalar_tensor_tensor(
                out=x_sb[:, k, n0:n0 + N], in0=x_sb[:, k, n0:n0 + N], scalar=g_sb[:, k, 1:2], in1=rstd[:, :N],
                op0=ALU.mult, op1=ALU.mult), [Bx, Bg, Br], [Bx])
    for q4 in range(4):
        P.dma(out[:, 4 * q4:4 * q4 + 4, :], x_sb[:, 4 * q4:4 * q4 + 4, :], reads=[Bx])
    return C.done()


def _mix_for_core(r2, b, q, T):
    mix = np.empty((128, 16, T), np.float32)
    for mi, key in enumerate(("oa", "ob", "oc", "od")):
        for hh in range(4):
            o = np.asarray(r2[4 * b + hh][key])
            mix[:, mi * 4 + hh, :1024] = o[:, 256 + q * 1024:256 + (q + 1) * 1024]
            if T > 1024:
                mix[:, mi * 4 + hh, 1024:] = o[:, q * 64:(q + 1) * 64]
    return mix


def _modv3(l, b, m, p):
    ch = lambda r, c: _fm(m[l, r, c * 2048:(c + 1) * 2048])
    cols = [_fm(p["out_norm"][l]), ch(b, 2), ch(2, 2), _fm(p["norm_ffn"][l]), ch(b, 4), ch(b, 3), ch(2, 4), ch(2, 3),
            ch(b, 5), ch(2, 5), _fm(p["final_norm"])]
    return np.ascontiguousarray(np.stack(cols, axis=2).astype(np.float32))


def run_k3_dense(l, xfm, r2, m, p):
    in_maps = []
    w_out = np.ascontiguousarray(p["w_out"][l]); w13 = np.ascontiguousarray(p["ffn_w13"][l // 2])
    w2 = np.ascontiguousarray(p["ffn_w2"][l // 2])
    for i in range(NCORES):
        b, q = i // 4, i % 4
        in_maps.append({"xT": xfm[i], "mixT": _mix_for_core(r2, b, q, T1), "modv": _modv3(l, b, m, p),
                        "w_out": w_out, "w13": w13, "w2": w2})
    res = _run(build_k3(False), in_maps)
    return [np.asarray(r["xo"]) for r in res]


def run_moe_pre(l, xfm, r2, m, p):
    w_out = np.ascontiguousarray(p["w_out"][l])
    router = np.ascontiguousarray(p["router"][l // 2].reshape(16, 128, 8).transpose(1, 0, 2))
    ident = np.eye(128, dtype=np.float32)
    sel = np.zeros((8, 8, 128), np.float32)
    for e in range(8):
        sel[e, e, :] = 1.0
    in_maps = []
    for i in range(NCORES):
        b, q = i // 4, i % 4
        in_maps.append({"xT": np.ascontiguousarray(xfm[i][:, :, :1024]), "mixT": _mix_for_core(r2, b, q, 1024),
                        "modv": _modv3(l, b, m, p), "w_out": w_out, "router": router, "ident": ident, "sel": sel})
    return _run(build_k3(True), in_maps)


def run_moe_dense(l, r3, m, p):
    h2_all = np.ascontiguousarray(np.stack([np.asarray(r["h2o"]) for r in r3], axis=0))
    gates = np.concatenate([np.asarray(r["gates"]) for r in r3], axis=0)
    in_maps = []
    for e in range(NCORES):
        in_maps.append({"h2": h2_all, "gate": np.ascontiguousarray(np.broadcast_to(gates[None, :, e], (128, 8192))),
                        "w13": np.ascontiguousarray(p["moe_w13"][l // 2][e]), "w2": np.ascontiguousarray(p["moe_w2"][l // 2][e])})
    r4 = _run(build_k4(), in_maps)
    in_maps = []
    for i in range(NCORES):
        b = i // 4
        ys = np.ascontiguousarray(np.stack([np.asarray(r4[e]["y"][i]) for e in range(8)], axis=0))
        gv = np.ascontiguousarray(np.stack([_fm(m[l, b, 5 * 2048:6 * 2048]), _fm(p["final_norm"])], axis=2).astype(np.float32))
        in_maps.append({"xm": np.asarray(r3[i]["xo"]), "ys": ys, "gv": gv})
    r5 = _run(build_k5(), in_maps)
    return [np.asarray(r["out"]) for r in r5]


def run_moe_sparse(l, r3, m, p):
    h2tm = np.ascontiguousarray(np.concatenate(
        [np.asarray(r["h2o"]).transpose(1, 0, 2).reshape(2048, 1024).T for r in r3], axis=0))
    gates = np.concatenate([np.asarray(r["gates"]) for r in r3], axis=0)
    n_e = [int(max(1, -(-int(c) // SB))) for c in (gates > 0).sum(axis=0)]
    units = [(e, b) for e in range(8) for b in range(n_e[e])]
    U = -(-len(units) // NCORES)
    nbmax = max(n_e)
    cap = nbmax * SB
    pi = np.arange(128)
    lstrict = (pi[:, None] < pi[None, :]).astype(np.float32)
    ti_ = np.arange(64)
    ustrict = (ti_[:, None] < ti_[None, :]).astype(np.float32)
    ident = np.eye(128, dtype=np.float32)
    identb = np.eye(128, dtype=np.float32).astype(h2tm.dtype)
    in_maps, assign = [], []
    for c in range(NCORES):
        mine = [units[i] if i < len(units) else None for i in range(c, NCORES * U, NCORES)]
        assign.append(mine)
        noff = np.empty((128, U), np.float32)
        d = {"h2tm": h2tm, "lstrict": lstrict, "ustrict": ustrict, "ident": ident, "identb": identb}
        gc = []
        for u, un in enumerate(mine):
            e, b = un if un is not None else (0, None)
            noff[:, u] = -float(b * SB) if un is not None else -1.0e5
            gc.append(gates[:, e].reshape(64, 128).T)
            d["w13_%d" % u] = np.ascontiguousarray(p["moe_w13"][l // 2][e])
            d["w2_%d" % u] = np.ascontiguousarray(p["moe_w2"][l // 2][e])
        d["gcol"] = np.ascontiguousarray(np.concatenate(gc, axis=1).astype(np.float32))
        d["noff"] = noff
        in_maps.append(d)
    r4 = _run(build_k4u(U), in_maps)
    yc_all = np.zeros((8, cap, 2048), np.float32)
    pos_all = np.empty((128, 64, 8), np.int32)
    for c, mine in enumerate(assign):
        for u, un in enumerate(mine):
            if un is None:
                continue
            e, b = un
            yc_all[e, b * SB:(b + 1) * SB] = np.asarray(r4[c]["yc"])[u * SB:(u + 1) * SB]
            if b == 0:
                pos_all[:, :, e] = np.asarray(r4[c]["pos"])[:, u * 64:(u + 1) * 64]
                if float(np.asarray(r4[c]["cnt"])[0, u]) > n_e[e] * SB:
                    return None
    gat_all = gates.reshape(64, 128, 8).transpose(1, 0, 2)
    in_maps = []
    for i in range(NCORES):
        b = i // 4
        xm = np.ascontiguousarray(np.asarray(r3[i]["xo"]).transpose(1, 0, 2).reshape(2048, 1024).T)
        gb = np.stack([np.broadcast_to(m[l, b, 5 * 2048:6 * 2048][None, :], (128, 2048)),
                       np.broadcast_to(p["final_norm"][None, :], (128, 2048))], axis=1).astype(np.float32)
        in_maps.append({"xm": xm, "yc": yc_all, "pos": np.ascontiguousarray(pos_all[:, 8 * i:8 * i + 8, :]),
                        "gat": np.ascontiguousarray(gat_all[:, 8 * i:8 * i + 8, :]).astype(np.float32),
                        "gb": np.ascontiguousarray(gb)})
    r5 = _run(build_k5s(nbmax), in_maps)
    return [np.asarray(r["out"]) for r in r5]


def run_moe_layer(l, xfm, r2, m, p):
    r3 = run_moe_pre(l, xfm, r2, m, p)
    outs = run_moe_sparse(l, r3, m, p)
    if outs is not None:
        return outs
    return [_fm_to_tok(o) for o in run_moe_dense(l, r3, m, p)]


def kernel(**inputs):
    p = {k: np.asarray(v) for k, v in inputs.items()}
    x, ctx = p["x"], p["ctx"]
    m = run_mod(p["c"], p["c_ctx"], p["w_mod"], p["b_mod"])
    xfm = []
    for i in range(NCORES):
        b, q = i // 4, i % 4
        tok = np.concatenate([x[b, q * 1024:(q + 1) * 1024], ctx[b, q * 64:(q + 1) * 64]], axis=0)
        xfm.append(_tok_to_fm(tok))
    r1 = run_k1(0, xfm, m, p)
    r2 = run_k2(0, r1, p)
    xfm = run_k3_dense(0, xfm, r2, m, p)
    r1 = run_k1(1, xfm, m, p)
    r2 = run_k2(1, r1, p)
    outs = run_moe_layer(1, xfm, r2, m, p)
    out = np.empty((2, 4096, 2048), np.float32)
    for i in range(NCORES):
        b, q = i // 4, i % 4
        out[b, q * 1024:(q + 1) * 1024] = outs[i]
    return out


SB = 1280
I32 = mybir.dt.int32


def _bc_reg(e, cache, val):
    if "r" not in cache:
        cache["r"] = e.to_reg(val)
    return cache["r"]


def build_k4u(U):
    C = Ctx()
    nc, P = C.nc, C.P
    bcc = {}
    h2tm = C.din("h2tm", [8192, 2048], BF16)
    gcol_d = C.din("gcol", [128, U * 64])
    noff_d = C.din("noff", [128, U])
    w13s = [C.din("w13_%d" % u, [2048, 2 * D_FFE]) for u in range(U)]
    w2s = [C.din("w2_%d" % u, [D_FFE, 2048]) for u in range(U)]
    d_ls = C.din("lstrict", [128, 128]); d_us = C.din("ustrict", [64, 64]); d_id = C.din("ident", [128, 128])
    d_idb = C.din("identb", [128, 128], BF16)
    yo = C.dout("yc", [U * SB, 2048])
    poso = C.dout("pos", [128, U * 64], I32)
    cnto = C.dout("cnt", [128, U])
    NT = SB // 128
    Hcs = [nc.dram_tensor("Hc%d" % u, [SB, 2048], BF16).ap() for u in range(U)]
    BZ = [[Buf() for _ in range(NT)] for _ in range(U)]
    Bsc = [[Buf() for _ in range(64)] for _ in range(U)]
    banks = []
    for i in range(6):
        banks.append((C.es.enter_context(nc.psum_tensor("ps%d" % i, [128, 512], F32)), Buf()))
    bbanks = []
    for i in range(2):
        bbanks.append((C.es.enter_context(nc.psum_tensor("pb%d" % i, [128, 1024], BF16)), Buf()))
    ps = Ring(banks); pb = Ring(bbanks)
    nsp = 8
    nh = (D_FFE // 128) // nsp

    gcol = C.sb([128, U * 64]); noff = C.sb([128, U]); Bg = Buf()
    ls = C.sb([128, 128]); us = C.sb([64, 64]); ident = C.sb([128, 128]); identb = C.sb([128, 128], BF16); Bc = Buf()
    ones = C.sb([128, 128]); zer = C.sb([128, 2048], BF16)
    P.dma(gcol[:], gcol_d, writes=[Bg]); P.dma(noff[:], noff_d, writes=[Bg])
    P.dma(ls[:], d_ls, writes=[Bc]); P.dma(us[:], d_us, writes=[Bc]); P.dma(ident[:], d_id, writes=[Bc])
    P.dma(identb[:], d_idb, writes=[Bc])
    P.add("pool", lambda e: e.memset(ones[:], 1.0), [], [Bc])
    P.add("pool", lambda e: e.memset(zer[:], 0.0), [], [Bc])
    for u in range(U):
        for t in range(NT):
            P.dma(Hcs[u][t * 128:(t + 1) * 128, :], zer[:], reads=[Bc], writes=[BZ[u][t]])

    mask = C.sb([128, 64]); Bm = Buf()
    maskT = C.sb([64, 128]); BmT = Buf()
    mu = C.sb([128, 64]); Bmu = Buf()
    big = C.sb([128, 64]); posf = C.sb([128, 64]); posi = C.sb([128, 64], I32); Bpos = Buf()
    sh = C.sb([128, 64]); ng = C.sb([128, 64]); Bsh = Buf()
    cnt = C.sb([128, U]); Bcnt = Buf()
    posis = [C.sb([128, 64], I32) for _ in range(U)]; Bps = [Buf() for _ in range(U)]
    for u in range(U):
        P.add("dve", lambda e, u=u: e.tensor_single_scalar(out=mask[:], in_=gcol[:, u * 64:(u + 1) * 64], scalar=0.0,
                                                           op=ALU.is_gt), [Bg], [Bm])
        pt, Bp = ps.next()
        P.add("pe", lambda e, pt=pt: e.transpose(pt[0:64, 0:128], mask[:], ident[:]), [Bm, Bc], [Bp])
        P.add("dve", lambda e, pt=pt: e.tensor_copy(out=maskT[:], in_=pt[0:64, 0:128]), [Bp], [BmT])
        p2, Bp2 = ps.next()
        P.mm(p2[:, 0:64], maskT[:], us[:], True, True, reads=[BmT, Bc], writes=[Bp2])
        P.add("dve", lambda e, p2=p2: e.tensor_copy(out=mu[:], in_=p2[:, 0:64]), [Bp2], [Bmu])
        p3, Bp3 = ps.next()
        P.mm(p3[:, 0:64], ones[:], mu[:], True, False, reads=[Bc, Bmu], writes=[Bp3])
        P.mm(p3[:, 0:64], ls[:], mask[:], False, True, reads=[Bc, Bm], writes=[Bp3])
        P.add("dve", lambda e: e.tensor_scalar(out=big[:], in0=mask[:], scalar1=-1.0e6, scalar2=1.0e6, op0=ALU.mult, op1=ALU.add),
              [Bm], [Bpos])
        P.add("dve", lambda e, p3=p3: e.tensor_tensor(out=posf[:], in0=p3[:, 0:64], in1=big[:], op=ALU.add), [Bp3, Bpos], [Bpos])
        P.add("dve", lambda e: e.tensor_copy(out=posi[:], in_=posf[:]), [Bpos], [Bpos])
        P.dma(poso[:, u * 64:(u + 1) * 64], posi[:], reads=[Bpos])
        P.add("dve", lambda e, u=u: e.tensor_scalar(out=sh[:], in0=posf[:], scalar1=noff[:, u:u + 1], scalar2=None, op0=ALU.add),
              [Bpos, Bg], [Bsh])
        P.add("dve", lambda e: e.tensor_scalar(out=ng[:], in0=sh[:], scalar1=0.0, scalar2=1.0e6, op0=ALU.is_lt, op1=ALU.mult),
              [Bsh], [Bsh])
        P.add("dve", lambda e: e.tensor_tensor(out=sh[:], in0=sh[:], in1=ng[:], op=ALU.add), [Bsh], [Bsh])
        P.add("dve", lambda e, u=u: e.tensor_copy(out=posis[u][:], in_=sh[:]), [Bsh], [Bps[u]])
        p4, Bp4 = ps.next()
        P.mm(p4[:, 0:64], ones[:], mask[:], True, True, reads=[Bc, Bm], writes=[Bp4])
        P.add("dve", lambda e, p4=p4, u=u: e.tensor_reduce(out=cnt[:, u:u + 1], in_=p4[:, 0:64], axis=AX.X, op=ALU.add),
              [Bp4], [Bcnt])
    P.dma(cnto, cnt[:], reads=[Bcnt])

    ld_ring = C.ring(3, [128, 2048], BF16)

    def scatter_steps(u):
        steps = []
        for t in range(64):
            def step(t=t):
                ht, Bht = ld_ring.next()
                P.dma(ht[:], h2tm[t * 128:(t + 1) * 128, :], writes=[Bht])
                P.add("pool", lambda e, ht=ht: e.indirect_dma_start(
                    out=Hcs[u][:, :], out_offset=bass.IndirectOffsetOnAxis(ap=posis[u][:, t:t + 1], axis=0), in_=ht[:, :],
                    in_offset=None, bounds_check=_bc_reg(e, bcc, SB - 1), oob_is_err=False),
                    [Bht, Bps[u]] + BZ[u], [Bsc[u][t]], dma=True)
            steps.append(step)
        return steps

    hfm = C.sb([128, 16, SB], BF16); Bh = Buf()
    act = C.sb([128, nh, SB], BF16); Bact = Buf()
    acc = C.sb([128, NT, 2048]); Bacc = Buf()
    w13_ring = C.ring(3, [128, 16, 256], BF16)
    w2_ring = C.ring(2, [128, nh, 512], BF16)
    tmp = C.ring(4, [128, 512])
    nblks = [(0, 512), (512, 512), (1024, 256)]
    for s in scatter_steps(0):
        s()
    for u in range(U):
        pending = scatter_steps(u + 1) if u + 1 < U else []
        Hc, w13, w2 = Hcs[u], w13s[u], w2s[u]
        s0 = u * SB
        for st in range(NT):
            ht, Bht = ld_ring.next()
            P.dma(ht[:], Hc[st * 128:(st + 1) * 128, :], reads=Bsc[u], writes=[Bht])
            for half in range(2):
                pbt, Bpb = pb.next()
                for kk in range(8):
                    k = half * 8 + kk
                    P.add("pe", lambda e, pbt=pbt, kk=kk, k=k, ht=ht: e.transpose(
                        pbt[:, kk * 128:(kk + 1) * 128], ht[:, k * 128:(k + 1) * 128], identb[:]), [Bht, Bc], [Bpb])
                P.add("dve", lambda e, pbt=pbt, half=half, st=st: e.tensor_copy(
                    out=hfm[:, half * 8:half * 8 + 8, st * 128:(st + 1) * 128],
                    in_=pbt[:, :].rearrange("p (k s) -> p k s", s=128)), [Bpb], [Bh])
        for sp in range(nsp):
            def load13(f):
                wt, Bw = w13_ring.next()
                c0 = (sp * nh + f) * 128
                P.dma(wt[:, :, 0:128], w13[:, c0:c0 + 128].rearrange("(k p) c -> p k c", p=128), writes=[Bw], q="pool")
                P.dma(wt[:, :, 128:256], w13[:, D_FFE + c0:D_FFE + c0 + 128].rearrange("(k p) c -> p k c", p=128),
                      writes=[Bw], q="pool")
                if pending:
                    pending.pop(0)()
                return wt, Bw
            q13 = [load13(0), load13(1)]
            for f in range(nh):
                wt, Bw = q13.pop(0)
                if f + 2 < nh:
                    q13.append(load13(f + 2))
                for (n0, N) in nblks:
                    pg, Bpg = ps.next(); pu, Bpu = ps.next()
                    for k in range(16):
                        P.mm(pg[:, :N], wt[:, k, 0:128], hfm[:, k, n0:n0 + N], k == 0, k == 15, reads=[Bw, Bh], writes=[Bpg])
                    for k in range(16):
                        P.mm(pu[:, :N], wt[:, k, 128:256], hfm[:, k, n0:n0 + N], k == 0, k == 15, reads=[Bw, Bh], writes=[Bpu])
                    t_, Bt = tmp.next()
                    P.add("act", lambda e, t_=t_, pg=pg, N=N: e.activation(out=t_[:, :N], in_=pg[:, :N], func=AF.Silu), [Bpg], [Bt])
                    P.add("dve", lambda e, t_=t_, pu=pu, f=f, n0=n0, N=N: e.tensor_tensor(
                        out=act[:, f, n0:n0 + N], in0=pu[:, :N], in1=t_[:, :N], op=ALU.mult), [Bpu, Bt], [Bact])

            def load2(db):
                wt, Bw = w2_ring.next()
                r0 = sp * nh * 128
                P.dma(wt[:], w2[r0:r0 + nh * 128, db * 512:(db + 1) * 512].rearrange("(f p) c -> p f c", p=128),
                      writes=[Bw], q="pool")
                if pending:
                    pending.pop(0)()
                return wt, Bw
            nxt = load2(0)
            for db in range(4):
                wt, Bw = nxt
                if db + 1 < 4:
                    nxt = load2(db + 1)
                for st in range(NT):
                    pt, Bp = ps.next()
                    for f in range(nh):
                        P.mm(pt[:, :], act[:, f, st * 128:(st + 1) * 128], wt[:, f, :], f == 0, f == nh - 1,
                             reads=[Bw, Bact], writes=[Bp])
                    dst = acc[:, st, db * 512:(db + 1) * 512]
                    if sp == 0:
                        P.add("act", lambda e, dst=dst, pt=pt: e.activation(out=dst, in_=pt[:, :], func=AF.Identity), [Bp], [Bacc])
                    else:
                        P.add("dve", lambda e, dst=dst, pt=pt: e.tensor_tensor(out=dst, in0=pt[:, :], in1=dst, op=ALU.add),
                              [Bp, Bacc], [Bacc])
        while pending:
            pending.pop(0)()
        for st in range(NT):
            P.dma(yo[s0 + st * 128:s0 + (st + 1) * 128, :], acc[:, st, :], reads=[Bacc])
    return C.done()


def build_k5s(nb):
    C = Ctx()
    nc, P = C.nc, C.P
    CAP = nb * SB
    bcc = {}
    xm = C.din("xm", [1024, 2048])
    yc = C.din("yc", [8, CAP, 2048])
    pos = C.din("pos", [128, 8, 8], I32)
    gat = C.din("gat", [128, 8, 8])
    gb = C.din("gb", [128, 2, 2048])
    out = C.dout("out", [1024, 2048])
    pos_sb = C.sb([128, 8, 8], I32); gat_sb = C.sb([128, 8, 8]); gb_sb = C.sb([128, 2, 2048]); Bin = Buf()
    P.dma(pos_sb[:], pos, writes=[Bin]); P.dma(gat_sb[:], gat, writes=[Bin]); P.dma(gb_sb[:], gb, writes=[Bin])
    g_ring = C.ring(4, [128, 2048])
    x_ring = C.ring(2, [128, 2048])
    a_ring = C.ring(2, [128, 2048])
    sq_ring = C.ring(2, [128, 2048])
    sm = C.ring(4, [128, 2])
    for ti in range(8):
        x, Bx = x_ring.next()
        P.dma(x[:], xm[ti * 128:(ti + 1) * 128, :], writes=[Bx])
        acc, Bacc = a_ring.next()
        for e_ in range(8):
            g, Bgt = g_ring.next()
            P.add("pool", lambda e, g=g: e.memset(g[:], 0.0), [], [Bgt])
            P.add("pool", lambda e, g=g, ti=ti, e_=e_: e.indirect_dma_start(
                out=g[:, :], out_offset=None, in_=yc.rearrange("e c d -> (e c) d"),
                in_offset=bass.IndirectOffsetOnAxis(ap=pos_sb[:, ti, e_:e_ + 1], axis=0),
                element_offset=e_ * CAP * 2048,
                bounds_check=_bc_reg(e, bcc, CAP - 1), oob_is_err=False), [Bin, Bgt], [Bgt], dma=True)
            if e_ == 0:
                P.add("dve", lambda e, acc=acc, g=g, ti=ti: e.tensor_scalar(
                    out=acc[:], in0=g[:], scalar1=gat_sb[:, ti, 0:1], scalar2=None, op0=ALU.mult), [Bgt, Bin], [Bacc])
            else:
                P.add("dve", lambda e, acc=acc, g=g, ti=ti, e_=e_: e.scalar_tensor_tensor(
                    out=acc[:], in0=g[:], scalar=gat_sb[:, ti, e_:e_ + 1], in1=acc[:], op0=ALU.mult, op1=ALU.add),
                      [Bgt, Bin, Bacc], [Bacc])
        P.add("pool", lambda e, acc=acc: e.tensor_tensor(out=acc[:], in0=acc[:], in1=gb_sb[:, 0, :], op=ALU.mult), [Bacc, Bin], [Bacc])
        P.add("dve", lambda e, acc=acc, x=x: e.tensor_tensor(out=x[:], in0=x[:], in1=acc[:], op=ALU.add), [Bacc, Bx], [Bx])
        sq, Bsq = sq_ring.next(); s_, Bs = sm.next()
        P.add("act", lambda e, sq=sq, x=x: e.activation(out=sq[:], in_=x[:], func=AF.Square), [Bx], [Bsq])
        P.add("dve", lambda e, sq=sq, s_=s_: e.tensor_reduce(out=s_[:, 0:1], in_=sq[:], axis=AX.X, op=ALU.add), [Bsq], [Bs])
        P.add("act", lambda e, s_=s_: e.activation(out=s_[:, 1:2], in_=s_[:, 0:1], func=AF.Ln, bias=EPS, scale=1.0 / 2048.0), [Bs], [Bs])
        P.add("act", lambda e, s_=s_: e.activation(out=s_[:, 0:1], in_=s_[:, 1:2], func=AF.Exp, scale=-0.5), [Bs], [Bs])
        P.add("dve", lambda e, x=x, s_=s_: e.scalar_tensor_tensor(
            out=x[:], in0=x[:], scalar=s_[:, 0:1], in1=gb_sb[:, 1, :], op0=ALU.mult, op1=ALU.mult), [Bx, Bs, Bin], [Bx])
        P.dma(out[ti * 128:(ti + 1) * 128, :], x[:], reads=[Bx])
    return C.done()
```
